# Optimizing a Trainium2 kernel written in Bass

```python
import math
import jax
import jax.numpy as jnp
from jax import lax
import numpy as np


D_MODEL = 1024
BATCH = 16
SEQ = 2048
DEPTH = 1

MIX_WIDTH = D_MODEL
MLSTM_WIDTH = MIX_WIDTH // 2
ATTN_WIDTH = MIX_WIDTH - MLSTM_WIDTH
MLSTM_HEADS = 4
MLSTM_HEAD_DIM = MLSTM_WIDTH // MLSTM_HEADS
MLSTM_QKV_BLOCK = 4
MLSTM_CONV = 5
MLSTM_CHUNK = 64
ATTN_HEAD_DIM = 64
ATTN_HEADS = ATTN_WIDTH // ATTN_HEAD_DIM
DILATED_PATTERNS = ((128, 1), (512, 4), (2048, 16))
ATTN_BLOCK = 64
REL_BUCKETS = 32
REL_MAX_DIST = 1024
N_EXPERTS = 16
EC_CAPACITY_FACTOR = 2
D_FF_EXPERT = 2 * D_MODEL
NORM_EPS = 1e-6
N_MOD = 6
PROJ_WIDTHS = (MLSTM_WIDTH, MLSTM_WIDTH, MLSTM_HEADS, MLSTM_HEADS, MLSTM_HEADS, MLSTM_HEADS,
               ATTN_WIDTH, ATTN_WIDTH, ATTN_WIDTH)
PROJ_TOTAL = sum(PROJ_WIDTHS)

kernel_name = "hybrid_mlstm_dilated_attn_ec_moe"


def rms_norm(x, g):
    xf = x.astype(jnp.float32)
    y = xf * lax.rsqrt(jnp.mean(xf * xf, axis=-1, keepdims=True) + NORM_EPS)
    return (y * g.astype(jnp.float32)).astype(x.dtype)


def t5_bucket(rel):
    half = REL_BUCKETS // 2
    exact = half // 2
    n = jnp.abs(rel)
    log_ratio = jnp.log(jnp.maximum(n, 1).astype(jnp.float32) / exact) / math.log(REL_MAX_DIST / exact)
    large = jnp.minimum(exact + (log_ratio * (half - exact)).astype(jnp.int32), half - 1)
    return jnp.where(rel > 0, half, 0) + jnp.where(n < exact, n, large)


def dilated_window_attention(q, k, v, rel_bias, window, dilation):
    B, S, H, E = q.shape
    half = window // (2 * dilation)
    L = S // dilation
    nb = -(-L // ATTN_BLOCK)
    pad = nb * ATTN_BLOCK - L

    def phases(t):
        return t.reshape(B, L, dilation, H, E).transpose(0, 2, 3, 1, 4).reshape(B * dilation, H, L, E)

    qb = jnp.pad(phases(q), ((0, 0), (0, 0), (0, pad), (0, 0))).reshape(B * dilation, H, nb, ATTN_BLOCK, E)

    def key_blocks(t):
        tp = jnp.pad(phases(t), ((0, 0), (0, 0), (ATTN_BLOCK, pad + ATTN_BLOCK), (0, 0)))
        tp = tp.reshape(B * dilation, H, nb + 2, ATTN_BLOCK, E)
        return jnp.concatenate([tp[:, :, 0:nb], tp[:, :, 1:nb + 1], tp[:, :, 2:nb + 2]], axis=3)

    kb, vb = key_blocks(k), key_blocks(v)
    qi = jnp.arange(ATTN_BLOCK)[:, None]
    kj = jnp.arange(3 * ATTN_BLOCK)[None, :] - ATTN_BLOCK
    rel = kj - qi
    key_pos = jnp.arange(nb)[:, None] * ATTN_BLOCK + kj
    valid = (jnp.abs(rel) <= half)[None] & ((key_pos >= 0) & (key_pos < L))[:, None, :]
    bias = rel_bias[t5_bucket(rel * dilation)].transpose(2, 0, 1).astype(jnp.float32)
    logits = jnp.einsum('nhcqe,nhcke->nhcqk', qb, kb).astype(jnp.float32) / math.sqrt(E) + bias[:, None]
    logits = jnp.where(valid, logits, -jnp.inf)
    lse = jax.nn.logsumexp(logits, axis=-1)
    p = jnp.exp(logits - lse[..., None]).astype(v.dtype)
    o = jnp.einsum('nhcqk,nhcke->nhcqe', p, vb)
    o = o.reshape(B * dilation, H, nb * ATTN_BLOCK, E)[:, :, :L]
    o = o.reshape(B, dilation, H, L, E).transpose(0, 3, 1, 2, 4).reshape(B, S, H, E)
    lse = lse.reshape(B * dilation, H, nb * ATTN_BLOCK)[:, :, :L]
    lse = lse.reshape(B, dilation, H, L).transpose(0, 3, 1, 2).reshape(B, S, H)
    return o, lse


def dilated_attention_mixer(a_q, a_k, a_v, q_norm_g, k_norm_g, rel_bias):
    B, S, _ = a_q.shape
    heads = lambda t: t.reshape(B, S, ATTN_HEADS, ATTN_HEAD_DIM)
    q = rms_norm(heads(a_q), q_norm_g)
    k = rms_norm(heads(a_k), k_norm_g)
    v = heads(a_v)
    outs, lses = zip(*[dilated_window_attention(q, k, v, rel_bias, w, d) for w, d in DILATED_PATTERNS])
    weights = jax.nn.softmax(jnp.stack(lses), axis=0)
    o = jnp.sum(weights[..., None] * jnp.stack(outs).astype(jnp.float32), axis=0)
    return o.reshape(B, S, ATTN_WIDTH).astype(a_q.dtype)


def mlstm_chunkwise(q, k, v, i_pre, log_f):
    B, H, S, E = q.shape
    NC = S // MLSTM_CHUNK
    ch = lambda t: t.reshape((B, H, NC, MLSTM_CHUNK) + t.shape[3:])
    q, k, v, i_pre, log_f = ch(q), ch(k), ch(v), ch(i_pre), ch(log_f)
    b = jnp.cumsum(log_f, axis=-1)
    g = b[..., -1]
    a = g[..., None] - b + i_pre

    def step(carry, xs):
        C, n, m = carry
        kc, vc, ac, gc = xs
        m_new = jnp.maximum(gc + m, jnp.max(ac, axis=-1))
        w = jnp.exp(ac - m_new[..., None])
        decay = jnp.exp(gc + m - m_new)
        C_new = decay[..., None, None] * C + jnp.einsum('bhl,bhld,bhle->bhde', w, kc, vc)
        n_new = decay[..., None] * n + jnp.einsum('bhl,bhld->bhd', w, kc)
        return (C_new, n_new, m_new), (C, n, m)

    init = (jnp.zeros((B, H, E, E), jnp.float32), jnp.zeros((B, H, E), jnp.float32), jnp.zeros((B, H), jnp.float32))
    xs = tuple(jnp.moveaxis(t, 2, 0) for t in (k, v, a, g))
    _, (C_prev, n_prev, m_prev) = lax.scan(step, init, xs)
    C_prev = jnp.moveaxis(C_prev, 0, 2)
    n_prev = jnp.moveaxis(n_prev, 0, 2)
    m_prev = jnp.moveaxis(m_prev, 0, 2)

    lower = jnp.tril(jnp.ones((MLSTM_CHUNK, MLSTM_CHUNK), dtype=bool))
    D = jnp.where(lower, b[..., :, None] - b[..., None, :] + i_pre[..., None, :], -jnp.inf)
    inter = b + m_prev[..., None]
    m_t = jnp.maximum(inter, jnp.max(D, axis=-1))
    w_intra = jnp.exp(D - m_t[..., None]) * jnp.einsum('bhcte,bhcje->bhctj', q, k)
    w_inter = jnp.exp(inter - m_t)
    num = w_inter[..., None] * jnp.einsum('bhcte,bhced->bhctd', q, C_prev) + jnp.einsum('bhctj,bhcjd->bhctd', w_intra, v)
    den = w_inter * jnp.einsum('bhcte,bhce->bhct', q, n_prev) + jnp.sum(w_intra, axis=-1)
    h = num / jnp.maximum(jnp.abs(den), jnp.exp(-m_t))[..., None]
    return h.reshape(B, H, S, E)


def mlstm_mixer(x_m, o_pre, i_fw, f_fw, i_bw, f_bw, conv_w, conv_b, w_q_blk, w_k_blk, w_v_blk,
                b_igate, b_fgate, out_norm_g, skip):
    B, S, _ = x_m.shape
    xc = lax.conv_general_dilated(x_m, conv_w[:, None, :], window_strides=(1,),
                                  padding=((MLSTM_CONV // 2, MLSTM_CONV // 2),),
                                  dimension_numbers=('NWC', 'WIO', 'NWC'),
                                  feature_group_count=MLSTM_WIDTH) + conv_b
    xc = jax.nn.silu(xc)

    def blockdiag(t, w):
        y = jnp.einsum('bsgi,gij->bsgj', t.reshape(B, S, -1, MLSTM_QKV_BLOCK), w)
        return y.reshape(B, S, MLSTM_HEADS, MLSTM_HEAD_DIM).transpose(0, 2, 1, 3).astype(jnp.float32)

    q = blockdiag(xc, w_q_blk)
    k = blockdiag(xc, w_k_blk) / math.sqrt(MLSTM_HEAD_DIM)
    v = blockdiag(x_m, w_v_blk)
    gate = lambda t, bias: (t.astype(jnp.float32) + bias.astype(jnp.float32)).transpose(0, 2, 1)
    i_f, logf_f = gate(i_fw, b_igate[0]), jax.nn.log_sigmoid(gate(f_fw, b_fgate[0]))
    i_b, logf_b = gate(i_bw, b_igate[1]), jax.nn.log_sigmoid(gate(f_bw, b_fgate[1]))
    flip = lambda t: jnp.flip(t, axis=2)
    h_fwd = mlstm_chunkwise(q, k, v, i_f, logf_f)
    h_bwd = flip(mlstm_chunkwise(flip(q), flip(k), flip(v), flip(i_b), flip(logf_b)))
    h = (h_fwd + h_bwd).transpose(0, 2, 1, 3)
    h = h * lax.rsqrt(jnp.mean(h * h, axis=-1, keepdims=True) + NORM_EPS)
    h = h * out_norm_g.astype(jnp.float32).reshape(MLSTM_HEADS, MLSTM_HEAD_DIM)
    h = h.reshape(B, S, MLSTM_WIDTH).astype(x_m.dtype) + skip * xc
    return h * jax.nn.sigmoid(o_pre)


def expert_choice_ffn(h, w_router, b_router, w_gate, w_up, w_down):
    B, S, D = h.shape
    cap = (EC_CAPACITY_FACTOR * S) // N_EXPERTS
    logits = (h @ w_router + b_router).astype(jnp.float32)
    affinity = jax.nn.softmax(logits, axis=-1)
    gates, idx = lax.top_k(affinity.transpose(0, 2, 1), cap)
    bidx = jnp.arange(B)[:, None, None]
    xin = h[bidx, idx]
    hid = jax.nn.silu(jnp.einsum('becd,edf->becf', xin, w_gate)) * jnp.einsum('becd,edf->becf', xin, w_up)
    y = jnp.einsum('becf,efd->becd', hid, w_down) * gates[..., None].astype(h.dtype)
    return jnp.zeros_like(h).at[bidx, idx].add(y)


def setup_inputs(seed: int = 0) -> dict:
    key = jax.random.key(seed)
    ks = jax.random.split(key, 26)
    f32 = jnp.float32
    nrm = lambda k, shape, scale: scale * jax.random.normal(k, shape, f32)
    L = DEPTH
    H = MLSTM_HEADS
    nblk = MLSTM_WIDTH // MLSTM_QKV_BLOCK
    return {
        "x": nrm(ks[0], (BATCH, SEQ, D_MODEL), 1.0),
        "c": nrm(ks[1], (BATCH, D_MODEL), 1.0),
        "w_ada": nrm(ks[2], (L, D_MODEL, N_MOD * D_MODEL), 0.5 * D_MODEL ** -0.5),
        "b_ada": nrm(ks[3], (L, N_MOD * D_MODEL), 0.02),
        "norm1_g": 1.0 + nrm(ks[4], (L, D_MODEL), 0.02),
        "w_in": nrm(ks[5], (L, D_MODEL, PROJ_TOTAL), D_MODEL ** -0.5),
        "conv_w": nrm(ks[6], (L, MLSTM_CONV, MLSTM_WIDTH), MLSTM_CONV ** -0.5),
        "conv_b": nrm(ks[7], (L, MLSTM_WIDTH), 0.02),
        "w_q_blk": nrm(ks[8], (L, nblk, MLSTM_QKV_BLOCK, MLSTM_QKV_BLOCK), MLSTM_QKV_BLOCK ** -0.5),
        "w_k_blk": nrm(ks[9], (L, nblk, MLSTM_QKV_BLOCK, MLSTM_QKV_BLOCK), MLSTM_QKV_BLOCK ** -0.5),
        "w_v_blk": nrm(ks[10], (L, nblk, MLSTM_QKV_BLOCK, MLSTM_QKV_BLOCK), MLSTM_QKV_BLOCK ** -0.5),
        "b_igate": nrm(ks[11], (L, 2, H), 0.1),
        "b_fgate": jnp.linspace(3.0, 6.0, H, dtype=f32)[None, None, :] + nrm(ks[12], (L, 2, H), 0.1),
        "mlstm_norm_g": 1.0 + nrm(ks[13], (L, MLSTM_WIDTH), 0.02),
        "mlstm_skip": 1.0 + nrm(ks[14], (L, MLSTM_WIDTH), 0.02),
        "q_norm_g": 1.0 + nrm(ks[15], (L, ATTN_HEAD_DIM), 0.02),
        "k_norm_g": 1.0 + nrm(ks[16], (L, ATTN_HEAD_DIM), 0.02),
        "rel_bias": nrm(ks[17], (REL_BUCKETS, ATTN_HEADS), 0.5),
        "w_out": nrm(ks[18], (L, MIX_WIDTH, D_MODEL), MIX_WIDTH ** -0.5),
        "norm2_g": 1.0 + nrm(ks[19], (L, D_MODEL), 0.02),
        "w_router": nrm(ks[20], (L, D_MODEL, N_EXPERTS), D_MODEL ** -0.5),
        "b_router": nrm(ks[21], (L, N_EXPERTS), 0.01),
        "w_gate": nrm(ks[22], (L, N_EXPERTS, D_MODEL, D_FF_EXPERT), D_MODEL ** -0.5),
        "w_up": nrm(ks[23], (L, N_EXPERTS, D_MODEL, D_FF_EXPERT), D_MODEL ** -0.5),
        "w_down": nrm(ks[24], (L, N_EXPERTS, D_FF_EXPERT, D_MODEL), D_FF_EXPERT ** -0.5),
    }


def reference(x, c, w_ada, b_ada, norm1_g, w_in, conv_w, conv_b, w_q_blk, w_k_blk, w_v_blk,
              b_igate, b_fgate, mlstm_norm_g, mlstm_skip, q_norm_g, k_norm_g, rel_bias, w_out,
              norm2_g, w_router, b_router, w_gate, w_up, w_down):
    split_points = [int(s) for s in np.cumsum(PROJ_WIDTHS)[:-1]]
    for l in range(DEPTH):
        mod = jax.nn.silu(c) @ w_ada[l] + b_ada[l]
        shift1, scale1, gate1, shift2, scale2, gate2 = jnp.split(mod[:, None, :], N_MOD, axis=-1)

        h = rms_norm(x, norm1_g[l]) * (1.0 + scale1) + shift1
        proj = h @ w_in[l]
        x_m, o_pre, i_fw, f_fw, i_bw, f_bw, a_q, a_k, a_v = jnp.split(proj, split_points, axis=-1)
        y_mlstm = mlstm_mixer(x_m, o_pre, i_fw, f_fw, i_bw, f_bw, conv_w[l], conv_b[l], w_q_blk[l],
                              w_k_blk[l], w_v_blk[l], b_igate[l], b_fgate[l], mlstm_norm_g[l], mlstm_skip[l])
        y_attn = dilated_attention_mixer(a_q, a_k, a_v, q_norm_g[l], k_norm_g[l], rel_bias)
        mix = jnp.concatenate([y_mlstm, y_attn], axis=-1) @ w_out[l]
        x = x + gate1 * mix

        h2 = rms_norm(x, norm2_g[l]) * (1.0 + scale2) + shift2
        x = x + gate2 * expert_choice_ffn(h2, w_router[l], b_router[l], w_gate[l], w_up[l], w_down[l])
    return x
```

```python
import math
from contextlib import ExitStack
import numpy as np
import concourse.bass as bass
import concourse.mybir as mybir
from concourse.bass_utils import run_bass_kernel_spmd

F32 = mybir.dt.float32
BF16 = mybir.dt.bfloat16
AF = mybir.ActivationFunctionType
ALU = mybir.AluOpType
AX = mybir.AxisListType

S_LEN = 2048
D = 1024
NT = 16
NEXP = 16
CAP = 256
DFF = 2048
EPS = 1e-6
NEG = -30000.0
SAME_ENGINE_SYNC = True
N_DMA_SEMS = 24


class _Rec:
    def __init__(self):
        self.call = None

    def __getattr__(self, name):
        def f(*a, **k):
            self.call = (name, a, k)
            return self
        return f


def _bind(fn):
    rec = _Rec()
    fn(rec)
    assert rec.call is not None
    return rec.call


class Sched:
    ENGS = ["pe", "act", "dve", "pool", "sp"]

    def __init__(self, sems):
        self.prog = {e: [] for e in self.ENGS}
        self.cnt = {}
        self.res = {}
        self.waited = {e: {} for e in self.ENGS}
        self.sems = sems
        self.eng_sem = {e: "c_" + e for e in self.ENGS}
        for e in self.ENGS:
            self.cnt["c_" + e] = 0
        self.dma_names = ["d%d" % i for i in range(N_DMA_SEMS)]
        for n in self.dma_names:
            self.cnt[n] = 0
        self.dma_rr = 0
        self.dma_rr_pool = 0

    def _deps(self, reads, writes):
        deps = {}

        def add(tok):
            if tok is None:
                return
            s, v = tok
            if deps.get(s, 0) < v:
                deps[s] = v
        for r in reads:
            st = self.res.get(r)
            if st is not None:
                add(st["w"])
        for w in writes:
            st = self.res.get(w)
            if st is not None:
                add(st["w"])
                for s, v in st["r"].items():
                    add((s, v))
        return deps

    def _commit(self, tok, reads, writes):
        s, v = tok
        for r in reads:
            st = self.res.setdefault(r, {"w": None, "r": {}})
            if st["r"].get(s, 0) < v:
                st["r"][s] = v
        for w in writes:
            self.res[w] = {"w": tok, "r": {}}

    def op(self, eng, fn, reads=(), writes=()):
        deps = self._deps(reads, writes)
        own = self.eng_sem[eng]
        waits = []
        for s, v in deps.items():
            if s == own and (eng == "pe" or not SAME_ENGINE_SYNC):
                continue
            if self.waited[eng].get(s, 0) >= v:
                continue
            self.waited[eng][s] = v
            waits.append((s, v))
        self.cnt[own] += 1
        tok = (own, self.cnt[own])
        self.prog[eng].append((_bind(fn), waits, (own, 1)))
        self._commit(tok, reads, writes)

    def dma(self, eng, fn, reads=(), writes=()):
        deps = self._deps(reads, writes)
        half = len(self.dma_names) // 2
        if eng == "pool":
            name = self.dma_names[half + self.dma_rr_pool % half]
            self.dma_rr_pool += 1
        else:
            name = self.dma_names[self.dma_rr % half]
            self.dma_rr += 1
        prev = self.cnt[name]
        if prev > 0 and deps.get(name, 0) < prev:
            deps[name] = prev
        waits = []
        for s, v in deps.items():
            if self.waited[eng].get(s, 0) >= v:
                continue
            self.waited[eng][s] = v
            waits.append((s, v))
        self.cnt[name] += 16
        tok = (name, self.cnt[name])
        self.prog[eng].append((_bind(fn), waits, (name, 16)))
        self._commit(tok, reads, writes)
        return tok

    def barrier(self):
        for e in self.ENGS:
            waits = []
            for s, v in self.cnt.items():
                if v == 0:
                    continue
                if self.waited[e].get(s, 0) >= v:
                    continue
                self.waited[e][s] = v
                waits.append((s, v))
            if waits:
                self.prog[e].append((None, waits, None))

    def emit(self, block):
        sems = self.sems

        def mk(engname):
            def body(e):
                for fn, waits, inc in self.prog[engname]:
                    for s, v in waits:
                        e.wait_ge(sems[s], v)
                    if fn is not None:
                        name, a, k = fn
                        ins = getattr(e, name)(*a, **k)
                        ins.then_inc(sems[inc[0]], inc[1])
            return body
        block.tensor(mk("pe"))
        block.scalar(mk("act"))
        block.vector(mk("dve"))
        block.gpsimd(mk("pool"))
        block.sync(mk("sp"))


class Arena:
    def __init__(self, ar, nbytes):
        self.ar = ar
        self.top = 0
        self.nbytes = nbytes
        self.limit = nbytes
        self.gen = 0
        self.peak = 0

    def alloc(self, name, shape, dt, parts=128):
        esz = 2 if dt == BF16 else 4
        n = 1
        for s in shape[1:]:
            n *= s
        nb = (n * esz + 31) // 32 * 32
        off = self.top
        self.top += nb
        self.peak = max(self.peak, self.top)
        assert self.top <= self.limit, (name, self.top, self.limit)
        v = self.ar[:, off // 4: (off + nb) // 4]
        if dt == BF16:
            v = v.bitcast(BF16)
        v = v[:, 0:n]
        if len(shape) == 3:
            v = v.rearrange("p (a b) -> p a b", a=shape[1])
        elif len(shape) == 4:
            v = v.rearrange("p (a b c) -> p a b c", a=shape[1], b=shape[2])
        if shape[0] < 128:
            v = v[0:shape[0]]
        self.gen += 1
        return v

    def mark(self):
        return self.top

    def release(self, m):
        self.top = m


def build_program(debug=None, nseq=2, max_phase=9):
    nc = bass.Bass("TRN2", target_bir_lowering=False)
    dr = {}

    def din(name, shape, dt=F32):
        dr[name] = nc.dram_tensor(name, list(shape), dt, kind="ExternalInput").ap()
        return dr[name]
    x_d = din("x", [2, S_LEN, D])
    cT_d = din("cT", [128, 8, 2])
    wada_d = din("w_ada", [D, 6 * D])
    bada_fm_d = din("b_ada_fm", [128, 48])
    bada_row_d = din("b_ada_row", [1, 6 * D])
    g1_d = din("g1_fm", [128, 8])
    g2_d = din("g2_row", [1, D])
    win_d = din("w_in", [D, 2576])
    convw_d = din("convw_fm", [128, 4, 5])
    convb_d = din("convb_fm", [128, 4])
    mng_d = din("mng_fm", [128, 4])
    msk_d = din("mskip_fm", [128, 4])
    wq_d = din("wq_bd", [4, 128, 128])
    wk_d = din("wk_bd", [4, 128, 128])
    wv_d = din("wv_bd", [4, 128, 128])
    gb_d = din("gate_bias", [16, 1])
    gq_d = din("gq", [128, 1])
    gk_d = din("gk", [128, 1])
    relb_d = din("rel_bias", [32, 8])
    wout_d = din("w_out", [D, D])
    wr_d = din("wr_fm", [128, 8, 16])
    br_d = din("b_router", [1, 16])
    wg_d = din("w_gate", [NEXP, D, DFF])
    wu_d = din("w_up", [NEXP, D, DFF])
    wd_d = din("w_down", [NEXP, DFF, D])
    ident_d = din("c_ident", [128, 128])
    jrev_d = din("c_jrev", [128, 128])
    selab_d = din("c_selab", [128, 2, 128])
    maskF_d = din("c_maskF", [128, 896])
    maskB_d = din("c_maskB", [128, 896])
    iotaf_d = din("c_iotaf", [128, 256])
    iotap_d = din("c_iotap", [128, 2])
    selfb_d = din("c_selfb", [16, 8, 128])
    sele_d = din("c_sele", [16, 16, 128])
    sel2_d = din("c_sel2", [2, 2, 128])
    comb_d = din("c_comb", [16, 3, 8])
    ones128_d = din("c_ones128", [128, 128])
    blk64_d = din("c_blk64", [128, 128])
    onehot_d = din("c_onehot", [32, 3, 384])
    out_d = nc.dram_tensor("out", [2, S_LEN, D], F32, kind="ExternalOutput").ap()
    modrow_d = nc.dram_tensor("modrow_s", [2, 4 * D], F32, kind="Internal").ap()
    ftab_d = nc.dram_tensor("ftab_s", [8, 3, 384], F32, kind="Internal").ap()
    dbg = {}
    if debug:
        for nm, shp in debug.items():
            dbg[nm] = nc.dram_tensor("dbg_" + nm, list(shp), F32, kind="ExternalOutput").ap()

    ARENA_BYTES = 206 * 1024
    with ExitStack() as es:
        arena_t = es.enter_context(nc.sbuf_tensor("arena", [128, ARENA_BYTES // 4], F32))
        PSD = [es.enter_context(nc.psum_tensor("psd%d" % i, [128, 1024], F32))[:] for i in range(4)]
        PS = []
        for i in range(4):
            PS.append(PSD[i][:, 0:512])
            PS.append(PSD[i][:, 512:1024])
        names = ["c_pe", "c_act", "c_dve", "c_pool", "c_sp"] + ["d%d" % i for i in range(N_DMA_SEMS)]
        sems = {n: es.enter_context(nc.semaphore(n)) for n in names}
        S = Sched(sems)
        A = Arena(arena_t, ARENA_BYTES)
        uid = [0]

        def R(name):
            uid[0] += 1
            return "%s#%d" % (name, uid[0])

        def PB(i):
            return ("ps", i)

        class Ring:
            def __init__(self, name, n, shape=None, dt=None, banks=None):
                self.n = n
                self.i = 0
                if banks is not None:
                    self.items = [(PS[bk], PB(bk)) for bk in banks]
                    self.n = len(banks)
                else:
                    self.items = [(A.alloc(name + str(j), shape, dt), R(name + str(j))) for j in range(n)]

            def next(self):
                it = self.items[self.i % self.n]
                self.i += 1
                return it

        def load(eng, dst, src, key, reads=()):
            S.dma(eng, lambda e: e.dma_start(out=dst, in_=src), reads=list(reads), writes=[key])

        def dump(name, src, key):
            if name in dbg:
                S.dma("sp", lambda e: e.dma_start(out=dbg[name], in_=src), reads=[key], writes=["dbg_" + name])

        ident = A.alloc("ident", [128, 128], F32)
        identb = A.alloc("identb", [128, 128], BF16)
        jrev = A.alloc("jrev", [128, 128], F32)
        selab = A.alloc("selab", [128, 2, 128], F32)
        onesb = A.alloc("onesb", [128, 128], BF16)
        ones128 = A.alloc("ones128", [128, 128], F32)
        blk64 = A.alloc("blk64", [128, 128], F32)
        maskF = A.alloc("maskF", [128, 896], F32)
        maskB = A.alloc("maskB", [128, 896], F32)
        iotaf = A.alloc("iotaf", [128, 256], F32)
        iotap = A.alloc("iotap", [128, 2], F32)
        selfb = A.alloc("selfb", [16, 8, 128], F32)
        sele = A.alloc("sele", [16, 16, 128], F32)
        sel2 = A.alloc("sel2", [2, 2, 128], F32)
        comb = A.alloc("comb", [16, 3, 8], F32)
        g1 = A.alloc("g1", [128, 8], F32)
        convw = A.alloc("convw", [128, 4, 5], F32)
        convb = A.alloc("convb", [128, 4], F32)
        mng = A.alloc("mng", [128, 4], F32)
        msk = A.alloc("msk", [128, 4], F32)
        gbias = A.alloc("gbias", [16, 1], F32)
        gq = A.alloc("gq", [128, 1], F32)
        gk = A.alloc("gk", [128, 1], F32)
        wr = A.alloc("wr", [128, 8, 16], F32)
        brbc = A.alloc("brbc", [128, 16], F32)
        wqb = A.alloc("wqb", [128, 4, 128], BF16)
        wkb = A.alloc("wkb", [128, 4, 128], BF16)
        wvb = A.alloc("wvb", [128, 4, 128], BF16)
        A1 = A.alloc("A1", [128, 2, 8], F32)
        B1 = A.alloc("B1", [128, 2, 8], F32)
        CONST = "const"
        for dst, src in [(ident, ident_d), (jrev, jrev_d), (selab, selab_d), (ones128, ones128_d), (blk64, blk64_d), (maskF, maskF_d),
                         (maskB, maskB_d), (iotaf, iotaf_d), (iotap, iotap_d), (selfb, selfb_d),
                         (sele, sele_d), (sel2, sel2_d), (comb, comb_d), (g1, g1_d), (convw, convw_d),
                         (convb, convb_d), (mng, mng_d), (msk, msk_d), (gbias, gb_d), (gq, gq_d),
                         (gk, gk_d), (wr, wr_d)]:
            S.dma("sp", lambda e, dst=dst, src=src: e.dma_start(out=dst, in_=src), writes=[R("cl")])
        S.dma("sp", lambda e: e.dma_start(out=brbc, in_=br_d.partition_broadcast(128)), writes=[R("cl")])
        for dst, src in [(wqb, wq_d), (wkb, wk_d), (wvb, wv_d)]:
            S.dma("pool", lambda e, dst=dst, src=src: e.dma_start(out=dst, in_=src.rearrange("h p n -> p h n")),
                  writes=[R("cl")])
        S.barrier()
        S.op("act", lambda e: e.activation(out=identb, in_=ident, func=AF.Copy), writes=[R("cl")])
        S.op("pool", lambda e: e.memset(onesb, 1.0), writes=[R("cl")])
        S.barrier()

        m0 = A.mark()
        sc = A.alloc("sc", [128, 8, 2], F32)
        wpiece = A.alloc("wpiece", [128, 8, 1024], F32)
        bfm = A.alloc("bfm", [128, 48], F32)
        brow = A.alloc("brow", [2, 4096], F32)
        mrow = A.alloc("mrow", [2, 4096], F32)
        modfm = A.alloc("modfm", [128, 2, 8, 2], F32)
        load("sp", sc, cT_d, "sc")
        load("sp", bfm, bada_fm_d, "bfm")
        load("sp", brow[0:1, :], bada_row_d[0:1, 2048:6144], "brow0")
        load("sp", brow[1:2, :], bada_row_d[0:1, 2048:6144], "brow1")
        S.op("act", lambda e: e.activation(out=sc, in_=sc, func=AF.Silu), reads=["sc"], writes=["sc"])
        wada_v = wada_d.rearrange("(k p) n -> p k n", p=128)
        for piece in range(6):
            for k in range(8):
                load("sp", wpiece[:, k, :], wada_v[:, k, piece * 1024:(piece + 1) * 1024], ("wpiece", k))
            wp_keys = [("wpiece", k) for k in range(8)]
            if piece < 2:
                for j in range(8):
                    for k in range(8):
                        S.op("pe", lambda e, j=j, k=k: e.matmul(PS[0][:, 2 * j:2 * j + 2], lhsT=wpiece[:, k, j * 128:(j + 1) * 128],
                                                                 rhs=sc[:, k, :], start=(k == 0), stop=(k == 7)),
                             reads=wp_keys + ["sc"], writes=[PB(0)])
                S.op("dve", lambda e, piece=piece: e.tensor_copy(out=modfm[:, piece, :, :], in_=PS[0][:, 0:16].rearrange("p (j b) -> p j b", b=2)),
                     reads=[PB(0)], writes=[("modfm", piece)])
            else:
                for half in range(2):
                    for k in range(8):
                        S.op("pe", lambda e, half=half, k=k: e.matmul(PS[1][0:2, :], lhsT=sc[:, k, :],
                                                                       rhs=wpiece[:, k, half * 512:(half + 1) * 512],
                                                                       start=(k == 0), stop=(k == 7)),
                             reads=wp_keys + ["sc"], writes=[PB(1)])
                    c0 = (piece - 2) * 1024 + half * 512
                    S.op("dve", lambda e, c0=c0: e.tensor_tensor(out=mrow[:, c0:c0 + 512], in0=PS[1][0:2, :], in1=brow[:, c0:c0 + 512], op=ALU.add),
                         reads=[PB(1), "brow0", "brow1"], writes=[("mrow", c0)])
        for b in range(2):
            S.op("dve", lambda e, b=b: e.tensor_tensor(out=B1[:, b, :], in0=modfm[:, 0, :, b], in1=bfm[:, 0:8], op=ALU.add),
                 reads=[("modfm", 0), "bfm"], writes=[("B1", b)])
            S.op("dve", lambda e, b=b: e.tensor_tensor(out=A1[:, b, :], in0=modfm[:, 1, :, b], in1=bfm[:, 8:16], op=ALU.add),
                 reads=[("modfm", 1), "bfm"], writes=[("A1", b)])
            S.op("dve", lambda e, b=b: e.scalar_tensor_tensor(out=A1[:, b, :], in0=A1[:, b, :], scalar=1.0, in1=g1, op0=ALU.add, op1=ALU.mult),
                 reads=[("A1", b)], writes=[("A1", b)])
        S.dma("sp", lambda e: e.dma_start(out=modrow_d, in_=mrow), reads=[("mrow", c) for c in range(0, 4096, 512)], writes=["modrow_d"])
        S.barrier()
        A.release(m0)

        m0 = A.mark()
        relb = A.alloc("relb", [32, 8], F32)
        onehot = A.alloc("onehot", [32, 3, 384], F32)
        ftab = A.alloc("ftab", [8, 3, 384], F32)
        load("sp", relb, relb_d, "relb")
        load("sp", onehot, onehot_d, "onehot")
        for p in range(3):
            S.op("pe", lambda e, p=p: e.matmul(PS[0][0:8, 0:384], lhsT=relb, rhs=onehot[:, p, :], start=True, stop=True),
                 reads=["relb", "onehot"], writes=[PB(0)])
            S.op("act", lambda e, p=p: e.activation(out=ftab[:, p, :], in_=PS[0][0:8, 0:384], func=AF.Exp), reads=[PB(0)], writes=[("ftab", p)])
        for p in range(3):
            S.op("pe", lambda e, p=p: e.matmul(PS[1][0:8, 0:384], lhsT=ones128[0:32, 0:8], rhs=onehot[:, p, :], start=True, stop=True),
                 reads=["onehot"], writes=[PB(1)])
            S.op("dve", lambda e, p=p: e.scalar_tensor_tensor(out=ftab[:, p, :], in0=PS[1][0:8, 0:384], scalar=128.0, in1=ftab[:, p, :],
                                                                op0=ALU.mult, op1=ALU.mult),
                 reads=[PB(1), ("ftab", p)], writes=[("ftab", p)])
        S.dma("sp", lambda e: e.dma_start(out=ftab_d, in_=ftab), reads=[("ftab", p) for p in range(3)], writes=["ftab_d"])
        S.barrier()
        A.release(m0)

        pers_mark = A.mark()
        win_v = win_d.rearrange("(k p) n -> p k n", p=128)
        wout_v = wout_d.rearrange("(k p) n -> p k n", p=128)

        for b in range(nseq):
            A.release(pers_mark)
            A.limit = ARENA_BYTES - 8 * S_LEN * 2
            catT = arena_t[:, (ARENA_BYTES - 8 * S_LEN * 2) // 4: ARENA_BYTES // 4].bitcast(BF16).rearrange("p (a b) -> p a b", a=8)
            hT = A.alloc("hT", [128, 8, S_LEN], BF16)
            seq_mark = A.mark()
            xt = A.alloc("xt", [128, D], F32)
            xs = A.alloc("xs", [128, D], F32)
            junk = A.alloc("junk", [128, D], F32)
            st1 = A.alloc("st1", [128, 4], F32)
            for i in range(NT):
                load("sp", xt, x_d[b, i * 128:(i + 1) * 128, :], "xt")
                S.op("act", lambda e: e.activation(out=junk, in_=xt, func=AF.Square, accum_out=st1[:, 0:1]), reads=["xt"], writes=["junk", "st1"])
                S.op("act", lambda e: e.activation(out=st1[:, 1:2], in_=st1[:, 0:1], func=AF.Ln, scale=1.0 / D, bias=EPS), reads=["st1"], writes=["st1"])
                S.op("act", lambda e: e.activation(out=st1[:, 2:3], in_=st1[:, 1:2], func=AF.Exp, scale=-0.5), reads=["st1"], writes=["st1"])
                S.op("dve", lambda e: e.tensor_scalar(out=xs, in0=xt, scalar1=st1[:, 2:3], scalar2=None, op0=ALU.mult), reads=["xt", "st1"], writes=["xs"])
                for k in range(8):
                    bank = k // 4
                    S.op("pe", lambda e, k=k, bank=bank: e.matmul(PS[bank][:, (k % 4) * 128:(k % 4 + 1) * 128], lhsT=xs[:, k * 128:(k + 1) * 128],
                                                                   rhs=ident, start=True, stop=True),
                         reads=["xs"], writes=[PB(bank)])
                for k in range(8):
                    bank = k // 4
                    eng = "dve" if bank == 0 else "act"
                    if eng == "dve":
                        S.op("dve", lambda e, k=k, bank=bank, i=i: e.tensor_scalar(out=hT[:, k, i * 128:(i + 1) * 128],
                                                                                   in0=PS[bank][:, (k % 4) * 128:(k % 4 + 1) * 128],
                                                                                   scalar1=A1[:, b, k:k + 1], scalar2=B1[:, b, k:k + 1],
                                                                                   op0=ALU.mult, op1=ALU.add),
                             reads=[PB(bank), ("A1", b), ("B1", b)], writes=[("hT", i)])
                    else:
                        S.op("act", lambda e, k=k, bank=bank, i=i: e.activation(out=hT[:, k, i * 128:(i + 1) * 128],
                                                                                in_=PS[bank][:, (k % 4) * 128:(k % 4 + 1) * 128],
                                                                                func=AF.Identity, scale=A1[:, b, k:k + 1], bias=B1[:, b, k:k + 1]),
                             reads=[PB(bank), ("A1", b), ("B1", b)], writes=[("hT", i)])
            hT_keys = [("hT", i) for i in range(NT)]
            if b == 0 and "hT" in dbg:
                S.dma("pool", lambda e: e.dma_start(out=dbg["hT"], in_=hT), reads=hT_keys, writes=["dbg_hT"])
            S.barrier()
            A.release(seq_mark)

            Cg = A.alloc("Cg", [16, S_LEN], F32)
            Dg = A.alloc("Dg", [16, S_LEN], F32)
            utok = A.alloc("utok", [128, 128], F32)
            g_mark = A.mark()
            Zg = A.alloc("Zg", [16, S_LEN], F32)
            Lg = A.alloc("Lg", [16, S_LEN], F32)
            onesr = A.alloc("onesr", [16, S_LEN], F32)
            wgt = A.alloc("wgt", [128, 8, 16], BF16)
            S.dma("pool", lambda e: e.dma_start(out=wgt, in_=win_v[:, :, 1024:1040]), writes=["wgt"])
            S.op("pool", lambda e: e.memset(onesr, 1.0), writes=["onesr"])
            for blk in range(4):
                for k in range(8):
                    S.op("pe", lambda e, k=k, blk=blk: e.matmul(PS[0][0:16, :], lhsT=wgt[:, k, :], rhs=hT[:, k, blk * 512:(blk + 1) * 512],
                                                                 start=(k == 0), stop=(k == 7)),
                         reads=["wgt"] + hT_keys, writes=[PB(0)])
                S.op("dve", lambda e, blk=blk: e.tensor_scalar(out=Zg[:, blk * 512:(blk + 1) * 512], in0=PS[0][0:16, :], scalar1=gbias[:, 0:1],
                                                               scalar2=None, op0=ALU.add),
                     reads=[PB(0)], writes=[("Zg", blk)])
            zk = [("Zg", blk) for blk in range(4)]
            S.op("act", lambda e: e.activation(out=Lg, in_=Zg, func=AF.Exp, scale=-1.0), reads=zk, writes=["Lg"])
            S.op("act", lambda e: e.activation(out=Lg, in_=Lg, func=AF.Ln, bias=1.0), reads=["Lg"], writes=["Lg"])
            S.op("dve", lambda e: e.tensor_tensor_scan(out=Cg, data0=onesr, data1=Lg, initial=0.0, op0=ALU.mult, op1=ALU.add),
                 reads=["onesr", "Lg"], writes=["Cg"])
            S.op("dve", lambda e: e.tensor_tensor(out=Dg, in0=Cg, in1=Lg, op=ALU.subtract), reads=["Cg", "Lg"], writes=["Dg"])
            for i in range(NT):
                sl = slice(i * 128, (i + 1) * 128)
                S.op("pe", lambda e, i=i, sl=sl: e.matmul(PS[1][:, i * 8:(i + 1) * 8], lhsT=Zg[:, sl], rhs=comb[:, 0, :], start=True, stop=False),
                     reads=zk, writes=[PB(1)])
                S.op("pe", lambda e, i=i, sl=sl: e.matmul(PS[1][:, i * 8:(i + 1) * 8], lhsT=Cg[:, sl], rhs=comb[:, 1, :], start=False, stop=False),
                     reads=["Cg"], writes=[PB(1)])
                S.op("pe", lambda e, i=i, sl=sl: e.matmul(PS[1][:, i * 8:(i + 1) * 8], lhsT=Lg[:, sl], rhs=comb[:, 2, :], start=False, stop=True),
                     reads=["Lg"], writes=[PB(1)])
            S.op("dve", lambda e: e.tensor_copy(out=utok, in_=PS[1][:, 0:128]), reads=[PB(1)], writes=["utok"])
            if b == 0:
                dump("Cg", Cg, "Cg")
                dump("utok", utok, "utok")
            S.barrier()
            A.release(g_mark)

            m_mark = A.mark()
            wxm = A.alloc("wxm", [128, 8, 128], BF16)
            wop = A.alloc("wop", [128, 8, 128], BF16)
            xmp = A.alloc("xmp", [128, S_LEN + 4], F32)
            sigo = A.alloc("sigo", [128, S_LEN], BF16)
            cacc = A.alloc("cacc", [128, S_LEN], F32)
            xc = A.alloc("xc", [128, S_LEN], BF16)
            xmb = A.alloc("xmb", [128, S_LEN], BF16)
            qT = A.alloc("qT", [128, S_LEN], BF16)
            kT = A.alloc("kT", [128, S_LEN], BF16)
            vtok = A.alloc("vtok", [128, NT, 128], BF16)
            bcR = Ring("bc", 4, [128, 512], F32)
            tmR = Ring("tmpm", 3, [128, 512], F32)
            wtR = Ring("Wt", 4, [128, 512], F32)
            swR = Ring("STw", 4, [128, 512], BF16)
            fR = Ring("fin", 8, [128, 512], F32)
            stR = Ring("st", 0, banks=[4, 5, 6, 7])
            S.op("pool", lambda e: e.memset(xmp, 0.0), writes=["xmp"])
            for h in range(4):
                S.dma("pool", lambda e, h=h: e.dma_start(out=wxm, in_=win_v[:, :, h * 128:(h + 1) * 128]), writes=["wxm"])
                S.dma("pool", lambda e, h=h: e.dma_start(out=wop, in_=win_v[:, :, 512 + h * 128:512 + (h + 1) * 128]), writes=["wop"])
                for blk in range(4):
                    bs = slice(blk * 512, (blk + 1) * 512)
                    for k in range(8):
                        S.op("pe", lambda e, k=k, bs=bs: e.matmul(PS[0], lhsT=wxm[:, k, :], rhs=hT[:, k, bs], start=(k == 0), stop=(k == 7)),
                             reads=["wxm"] + hT_keys, writes=[PB(0)])
                    S.op("act", lambda e, blk=blk: e.activation(out=xmp[:, 2 + blk * 512:2 + (blk + 1) * 512], in_=PS[0], func=AF.Copy),
                         reads=[PB(0)], writes=["xmp"])
                    for k in range(8):
                        S.op("pe", lambda e, k=k, bs=bs: e.matmul(PS[1], lhsT=wop[:, k, :], rhs=hT[:, k, bs], start=(k == 0), stop=(k == 7)),
                             reads=["wop"] + hT_keys, writes=[PB(1)])
                    S.op("act", lambda e, bs=bs: e.activation(out=sigo[:, bs], in_=PS[1], func=AF.Sigmoid), reads=[PB(1)], writes=["sigo"])
                S.op("dve", lambda e, h=h: e.tensor_scalar(out=cacc, in0=xmp[:, 0:S_LEN], scalar1=convw[:, h, 0:1], scalar2=None, op0=ALU.mult),
                     reads=["xmp"], writes=["cacc"])
                for j in range(1, 5):
                    S.op("dve", lambda e, h=h, j=j: e.scalar_tensor_tensor(out=cacc, in0=xmp[:, j:j + S_LEN], scalar=convw[:, h, j:j + 1], in1=cacc,
                                                                          op0=ALU.mult, op1=ALU.add),
                         reads=["xmp", "cacc"], writes=["cacc"])
                S.op("act", lambda e, h=h: e.activation(out=xc, in_=cacc, func=AF.Silu, bias=convb[:, h:h + 1]), reads=["cacc"], writes=["xc"])
                S.op("act", lambda e: e.activation(out=xmb, in_=xmp[:, 2:2 + S_LEN], func=AF.Copy), reads=["xmp"], writes=["xmb"])
                for blk in range(4):
                    bs = slice(blk * 512, (blk + 1) * 512)
                    S.op("pe", lambda e, h=h, bs=bs: e.matmul(PS[0], lhsT=wqb[:, h, :], rhs=xc[:, bs], start=True, stop=True), reads=["xc"], writes=[PB(0)])
                    S.op("act", lambda e, bs=bs: e.activation(out=qT[:, bs], in_=PS[0], func=AF.Copy), reads=[PB(0)], writes=["qT"])
                    S.op("pe", lambda e, h=h, bs=bs: e.matmul(PS[1], lhsT=wkb[:, h, :], rhs=xc[:, bs], start=True, stop=True), reads=["xc"], writes=[PB(1)])
                    S.op("dve", lambda e, bs=bs: e.tensor_scalar(out=kT[:, bs], in0=PS[1], scalar1=1.0 / math.sqrt(128.0), scalar2=None, op0=ALU.mult),
                         reads=[PB(1)], writes=["kT"])
                for i4 in range(4):
                    for ii in range(4):
                        i = i4 * 4 + ii
                        S.op("pe", lambda e, h=h, i=i, ii=ii: e.matmul(PS[0][:, ii * 128:(ii + 1) * 128], lhsT=xmb[:, i * 128:(i + 1) * 128],
                                                                        rhs=wvb[:, h, :], start=True, stop=True),
                             reads=["xmb"], writes=[PB(0)])
                    S.op("act", lambda e, i4=i4: e.activation(out=vtok[:, i4 * 4:(i4 + 1) * 4, :], in_=PS[0].rearrange("p (a b) -> p a b", a=4), func=AF.Copy),
                         reads=[PB(0)], writes=["vtok"])
                for T in range(4):
                    bs = slice(T * 512, (T + 1) * 512)
                    pb1, pb1k = stR.next()
                    S.op("pe", lambda e, h=h, bs=bs, pb1=pb1: e.matmul(pb1, lhsT=selfb[:, h, :], rhs=Cg[:, bs], start=True, stop=True), reads=["Cg"], writes=[pb1k])
                    bcf, bcfk = bcR.next()
                    S.op("act", lambda e, bcf=bcf, pb1=pb1: e.activation(out=bcf, in_=pb1, func=AF.Copy), reads=[pb1k], writes=[bcfk])
                    pb2, pb2k = stR.next()
                    S.op("pe", lambda e, h=h, bs=bs, pb2=pb2: e.matmul(pb2, lhsT=selfb[:, 4 + h, :], rhs=Dg[:, bs], start=True, stop=True), reads=["Dg"], writes=[pb2k])
                    bcb, bcbk = bcR.next()
                    S.op("act", lambda e, bcb=bcb, pb2=pb2: e.activation(out=bcb, in_=pb2, func=AF.Copy), reads=[pb2k], writes=[bcbk])
                    nf = 4 * T + 4
                    nb = 16 - 4 * T
                    cf = 0
                    cb = 0
                    sts = {}

                    def emit_st(j, bs=bs, sts=sts):
                        st, stk = stR.next()
                        S.op("pe", lambda e, j=j, bs=bs, st=st: e.matmul(st, lhsT=kT[:, j * 128:(j + 1) * 128], rhs=qT[:, bs], start=True, stop=True),
                             reads=["kT", "qT"], writes=[stk])
                        sts[j] = (st, stk)
                    LOOK = 2
                    for j in range(LOOK):
                        emit_st(j)
                    for j in range(NT):
                        fwd = j <= 4 * T + 3
                        bwd = j >= 4 * T
                        if j + LOOK < NT:
                            emit_st(j + LOOK)
                        st, stk = sts[j]
                        for dirn in (0, 1):
                            if (dirn == 0 and not fwd) or (dirn == 1 and not bwd):
                                continue
                            bc, bck = (bcf, bcfk) if dirn == 0 else (bcb, bcbk)
                            src, srck = bc, bck
                            if 4 * T <= j <= 4 * T + 3:
                                o = (j - 4 * T) * 128
                                mk = maskF if dirn == 0 else maskB
                                tmpm, tmpk = tmR.next()
                                S.op("pool", lambda e, bc=bc, mk=mk, o=o, tmpm=tmpm: e.tensor_tensor(out=tmpm, in0=bc, in1=mk[:, 384 - o:384 - o + 512], op=ALU.add),
                                     reads=[bck], writes=[tmpk])
                                src, srck = tmpm, tmpk
                            ucol = j * 8 + dirn * 4 + h
                            Wt, Wtk = wtR.next()
                            S.op("act", lambda e, src=src, ucol=ucol, Wt=Wt: e.activation(out=Wt, in_=src, func=AF.Exp, bias=utok[:, ucol:ucol + 1]),
                                 reads=[srck, "utok"], writes=[Wtk])
                            STw, STwk = swR.next()
                            S.op("dve", lambda e, st=st, Wt=Wt, STw=STw: e.tensor_tensor(out=STw, in0=st, in1=Wt, op=ALU.mult), reads=[stk, Wtk], writes=[STwk])
                            if dirn == 0:
                                first, last = (cf == 0), (cf == nf - 1)
                                cf += 1
                                nb_, db_ = 0, 1
                            else:
                                first, last = (cb == 0), (cb == nb - 1)
                                cb += 1
                                nb_, db_ = 2, 3
                            S.op("pe", lambda e, j=j, nb_=nb_, first=first, last=last, STw=STw: e.matmul(PS[nb_], lhsT=vtok[:, j, :], rhs=STw, start=first, stop=last),
                                 reads=["vtok", STwk], writes=[PB(nb_)])
                            S.op("pe", lambda e, db_=db_, first=first, last=last, STw=STw: e.matmul(PS[db_], lhsT=onesb, rhs=STw, start=first, stop=last),
                                 reads=[STwk], writes=[PB(db_)])
                    (fa, fak), (fb_, fbk), (fc_, fck), (fd_, fdk) = fR.next(), fR.next(), fR.next(), fR.next()
                    for (nb_, db_, dst, dk) in ((0, 1, fa, fak), (2, 3, fb_, fbk)):
                        S.op("act", lambda e, db_=db_, fd_=fd_: e.activation(out=fd_, in_=PS[db_], func=AF.Copy), reads=[PB(db_)], writes=[fdk])
                        S.op("dve", lambda e, fc_=fc_, fd_=fd_: e.scalar_tensor_tensor(out=fc_, in0=fd_, scalar=-1.0, in1=fd_, op0=ALU.mult, op1=ALU.max),
                             reads=[fdk], writes=[fck])
                        S.op("dve", lambda e, fc_=fc_: e.tensor_scalar(out=fc_, in0=fc_, scalar1=1.0, scalar2=None, op0=ALU.max), reads=[fck], writes=[fck])
                        S.op("dve", lambda e, fc_=fc_: e.reciprocal(out=fc_, in_=fc_), reads=[fck], writes=[fck])
                        S.op("dve", lambda e, nb_=nb_, dst=dst, fc_=fc_: e.tensor_tensor(out=dst, in0=PS[nb_], in1=fc_, op=ALU.mult), reads=[PB(nb_), fck], writes=[dk])
                    S.op("pool", lambda e, fa=fa, fb_=fb_: e.tensor_tensor(out=fa, in0=fa, in1=fb_, op=ALU.add), reads=[fak, fbk], writes=[fak])
                    S.op("act", lambda e, fa=fa, fd_=fd_: e.activation(out=fd_, in_=fa, func=AF.Square), reads=[fak], writes=[fdk])
                    pb3, pb3k = stR.next()
                    S.op("pe", lambda e, fd_=fd_, pb3=pb3: e.matmul(pb3, lhsT=ones128, rhs=fd_, start=True, stop=True), reads=[fdk], writes=[pb3k])
                    S.op("act", lambda e, fd_=fd_, pb3=pb3: e.activation(out=fd_, in_=pb3, func=AF.Ln, bias=EPS), reads=[pb3k], writes=[fdk])
                    S.op("act", lambda e, fd_=fd_: e.activation(out=fd_, in_=fd_, func=AF.Exp, scale=-0.5), reads=[fdk], writes=[fdk])
                    S.op("dve", lambda e, h=h, fa=fa, fd_=fd_: e.scalar_tensor_tensor(out=fa, in0=fa, scalar=mng[:, h:h + 1], in1=fd_, op0=ALU.mult, op1=ALU.mult),
                         reads=[fak, fdk], writes=[fak])
                    S.op("dve", lambda e, h=h, bs=bs, fa=fa: e.scalar_tensor_tensor(out=fa, in0=xc[:, bs], scalar=msk[:, h:h + 1], in1=fa, op0=ALU.mult, op1=ALU.add),
                         reads=[fak, "xc"], writes=[fak])
                    S.op("dve", lambda e, h=h, bs=bs, fa=fa: e.tensor_tensor(out=catT[:, h, bs], in0=fa, in1=sigo[:, bs], op=ALU.mult),
                         reads=[fak, "sigo"], writes=[("catT", h)])
            S.barrier()
            A.release(m_mark)

            a_mark = A.mark()
            wq3 = A.alloc("wq3", [128, 8, 128], BF16)
            wk3 = A.alloc("wk3", [128, 8, 128], BF16)
            wv3 = A.alloc("wv3", [128, 8, 128], BF16)
            a32 = A.alloc("a32", [128, S_LEN], F32)
            tq = A.alloc("tq", [128, 512], F32)
            qn = A.alloc("qn", [128, S_LEN], BF16)
            kn = A.alloc("kn", [128, S_LEN], BF16)
            av = A.alloc("av", [128, S_LEN], BF16)
            qd = A.alloc("qd", [128, S_LEN], BF16)
            kd = A.alloc("kd", [128, S_LEN], BF16)
            avd = A.alloc("avd", [128, S_LEN], BF16)
            VpA = A.alloc("VpA", [128, NT, 128], BF16)
            VpB = A.alloc("VpB", [128, NT, 128], BF16)
            tabs = A.alloc("tabs", [128, 3, 2, 256], BF16)
            recb = A.alloc("recb", [128, 512], F32)
            S.op("pool", lambda e: e.memset(VpA, 1.0), writes=["VpA"])
            S.op("pool", lambda e: e.memset(VpB, 1.0), writes=["VpB"])
            tabsR = A.alloc("tabsR", [128, 3, 2, 256], F32)
            accN = A.alloc("accN", [128, S_LEN], F32)
            accD = A.alloc("accD", [128, S_LEN], F32)
            etR = Ring("Et", 4, [128, 2, 256], BF16)
            ptR = Ring("Pt", 4, [128, 2, 256], BF16)
            stA_items = [(PSD[2], 4, 5), (PSD[3], 6, 7)]
            stA_i = [0]
            for c in range(4):
                for (wt, col0, wkey) in ((wq3, 1040, "wq3"), (wk3, 1552, "wk3"), (wv3, 2064, "wv3")):
                    S.dma("pool", lambda e, wt=wt, col0=col0, c=c: e.dma_start(out=wt, in_=win_v[:, :, col0 + c * 128:col0 + (c + 1) * 128]), writes=[wkey])
                for p in range(3):
                    for hh in range(2):
                        hd = 2 * c + hh
                        src = bass.AP(tensor=ftab_d.tensor, offset=(hd * 3 + p) * 384, ap=[[1, 128], [1, 256]])
                        S.dma("sp", lambda e, p=p, hh=hh, src=src: e.dma_start(out=tabsR[:, p, hh, :], in_=src), reads=["ftab_d"], writes=[("tabsR", p, hh)])
                    for hh in range(2):
                        S.op("pe", lambda e, p=p, hh=hh: e.matmul(PS[0][:, hh * 256:(hh + 1) * 256], lhsT=jrev, rhs=tabsR[:, p, hh, :], start=True, stop=True),
                             reads=[("tabsR", p, hh)], writes=[PB(0)])
                    S.op("dve", lambda e, p=p: e.tensor_copy(out=tabs[:, p, :, :], in_=PS[0].rearrange("p (a b) -> p a b", a=2)), reads=[PB(0)], writes=["tabs"])
                for (wt, wkey, dst, dkey, gvec) in ((wq3, "wq3", qn, "qn", gq), (wk3, "wk3", kn, "kn", gk)):
                    for blk in range(4):
                        bs = slice(blk * 512, (blk + 1) * 512)
                        for k in range(8):
                            S.op("pe", lambda e, k=k, bs=bs, wt=wt: e.matmul(PS[0], lhsT=wt[:, k, :], rhs=hT[:, k, bs], start=(k == 0), stop=(k == 7)),
                                 reads=[wkey] + hT_keys, writes=[PB(0)])
                        S.op("act", lambda e, bs=bs: e.activation(out=a32[:, bs], in_=PS[0], func=AF.Copy), reads=[PB(0)], writes=["a32"])
                        S.op("act", lambda e, bs=bs: e.activation(out=tq, in_=PS[0], func=AF.Square), reads=[PB(0)], writes=["tq"])
                        S.op("pe", lambda e: e.matmul(PS[1], lhsT=blk64, rhs=tq, start=True, stop=True), reads=["tq"], writes=[PB(1)])
                        S.op("act", lambda e: e.activation(out=tq, in_=PS[1], func=AF.Ln, bias=EPS), reads=[PB(1)], writes=["tq"])
                        S.op("act", lambda e: e.activation(out=tq, in_=tq, func=AF.Exp, scale=-0.5), reads=["tq"], writes=["tq"])
                        S.op("dve", lambda e, bs=bs, dst=dst, gvec=gvec: e.scalar_tensor_tensor(out=dst[:, bs], in0=a32[:, bs], scalar=gvec[:, 0:1], in1=tq,
                                                                                               op0=ALU.mult, op1=ALU.mult),
                             reads=["a32", "tq"], writes=[dkey])
                for blk in range(4):
                    bs = slice(blk * 512, (blk + 1) * 512)
                    for k in range(8):
                        S.op("pe", lambda e, k=k, bs=bs: e.matmul(PS[0], lhsT=wv3[:, k, :], rhs=hT[:, k, bs], start=(k == 0), stop=(k == 7)),
                             reads=["wv3"] + hT_keys, writes=[PB(0)])
                    S.op("act", lambda e, bs=bs: e.activation(out=av[:, bs], in_=PS[0], func=AF.Copy), reads=[PB(0)], writes=["av"])
                for p, d in enumerate((1, 4, 16)):
                    L = S_LEN // d
                    if d == 1:
                        qv, kv, vv = qn, kn, av
                        qk_, kk_, vk_ = "qn", "kn", "av"
                    else:
                        S.op("pool", lambda e, d=d: e.tensor_copy(out=qd.rearrange("p (d l) -> p d l", d=d), in_=qn.rearrange("p (l d) -> p d l", d=d)),
                             reads=["qn"], writes=["qd"])
                        S.op("pool", lambda e, d=d: e.tensor_copy(out=kd.rearrange("p (d l) -> p d l", d=d), in_=kn.rearrange("p (l d) -> p d l", d=d)),
                             reads=["kn"], writes=["kd"])
                        S.op("pool", lambda e, d=d: e.tensor_copy(out=avd.rearrange("p (d l) -> p d l", d=d), in_=av.rearrange("p (l d) -> p d l", d=d)),
                             reads=["av"], writes=["avd"])
                        qv, kv, vv = qd, kd, avd
                        qk_, kk_, vk_ = "qd", "kd", "avd"
                    for i4 in range(4):
                        for ii in range(4):
                            i = i4 * 4 + ii
                            S.op("pe", lambda e, i=i, ii=ii, vv=vv: e.matmul(PS[0][:, ii * 128:(ii + 1) * 128], lhsT=vv[:, i * 128:(i + 1) * 128], rhs=identb,
                                                                               start=True, stop=True),
                                 reads=[vk_], writes=[PB(0)])
                        S.op("act", lambda e, i4=i4: e.activation(out=VpA[:, i4 * 4:(i4 + 1) * 4, 0:64], in_=PS[0].rearrange("p (a b) -> p a b", a=4)[:, :, 0:64], func=AF.Copy),
                             reads=[PB(0)], writes=["VpA"])
                        S.op("dve", lambda e, i4=i4: e.tensor_copy(out=VpB[:, i4 * 4:(i4 + 1) * 4, 64:128], in_=PS[0].rearrange("p (a b) -> p a b", a=4)[:, :, 64:128]),
                             reads=[PB(0), "VpA"], writes=["VpB"])
                    nkt = L // 128
                    for qb in range(4):
                        S.op("dve", lambda e: e.memset(PS[2], 0.0), writes=[PB(2)])
                        S.op("dve", lambda e: e.memset(PS[3], 0.0), writes=[PB(3)])
                        if L >= 512:
                            phases = [(qb * 512) // L]
                        else:
                            phases = list(range((qb * 512) // L, (qb * 512 + 512) // L))
                        tiles = []
                        for r in phases:
                            base = r * L
                            blo = max(qb * 512, base) - base
                            bhi = min(qb * 512 + 512, base + L) - base
                            for n in range(nkt):
                                qlo = max(blo, 128 * n - 64)
                                qhi = min(bhi, 128 * n + 192)
                                if qhi <= qlo:
                                    continue
                                nq = qhi - qlo
                                toff = qlo - (128 * n - 64)
                                gk0 = base + 128 * n
                                gq0 = base + qlo
                                col0 = gq0 - qb * 512
                                tiles.append((nq, toff, gk0, gq0, col0))
                        stt = {}

                        def emit_qk(t, tiles=tiles, stt=stt, kv=kv, qv=qv, kk_=kk_, qk_=qk_):
                            nq, toff, gk0, gq0, col0 = tiles[t]
                            std, bka, bkb = stA_items[stA_i[0] % 2]
                            stA_i[0] += 1
                            st3 = std.rearrange("p (a b) -> p a b", a=2)
                            for hh in range(2):
                                ps_ = slice(64 * hh, 64 * hh + 64)
                                S.op("pe", lambda e, ps_=ps_, hh=hh, gk0=gk0, gq0=gq0, nq=nq, st3=st3: e.matmul(
                                    st3[:, hh, 0:nq], lhsT=kv[ps_, gk0:gk0 + 128], rhs=qv[ps_, gq0:gq0 + nq], start=True, stop=True),
                                    reads=[kk_, qk_], writes=[PB(bka if hh == 0 else bkb)])
                            stt[t] = (st3, bka, bkb)
                        if tiles:
                            emit_qk(0)
                        for t in range(len(tiles)):
                            if t + 1 < len(tiles):
                                emit_qk(t + 1)
                            nq, toff, gk0, gq0, col0 = tiles[t]
                            st3, bka, bkb = stt[t]
                            Et, Etk = etR.next()
                            Pt, Ptk = ptR.next()
                            S.op("act", lambda e, st3=st3, nq=nq, Et=Et: e.activation(out=Et[:, :, 0:nq], in_=st3[:, :, 0:nq], func=AF.Exp, scale=0.125),
                                 reads=[PB(bka), PB(bkb)], writes=[Etk])
                            S.op("dve", lambda e, nq=nq, p=p, toff=toff, Et=Et, Pt=Pt: e.tensor_tensor(out=Pt[:, :, 0:nq], in0=Et[:, :, 0:nq],
                                                                                                       in1=tabs[:, p, :, toff:toff + nq], op=ALU.mult),
                                 reads=[Etk, "tabs"], writes=[Ptk])
                            ti = gk0 // 128
                            S.op("pe", lambda e, ti=ti, col0=col0, nq=nq, Pt=Pt: e.matmul(PS[2][:, col0:col0 + nq], lhsT=VpA[:, ti, :], rhs=Pt[:, 0, 0:nq],
                                                                                         start=False, stop=False, skip_group_check=True),
                                 reads=["VpA", Ptk], writes=[PB(2)])
                            S.op("pe", lambda e, ti=ti, col0=col0, nq=nq, Pt=Pt: e.matmul(PS[3][:, col0:col0 + nq], lhsT=VpB[:, ti, :], rhs=Pt[:, 1, 0:nq],
                                                                                         start=False, stop=False, skip_group_check=True),
                                 reads=["VpB", Ptk], writes=[PB(3)])
                        if d == 1:
                            S.op("act", lambda e, qb=qb: e.activation(out=accN[:, qb * 512:(qb + 1) * 512], in_=PS[2], func=AF.Copy), reads=[PB(2)], writes=["accN"])
                            S.op("dve", lambda e, qb=qb: e.tensor_copy(out=accD[:, qb * 512:(qb + 1) * 512], in_=PS[3]), reads=[PB(3)], writes=["accD"])
                        else:
                            npb = 512 // L if L < 512 else 1
                            r0 = (qb * 512) // L
                            for (acc, ak, bank) in ((accN, "accN", 2), (accD, "accD", 3)):
                                if npb == 1:
                                    view = acc.rearrange("p (l d) -> p d l", d=d)[:, r0, :]
                                    pin = PS[bank]
                                else:
                                    view = acc.rearrange("p (l d) -> p d l", d=d)[:, r0:r0 + npb, :]
                                    pin = PS[bank].rearrange("p (a b) -> p a b", a=npb)
                                S.op("dve", lambda e, view=view, pin=pin: e.tensor_tensor(out=view, in0=pin, in1=view, op=ALU.add), reads=[PB(bank), ak], writes=[ak])
                if b == 0 and c == 0:
                    dump("tabs", tabs, "tabs")
                    dump("accN", accN, "accN")
                    dump("accD", accD, "accD")
                    if "qn" in dbg:
                        S.dma("pool", lambda e: e.dma_start(out=dbg["qn"], in_=qn), reads=["qn"], writes=["dbg_qn"])
                        S.dma("pool", lambda e: e.dma_start(out=dbg["kn"], in_=kn), reads=["kn"], writes=["dbg_kn"])
                        S.dma("pool", lambda e: e.dma_start(out=dbg["av"], in_=av), reads=["av"], writes=["dbg_av"])
                for blk in range(4):
                    bs = slice(blk * 512, (blk + 1) * 512)
                    S.op("pe", lambda e, bs=bs: e.matmul(PS[0], lhsT=selab[:, 0, :], rhs=accN[:, bs], start=True, stop=False), reads=["accN"], writes=[PB(0)])
                    S.op("pe", lambda e, bs=bs: e.matmul(PS[0], lhsT=selab[:, 1, :], rhs=accD[:, bs], start=False, stop=True), reads=["accD"], writes=[PB(0)])
                    S.op("dve", lambda e: e.reciprocal(out=recb, in_=PS[0]), reads=[PB(0)], writes=["recb"])
                    S.op("dve", lambda e, c=c, bs=bs: e.tensor_tensor(out=catT[0:64, 4 + c, bs], in0=accN[0:64, bs], in1=recb[0:64, :], op=ALU.mult),
                         reads=["accN", "recb"], writes=[("catT", 4 + c)])
                    S.op("dve", lambda e, c=c, bs=bs: e.tensor_tensor(out=catT[64:128, 4 + c, bs], in0=accD[64:128, bs], in1=recb[64:128, :], op=ALU.mult),
                         reads=["accD", "recb"], writes=[("catT", 4 + c)])
            if b == 0 and "catT" in dbg:
                S.dma("pool", lambda e: e.dma_start(out=dbg["catT"], in_=catT), reads=[("catT", k) for k in range(8)], writes=["dbg_catT"])
            if max_phase < 5:
                S.barrier()
                continue
            S.barrier()
            A.release(a_mark)
            A.release(pers_mark)

            h2tok = A.alloc("h2tok", [128, NT, D], BF16)
            afft = A.alloc("afft", [128, NT, 16], F32)
            slott = A.alloc("slott", [128, NT, 16], F32)
            slotv = A.alloc("slotv", [16, S_LEN], F32)
            f_mark = A.mark()
            wo = A.alloc("wo", [128, 8, D], BF16)
            h2 = A.alloc("h2", [128, D], F32)
            h2T = A.alloc("h2T", [128, 8, 128], F32)
            g1bc = A.alloc("g1bc", [128, D], F32)
            a2bc = A.alloc("a2bc", [128, D], F32)
            b2bc = A.alloc("b2bc", [128, D], F32)
            affT = A.alloc("affT", [16, S_LEN], F32)
            work = A.alloc("work", [16, S_LEN], F32)
            mx8 = A.alloc("mx8", [16, 8], F32)
            onesr = A.alloc("onesr2", [16, S_LEN], F32)
            for k in range(8):
                S.dma("pool", lambda e, k=k: e.dma_start(out=wo[:, k, :], in_=wout_v[:, k, :]), writes=[("wo", k)])
            wo_keys = [("wo", k) for k in range(8)]
            S.dma("sp", lambda e: e.dma_start(out=g1bc, in_=modrow_d[b:b + 1, 0:1024].partition_broadcast(128)), reads=["modrow_d"], writes=["g1bc"])
            S.dma("sp", lambda e: e.dma_start(out=b2bc, in_=modrow_d[b:b + 1, 1024:2048].partition_broadcast(128)), reads=["modrow_d"], writes=["b2bc"])
            S.dma("sp", lambda e: e.dma_start(out=a2bc, in_=modrow_d[b:b + 1, 2048:3072].partition_broadcast(128)), reads=["modrow_d"], writes=["a2bc"])
            S.dma("sp", lambda e: e.dma_start(out=h2, in_=g2_d.partition_broadcast(128)), writes=["h2"])
            S.op("dve", lambda e: e.scalar_tensor_tensor(out=a2bc, in0=a2bc, scalar=1.0, in1=h2, op0=ALU.add, op1=ALU.mult), reads=["a2bc", "h2"], writes=["a2bc"])
            S.op("pool", lambda e: e.memset(onesr, 1.0), writes=["onesr2"])
            cat_keys = [("catT", k) for k in range(8)]
            xtR = Ring("xtr", 2, [128, D], F32)
            x1R = Ring("x1r", 2, [128, D], F32)
            h2R = Ring("h2r", 3, [128, D], F32)
            stR5 = Ring("st2r", 3, [128, 8], F32)
            lgR = Ring("lgr", 2, [128, 16], F32)
            opb = [(0, 1), (6, 7)]
            stA5 = {}

            def emit_A(i):
                ts_ = slice(i * 128, (i + 1) * 128)
                xt_, xtk = xtR.next()
                x1_, x1k = x1R.next()
                h2_, h2k_ = h2R.next()
                st_, stk_ = stR5.next()
                load("sp", xt_, x_d[b, ts_, :], xtk)
                for half in range(2):
                    hs = slice(half * 512, (half + 1) * 512)
                    bank = opb[i % 2][half]
                    for k in range(8):
                        S.op("pe", lambda e, k=k, ts_=ts_, hs=hs, bank=bank: e.matmul(PS[bank], lhsT=catT[:, k, ts_], rhs=wo[:, k, hs], start=(k == 0), stop=(k == 7)),
                             reads=cat_keys + wo_keys, writes=[PB(bank)])
                    S.op("dve", lambda e, hs=hs, bank=bank, x1_=x1_: e.tensor_tensor(out=x1_[:, hs], in0=PS[bank], in1=g1bc[:, hs], op=ALU.mult), reads=[PB(bank), "g1bc"], writes=[x1k])
                S.op("pool", lambda e, x1_=x1_, xt_=xt_: e.tensor_tensor(out=x1_, in0=x1_, in1=xt_, op=ALU.add), reads=[x1k, xtk], writes=[x1k])
                S.dma("sp", lambda e, ts_=ts_, x1_=x1_: e.dma_start(out=out_d[b, ts_, :], in_=x1_), reads=[x1k], writes=[("outd", b, i)])
                S.op("act", lambda e, x1_=x1_, h2_=h2_, st_=st_: e.activation(out=h2_, in_=x1_, func=AF.Square, accum_out=st_[:, 0:1]), reads=[x1k], writes=[h2k_, stk_])
                S.op("act", lambda e, st_=st_: e.activation(out=st_[:, 1:2], in_=st_[:, 0:1], func=AF.Ln, scale=1.0 / D, bias=EPS), reads=[stk_], writes=[stk_])
                S.op("act", lambda e, st_=st_: e.activation(out=st_[:, 2:3], in_=st_[:, 1:2], func=AF.Exp, scale=-0.5), reads=[stk_], writes=[stk_])
                S.op("dve", lambda e, x1_=x1_, h2_=h2_, st_=st_: e.scalar_tensor_tensor(out=h2_, in0=x1_, scalar=st_[:, 2:3], in1=a2bc, op0=ALU.mult, op1=ALU.mult),
                     reads=[x1k, stk_, "a2bc"], writes=[h2k_])
                S.op("pool", lambda e, h2_=h2_: e.tensor_tensor(out=h2_, in0=h2_, in1=b2bc, op=ALU.add), reads=[h2k_, "b2bc"], writes=[h2k_])
                S.op("act", lambda e, i=i, h2_=h2_: e.activation(out=h2tok[:, i, :], in_=h2_, func=AF.Copy), reads=[h2k_], writes=[("h2tok", i)])
                stA5[i] = (h2_, h2k_, st_, stk_)

            def emit_B(i):
                ts_ = slice(i * 128, (i + 1) * 128)
                h2_, h2k_, st_, stk_ = stA5[i]
                lg_, lgk = lgR.next()
                for k in range(8):
                    bank = 2 + k // 4
                    S.op("pe", lambda e, k=k, bank=bank, h2_=h2_: e.matmul(PS[bank][:, (k % 4) * 128:(k % 4 + 1) * 128], lhsT=h2_[:, k * 128:(k + 1) * 128], rhs=ident,
                                                                            start=True, stop=True), reads=[h2k_], writes=[PB(bank)])
                S.op("dve", lambda e: e.tensor_copy(out=h2T[:, 0:4, :], in_=PS[2].rearrange("p (a b) -> p a b", a=4)), reads=[PB(2)], writes=["h2T"])
                S.op("act", lambda e: e.activation(out=h2T[:, 4:8, :], in_=PS[3].rearrange("p (a b) -> p a b", a=4), func=AF.Copy), reads=[PB(3)], writes=["h2T"])
                for k in range(8):
                    S.op("pe", lambda e, k=k: e.matmul(PS[4][:, 0:16], lhsT=h2T[:, k, :], rhs=wr[:, k, :], start=(k == 0), stop=(k == 7)), reads=["h2T"], writes=[PB(4)])
                S.op("dve", lambda e, lg_=lg_: e.tensor_tensor(out=lg_, in0=PS[4][:, 0:16], in1=brbc, op=ALU.add), reads=[PB(4)], writes=[lgk])
                S.op("dve", lambda e, lg_=lg_, st_=st_: e.tensor_reduce(out=st_[:, 3:4], in_=lg_, axis=AX.X, op=ALU.max), reads=[lgk], writes=[stk_])
                S.op("dve", lambda e, st_=st_: e.tensor_scalar(out=st_[:, 4:5], in0=st_[:, 3:4], scalar1=-1.0, scalar2=None, op0=ALU.mult), reads=[stk_], writes=[stk_])
                S.op("act", lambda e, lg_=lg_, st_=st_: e.activation(out=lg_, in_=lg_, func=AF.Exp, bias=st_[:, 4:5], accum_out=st_[:, 5:6]), reads=[lgk, stk_], writes=[lgk, stk_])
                S.op("dve", lambda e, st_=st_: e.reciprocal(out=st_[:, 6:7], in_=st_[:, 5:6]), reads=[stk_], writes=[stk_])
                S.op("dve", lambda e, i=i, lg_=lg_, st_=st_: e.tensor_scalar(out=afft[:, i, :], in0=lg_, scalar1=st_[:, 6:7], scalar2=None, op0=ALU.mult), reads=[lgk, stk_], writes=[("afft", i)])
                S.op("pe", lambda e, i=i: e.matmul(PS[5][0:16, 0:128], lhsT=afft[:, i, :], rhs=ident, start=True, stop=True), reads=[("afft", i)], writes=[PB(5)])
                S.op("act", lambda e, ts_=ts_: e.activation(out=affT[:, ts_], in_=PS[5][0:16, 0:128], func=AF.Copy), reads=[PB(5)], writes=["affT"])

            emit_A(0)
            for i in range(NT):
                if i + 1 < NT:
                    emit_A(i + 1)
                emit_B(i)
            S.op("dve", lambda e: e.tensor_copy(out=work, in_=affT), reads=["affT"], writes=["work"])
            for rnd in range(CAP // 8):
                S.op("dve", lambda e: e.max(out=mx8, in_=work), reads=["work"], writes=["mx8"])
                S.op("dve", lambda e: e.match_replace(out=work, in_to_replace=mx8, in_values=work, imm_value=0.0), reads=["work", "mx8"], writes=["work"])
            S.op("dve", lambda e: e.tensor_tensor(out=work, in0=affT, in1=work, op=ALU.subtract), reads=["affT", "work"], writes=["work"])
            S.op("dve", lambda e: e.tensor_scalar(out=work, in0=work, scalar1=0.0, scalar2=None, op0=ALU.is_gt), reads=["work"], writes=["work"])
            S.op("dve", lambda e: e.tensor_tensor_scan(out=slotv, data0=onesr, data1=work, initial=0.0, op0=ALU.mult, op1=ALU.add), reads=["onesr2", "work"], writes=["slotv"])
            S.op("dve", lambda e: e.tensor_tensor(out=slotv, in0=slotv, in1=work, op=ALU.mult), reads=["slotv", "work"], writes=["slotv"])
            S.op("dve", lambda e: e.tensor_scalar(out=slotv, in0=slotv, scalar1=-1.0, scalar2=None, op0=ALU.add), reads=["slotv"], writes=["slotv"])
            for i in range(NT):
                S.op("pe", lambda e, i=i: e.matmul(PS[6][:, i * 16:(i + 1) * 16], lhsT=slotv[:, i * 128:(i + 1) * 128], rhs=ident[0:16, 0:16], start=True, stop=True),
                     reads=["slotv"], writes=[PB(6)])
            S.op("dve", lambda e: e.tensor_copy(out=slott, in_=PS[6][:, 0:256].rearrange("p (a b) -> p a b", a=NT)), reads=[PB(6)], writes=["slott"])
            if b == 0:
                dump("slotv", slotv, "slotv")
                dump("affT", affT, "affT")
                if "h2tok" in dbg:
                    S.dma("pool", lambda e: e.dma_start(out=dbg["h2tok"], in_=h2tok), reads=[("h2tok", i) for i in range(NT)], writes=["dbg_h2tok"])
            if max_phase < 6:
                S.barrier()
                continue
            S.barrier()
            A.release(f_mark)

            A.limit = ARENA_BYTES
            yacc = A.alloc("yacc", [128, NT, D], F32)
            y_mark = A.mark()
            Pm = A.alloc("Pm", [128, NT, CAP], BF16)
            PTm = A.alloc("PTm", [128, 2, S_LEN], BF16)
            xin = A.alloc("xin", [128, 8, CAP], BF16)
            wR = Ring("wbuf", 4, [128, 4096], BF16)
            hid = A.alloc("hid", [128, 16, CAP], BF16)
            yex = A.alloc("yex", [128, 2, D], BF16)
            sgR = Ring("sg", 2, [128, CAP], F32)
            fR2 = Ring("ffps", 0, banks=[0, 1, 2, 3])
            h2k = [("h2tok", i) for i in range(NT)]
            for ex in range(NEXP):
                for i in range(NT):
                    S.op("dve", lambda e, i=i, ex=ex: e.tensor_scalar(out=Pm[:, i, :], in0=iotaf, scalar1=slott[:, i, ex:ex + 1], scalar2=None, op0=ALU.is_equal),
                         reads=["slott"], writes=["Pm"])
                for blk in range(4):
                    bs = slice(blk * 512, (blk + 1) * 512)
                    pb, pbk = fR2.next()
                    S.op("pe", lambda e, ex=ex, bs=bs, pb=pb: e.matmul(pb, lhsT=sele[:, ex, :], rhs=slotv[:, bs], start=True, stop=True), reads=["slotv"], writes=[pbk])
                    for ch in range(2):
                        S.op("dve", lambda e, ch=ch, bs=bs, pb=pb: e.tensor_scalar(out=PTm[:, ch, bs], in0=pb, scalar1=iotap[:, ch:ch + 1], scalar2=None, op0=ALU.is_equal),
                             reads=[pbk], writes=["PTm"])
                for k2 in range(4):
                    pb, pbk = fR2.next()
                    for kk in range(2):
                        k = k2 * 2 + kk
                        for i in range(NT):
                            S.op("pe", lambda e, k=k, kk=kk, i=i, pb=pb: e.matmul(pb[:, kk * 256:(kk + 1) * 256], lhsT=h2tok[:, i, k * 128:(k + 1) * 128], rhs=Pm[:, i, :],
                                                                                  start=(i == 0), stop=(i == NT - 1)),
                                 reads=h2k + ["Pm"], writes=[pbk])
                    S.op("act", lambda e, k2=k2, pb=pb: e.activation(out=xin[:, 2 * k2:2 * k2 + 2, :], in_=pb.rearrange("p (a b) -> p a b", a=2), func=AF.Copy),
                         reads=[pbk], writes=["xin"])
                for fb in range(4):
                    wg, wgk = wR.next()
                    wg = wg.rearrange("p (k f) -> p k f", k=8)
                    S.dma("pool", lambda e, ex=ex, fb=fb, wg=wg: e.dma_start(out=wg, in_=wg_d[ex].rearrange("(k p) f -> p k f", p=128)[:, :, fb * 512:(fb + 1) * 512]), writes=[wgk])
                    wu, wuk = wR.next()
                    wu = wu.rearrange("p (k f) -> p k f", k=8)
                    S.dma("pool", lambda e, ex=ex, fb=fb, wu=wu: e.dma_start(out=wu, in_=wu_d[ex].rearrange("(k p) f -> p k f", p=128)[:, :, fb * 512:(fb + 1) * 512]), writes=[wuk])
                    for fc in range(4):
                        f = fb * 4 + fc
                        pb, pbk = fR2.next()
                        for k in range(8):
                            S.op("pe", lambda e, k=k, fc=fc, pb=pb, wg=wg: e.matmul(pb[:, 0:CAP], lhsT=wg[:, k, fc * 128:(fc + 1) * 128], rhs=xin[:, k, :], start=(k == 0), stop=(k == 7)),
                                 reads=[wgk, "xin"], writes=[pbk])
                        for k in range(8):
                            S.op("pe", lambda e, k=k, fc=fc, pb=pb, wu=wu: e.matmul(pb[:, CAP:2 * CAP], lhsT=wu[:, k, fc * 128:(fc + 1) * 128], rhs=xin[:, k, :], start=(k == 0), stop=(k == 7)),
                                 reads=[wuk, "xin"], writes=[pbk])
                        sg, sgk = sgR.next()
                        S.op("act", lambda e, pb=pb, sg=sg: e.activation(out=sg, in_=pb[:, 0:CAP], func=AF.Silu), reads=[pbk], writes=[sgk])
                        S.op("dve", lambda e, f=f, pb=pb, sg=sg: e.tensor_tensor(out=hid[:, f, :], in0=pb[:, CAP:2 * CAP], in1=sg, op=ALU.mult), reads=[pbk, sgk], writes=["hid"])
                for fb in range(4):
                    wd, wdk = wR.next()
                    wd = wd.rearrange("p (k n) -> p k n", k=4)
                    S.dma("pool", lambda e, ex=ex, fb=fb, wd=wd: e.dma_start(out=wd, in_=wd_d[ex].rearrange("(k p) n -> p k n", p=128)[:, fb * 4:(fb + 1) * 4, :]), writes=[wdk])
                    for fc in range(4):
                        f = fb * 4 + fc
                        for ct in range(2):
                            for dh in range(2):
                                bank = 4 + ct * 2 + dh
                                S.op("pe", lambda e, f=f, fc=fc, ct=ct, dh=dh, bank=bank, wd=wd: e.matmul(PS[bank], lhsT=hid[:, f, ct * 128:(ct + 1) * 128],
                                                                                                          rhs=wd[:, fc, dh * 512:(dh + 1) * 512], start=(f == 0), stop=(f == 15)),
                                     reads=["hid", wdk], writes=[PB(bank)])
                for ct in range(2):
                    for dh in range(2):
                        bank = 4 + ct * 2 + dh
                        if dh == 0:
                            S.op("act", lambda e, ct=ct, dh=dh, bank=bank: e.activation(out=yex[:, ct, dh * 512:(dh + 1) * 512], in_=PS[bank], func=AF.Copy),
                                 reads=[PB(bank)], writes=["yex"])
                        else:
                            S.op("dve", lambda e, ct=ct, dh=dh, bank=bank: e.tensor_copy(out=yex[:, ct, dh * 512:(dh + 1) * 512], in_=PS[bank]), reads=[PB(bank)], writes=["yex"])
                for i in range(NT):
                    for dh in range(2):
                        pb, pbk = fR2.next()
                        for ch in range(2):
                            S.op("pe", lambda e, i=i, dh=dh, ch=ch, pb=pb: e.matmul(pb, lhsT=PTm[:, ch, i * 128:(i + 1) * 128], rhs=yex[:, ch, dh * 512:(dh + 1) * 512],
                                                                                    start=(ch == 0), stop=(ch == 1)),
                                 reads=["PTm", "yex"], writes=[pbk])
                        if ex == 0:
                            S.op("dve", lambda e, i=i, dh=dh, pb=pb, ex=ex: e.tensor_scalar(out=yacc[:, i, dh * 512:(dh + 1) * 512], in0=pb, scalar1=afft[:, i, ex:ex + 1],
                                                                                            scalar2=None, op0=ALU.mult),
                                 reads=[pbk], writes=[("yacc", i, dh)])
                        else:
                            S.op("dve", lambda e, i=i, dh=dh, pb=pb, ex=ex: e.scalar_tensor_tensor(out=yacc[:, i, dh * 512:(dh + 1) * 512], in0=pb, scalar=afft[:, i, ex:ex + 1],
                                                                                                   in1=yacc[:, i, dh * 512:(dh + 1) * 512], op0=ALU.mult, op1=ALU.add),
                                 reads=[pbk, ("yacc", i, dh)], writes=[("yacc", i, dh)])
            S.barrier()
            A.release(y_mark)
            g2bc = A.alloc("g2bc", [128, D], F32)
            xt = A.alloc("xt", [128, D], F32)
            ot = A.alloc("ot", [128, D], F32)
            S.dma("sp", lambda e: e.dma_start(out=g2bc, in_=modrow_d[b:b + 1, 3072:4096].partition_broadcast(128)), reads=["modrow_d"], writes=["g2bc"])
            for i in range(NT):
                ts_ = slice(i * 128, (i + 1) * 128)
                S.dma("sp", lambda e, ts_=ts_: e.dma_start(out=xt, in_=out_d[b, ts_, :]), reads=[("outd", b, i)], writes=["xt"])
                S.op("dve", lambda e, i=i: e.tensor_tensor(out=ot, in0=yacc[:, i, :], in1=g2bc, op=ALU.mult), reads=[("yacc", i, 0), ("yacc", i, 1), "g2bc"], writes=["ot"])
                S.op("pool", lambda e: e.tensor_tensor(out=ot, in0=ot, in1=xt, op=ALU.add), reads=["ot", "xt"], writes=["ot"])
                S.dma("sp", lambda e, ts_=ts_: e.dma_start(out=out_d[b, ts_, :], in_=ot), reads=["ot", ("outd", b, i)], writes=[("outd", b, i)])
            S.barrier()

        S.barrier()
        print("arena peak bytes", A.peak, "instr counts", {e: len(v) for e, v in S.prog.items()})
        with nc.Block() as block:
            S.emit(block)
    return nc


def _t5_bucket(rel):
    half, exact = 16, 8
    n = np.abs(rel)
    log_ratio = np.log(np.maximum(n, 1).astype(np.float32) / exact) / math.log(1024 / exact)
    large = np.minimum(exact + (log_ratio * (half - exact)).astype(np.int32), half - 1)
    return np.where(rel > 0, half, 0) + np.where(n < exact, n, large)


def _consts():
    c = {}
    c["c_ident"] = np.eye(128, dtype=np.float32)
    c["c_jrev"] = np.ascontiguousarray(np.eye(128, dtype=np.float32)[::-1])
    selab = np.zeros((128, 2, 128), np.float32)
    for m in range(64):
        selab[m + 64, 0, m] = 1.0
        selab[m, 1, m + 64] = 1.0
    c["c_selab"] = selab
    x = np.arange(896)[None, :] - 384
    kp = np.arange(128)[:, None]
    c["c_maskF"] = np.where(x >= kp, 0.0, NEG).astype(np.float32)
    c["c_maskB"] = np.where(x <= kp, 0.0, NEG).astype(np.float32)
    c["c_iotaf"] = np.broadcast_to(np.arange(256, dtype=np.float32)[None, :], (128, 256)).copy()
    c["c_iotap"] = np.stack([np.arange(128, dtype=np.float32), np.arange(128, dtype=np.float32) + 128], axis=1)
    selfb = np.zeros((16, 8, 128), np.float32)
    for h in range(4):
        selfb[4 + h, h, :] = -1.0
        selfb[12 + h, 4 + h, :] = 1.0
    c["c_selfb"] = selfb
    sele = np.zeros((16, 16, 128), np.float32)
    for e in range(16):
        sele[e, e, :] = 1.0
    c["c_sele"] = sele
    sel2 = np.zeros((2, 2, 128), np.float32)
    sel2[0, 0, :] = 1.0
    sel2[1, 1, :] = 1.0
    c["c_sel2"] = sel2
    comb = np.zeros((16, 3, 8), np.float32)
    for h in range(4):
        comb[h, 0, h] = 1.0
        comb[4 + h, 1, h] = 1.0
        comb[8 + h, 0, 4 + h] = 1.0
        comb[12 + h, 1, 4 + h] = -1.0
        comb[12 + h, 2, 4 + h] = 1.0
    c["c_comb"] = comb
    c["c_ones128"] = np.full((128, 128), 1.0 / 128.0, np.float32)
    blk = np.zeros((128, 128), np.float32)
    blk[0:64, 0:64] = 1.0 / 64.0
    blk[64:128, 64:128] = 1.0 / 64.0
    c["c_blk64"] = blk
    oh = np.zeros((32, 3, 384), np.float32)
    for p, d in enumerate((1, 4, 16)):
        for y in range(0, 129):
            rel = 64 - y
            bkt = int(_t5_bucket(np.array(rel * d)))
            oh[bkt, p, 127 + y] = 1.0
    c["c_onehot"] = oh
    return c


_NC_CACHE = {}


def _blockdiag(wblk):
    out = np.zeros((4, 128, 128), np.float32)
    for h in range(4):
        for g in range(32):
            out[h, 4 * g:4 * g + 4, 4 * g:4 * g + 4] = wblk[32 * h + g]
    return out


def make_in_maps(inputs, n_cores=8):
    f = lambda a: np.ascontiguousarray(np.asarray(a, dtype=np.float32))
    x = f(inputs["x"]); c = f(inputs["c"])
    shared = {}
    shared["w_ada"] = f(inputs["w_ada"][0])
    shared["b_ada_fm"] = f(inputs["b_ada"][0].reshape(48, 128).T)
    shared["b_ada_row"] = f(inputs["b_ada"][0].reshape(1, 6 * D))
    shared["g1_fm"] = f(inputs["norm1_g"][0].reshape(8, 128).T)
    shared["g2_row"] = f(inputs["norm2_g"][0].reshape(1, D))
    shared["w_in"] = f(inputs["w_in"][0])
    shared["convw_fm"] = f(np.transpose(inputs["conv_w"][0].reshape(5, 4, 128), (2, 1, 0)))
    shared["convb_fm"] = f(inputs["conv_b"][0].reshape(4, 128).T)
    shared["mng_fm"] = f(inputs["mlstm_norm_g"][0].reshape(4, 128).T)
    shared["mskip_fm"] = f(inputs["mlstm_skip"][0].reshape(4, 128).T)
    shared["wq_bd"] = _blockdiag(np.asarray(inputs["w_q_blk"][0]))
    shared["wk_bd"] = _blockdiag(np.asarray(inputs["w_k_blk"][0]))
    shared["wv_bd"] = _blockdiag(np.asarray(inputs["w_v_blk"][0]))
    bi = np.asarray(inputs["b_igate"][0]); bf = np.asarray(inputs["b_fgate"][0])
    shared["gate_bias"] = f(np.concatenate([bi[0], bf[0], bi[1], bf[1]]).reshape(16, 1))
    shared["gq"] = f(np.tile(np.asarray(inputs["q_norm_g"][0]), 2).reshape(128, 1))
    shared["gk"] = f(np.tile(np.asarray(inputs["k_norm_g"][0]), 2).reshape(128, 1))
    shared["rel_bias"] = f(inputs["rel_bias"])
    shared["w_out"] = f(inputs["w_out"][0])
    shared["wr_fm"] = f(np.transpose(np.asarray(inputs["w_router"][0]).reshape(8, 128, 16), (1, 0, 2)))
    shared["b_router"] = f(inputs["b_router"][0].reshape(1, 16))
    shared["w_gate"] = f(inputs["w_gate"][0])
    shared["w_up"] = f(inputs["w_up"][0])
    shared["w_down"] = f(inputs["w_down"][0])
    shared.update(_consts())
    maps = []
    for i in range(n_cores):
        m = dict(shared)
        m["x"] = np.ascontiguousarray(x[2 * i:2 * i + 2])
        cc = c[2 * i:2 * i + 2]
        m["cT"] = np.ascontiguousarray(np.transpose(cc.reshape(2, 8, 128), (2, 1, 0)))
        maps.append(m)
    return maps


def kernel(**inputs):
    if "nc" not in _NC_CACHE:
        _NC_CACHE["nc"] = build_program()
    nc = _NC_CACHE["nc"]
    maps = make_in_maps(inputs, 8)
    res = run_bass_kernel_spmd(nc, maps, core_ids=list(range(8)))
    out = np.concatenate([np.asarray(r["out"]) for r in res.results], axis=0)
    return out.astype(np.float32)
```

```python
import math
from contextlib import ExitStack
import numpy as np
import concourse.bass as bass
import concourse.mybir as mybir
from concourse.bass_utils import run_bass_kernel_spmd

F32 = mybir.dt.float32
BF16 = mybir.dt.bfloat16
AF = mybir.ActivationFunctionType
ALU = mybir.AluOpType
AX = mybir.AxisListType

S_LEN = 2048
D = 1024
NT = 16
NEXP = 16
CAP = 256
DFF = 2048
EPS = 1e-6
NEG = -30000.0
SAME_ENGINE_SYNC = True
N_DMA_SEMS = 24


class _Rec:
    def __init__(self):
        self.call = None

    def __getattr__(self, name):
        def f(*a, **k):
            self.call = (name, a, k)
            return self
        return f


def _bind(fn):
    rec = _Rec()
    fn(rec)
    assert rec.call is not None
    return rec.call


class Sched:
    ENGS = ["pe", "act", "dve", "pool", "sp"]

    def __init__(self, sems):
        self.prog = {e: [] for e in self.ENGS}
        self.cnt = {}
        self.res = {}
        self.waited = {e: {} for e in self.ENGS}
        self.sems = sems
        self.eng_sem = {e: "c_" + e for e in self.ENGS}
        for e in self.ENGS:
            self.cnt["c_" + e] = 0
        self.dma_names = ["d%d" % i for i in range(N_DMA_SEMS)]
        for n in self.dma_names:
            self.cnt[n] = 0
        self.dma_rr = 0
        self.dma_rr_pool = 0

    def _deps(self, reads, writes):
        deps = {}

        def add(tok):
            if tok is None:
                return
            s, v = tok
            if deps.get(s, 0) < v:
                deps[s] = v
        for r in reads:
            st = self.res.get(r)
            if st is not None:
                add(st["w"])
        for w in writes:
            st = self.res.get(w)
            if st is not None:
                add(st["w"])
                for s, v in st["r"].items():
                    add((s, v))
        return deps

    def _commit(self, tok, reads, writes):
        s, v = tok
        for r in reads:
            st = self.res.setdefault(r, {"w": None, "r": {}})
            if st["r"].get(s, 0) < v:
                st["r"][s] = v
        for w in writes:
            self.res[w] = {"w": tok, "r": {}}

    def op(self, eng, fn, reads=(), writes=()):
        deps = self._deps(reads, writes)
        own = self.eng_sem[eng]
        waits = []
        for s, v in deps.items():
            if s == own and (eng == "pe" or not SAME_ENGINE_SYNC):
                continue
            if self.waited[eng].get(s, 0) >= v:
                continue
            self.waited[eng][s] = v
            waits.append((s, v))
        self.cnt[own] += 1
        tok = (own, self.cnt[own])
        self.prog[eng].append((_bind(fn), waits, (own, 1)))
        self._commit(tok, reads, writes)

    def dma(self, eng, fn, reads=(), writes=()):
        deps = self._deps(reads, writes)
        half = len(self.dma_names) // 2
        if eng == "pool":
            name = self.dma_names[half + self.dma_rr_pool % half]
            self.dma_rr_pool += 1
        else:
            name = self.dma_names[self.dma_rr % half]
            self.dma_rr += 1
        prev = self.cnt[name]
        if prev > 0 and deps.get(name, 0) < prev:
            deps[name] = prev
        waits = []
        for s, v in deps.items():
            if self.waited[eng].get(s, 0) >= v:
                continue
            self.waited[eng][s] = v
            waits.append((s, v))
        self.cnt[name] += 16
        tok = (name, self.cnt[name])
        self.prog[eng].append((_bind(fn), waits, (name, 16)))
        self._commit(tok, reads, writes)
        return tok

    def barrier(self):
        for e in self.ENGS:
            waits = []
            for s, v in self.cnt.items():
                if v == 0:
                    continue
                if self.waited[e].get(s, 0) >= v:
                    continue
                self.waited[e][s] = v
                waits.append((s, v))
            if waits:
                self.prog[e].append((None, waits, None))

    def emit(self, block):
        sems = self.sems

        def mk(engname):
            def body(e):
                for fn, waits, inc in self.prog[engname]:
                    for s, v in waits:
                        e.wait_ge(sems[s], v)
                    if fn is not None:
                        name, a, k = fn
                        ins = getattr(e, name)(*a, **k)
                        ins.then_inc(sems[inc[0]], inc[1])
            return body
        block.tensor(mk("pe"))
        block.scalar(mk("act"))
        block.vector(mk("dve"))
        block.gpsimd(mk("pool"))
        block.sync(mk("sp"))


class Arena:
    def __init__(self, ar, nbytes):
        self.ar = ar
        self.top = 0
        self.nbytes = nbytes
        self.limit = nbytes
        self.gen = 0
        self.peak = 0

    def alloc(self, name, shape, dt, parts=128):
        esz = 2 if dt == BF16 else 4
        n = 1
        for s in shape[1:]:
            n *= s
        nb = (n * esz + 31) // 32 * 32
        off = self.top
        self.top += nb
        self.peak = max(self.peak, self.top)
        assert self.top <= self.limit, (name, self.top, self.limit)
        v = self.ar[:, off // 4: (off + nb) // 4]
        if dt == BF16:
            v = v.bitcast(BF16)
        v = v[:, 0:n]
        if len(shape) == 3:
            v = v.rearrange("p (a b) -> p a b", a=shape[1])
        elif len(shape) == 4:
            v = v.rearrange("p (a b c) -> p a b c", a=shape[1], b=shape[2])
        if shape[0] < 128:
            v = v[0:shape[0]]
        self.gen += 1
        return v

    def mark(self):
        return self.top

    def release(self, m):
        self.top = m


def build_program(debug=None, nseq=2, max_phase=9):
    nc = bass.Bass("TRN2", target_bir_lowering=False)
    dr = {}

    def din(name, shape, dt=F32):
        dr[name] = nc.dram_tensor(name, list(shape), dt, kind="ExternalInput").ap()
        return dr[name]
    x_d = din("x", [2, S_LEN, D])
    cT_d = din("cT", [128, 8, 2])
    wada_d = din("w_ada", [D, 6 * D])
    bada_fm_d = din("b_ada_fm", [128, 48])
    bada_row_d = din("b_ada_row", [1, 6 * D])
    g1_d = din("g1_fm", [128, 8])
    g2_d = din("g2_row", [1, D])
    win_d = din("w_in", [D, 2576])
    convw_d = din("convw_fm", [128, 4, 5])
    convb_d = din("convb_fm", [128, 4])
    mng_d = din("mng_fm", [128, 4])
    msk_d = din("mskip_fm", [128, 4])
    wq_d = din("wq_bd", [4, 128, 128])
    wk_d = din("wk_bd", [4, 128, 128])
    wv_d = din("wv_bd", [4, 128, 128])
    gb_d = din("gate_bias", [16, 1])
    gq_d = din("gq", [128, 1])
    gk_d = din("gk", [128, 1])
    relb_d = din("rel_bias", [32, 8])
    wout_d = din("w_out", [D, D])
    wr_d = din("wr_fm", [128, 8, 16])
    br_d = din("b_router", [1, 16])
    wg_d = din("w_gate", [NEXP, D, DFF])
    wu_d = din("w_up", [NEXP, D, DFF])
    wd_d = din("w_down", [NEXP, DFF, D])
    ident_d = din("c_ident", [128, 128])
    jrev_d = din("c_jrev", [128, 128])
    selab_d = din("c_selab", [128, 2, 128])
    maskF_d = din("c_maskF", [128, 896])
    maskB_d = din("c_maskB", [128, 896])
    iotaf_d = din("c_iotaf", [128, 256])
    iotap_d = din("c_iotap", [128, 2])
    selfb_d = din("c_selfb", [16, 8, 128])
    sele_d = din("c_sele", [16, 16, 128])
    sel2_d = din("c_sel2", [2, 2, 128])
    comb_d = din("c_comb", [16, 3, 8])
    ones128_d = din("c_ones128", [128, 128])
    blk64_d = din("c_blk64", [128, 128])
    onehot_d = din("c_onehot", [32, 3, 384])
    out_d = nc.dram_tensor("out", [2, S_LEN, D], F32, kind="ExternalOutput").ap()
    modrow_d = nc.dram_tensor("modrow_s", [2, 4 * D], F32, kind="Internal").ap()
    ftab_d = nc.dram_tensor("ftab_s", [8, 3, 384], F32, kind="Internal").ap()
    dbg = {}
    if debug:
        for nm, shp in debug.items():
            dbg[nm] = nc.dram_tensor("dbg_" + nm, list(shp), F32, kind="ExternalOutput").ap()

    ARENA_BYTES = 206 * 1024
    with ExitStack() as es:
        arena_t = es.enter_context(nc.sbuf_tensor("arena", [128, ARENA_BYTES // 4], F32))
        PSD = [es.enter_context(nc.psum_tensor("psd%d" % i, [128, 1024], F32))[:] for i in range(4)]
        PS = []
        for i in range(4):
            PS.append(PSD[i][:, 0:512])
            PS.append(PSD[i][:, 512:1024])
        names = ["c_pe", "c_act", "c_dve", "c_pool", "c_sp"] + ["d%d" % i for i in range(N_DMA_SEMS)]
        sems = {n: es.enter_context(nc.semaphore(n)) for n in names}
        S = Sched(sems)
        A = Arena(arena_t, ARENA_BYTES)
        uid = [0]

        def R(name):
            uid[0] += 1
            return "%s#%d" % (name, uid[0])

        def PB(i):
            return ("ps", i)

        class Ring:
            def __init__(self, name, n, shape=None, dt=None, banks=None):
                self.n = n
                self.i = 0
                if banks is not None:
                    self.items = [(PS[bk], PB(bk)) for bk in banks]
                    self.n = len(banks)
                else:
                    self.items = [(A.alloc(name + str(j), shape, dt), R(name + str(j))) for j in range(n)]

            def next(self):
                it = self.items[self.i % self.n]
                self.i += 1
                return it

        def load(eng, dst, src, key, reads=()):
            S.dma(eng, lambda e: e.dma_start(out=dst, in_=src), reads=list(reads), writes=[key])

        def dump(name, src, key):
            if name in dbg:
                S.dma("sp", lambda e: e.dma_start(out=dbg[name], in_=src), reads=[key], writes=["dbg_" + name])

        ident = A.alloc("ident", [128, 128], F32)
        identb = A.alloc("identb", [128, 128], BF16)
        jrev = A.alloc("jrev", [128, 128], F32)
        selab = A.alloc("selab", [128, 2, 128], F32)
        onesb = A.alloc("onesb", [128, 128], BF16)
        ones128 = A.alloc("ones128", [128, 128], F32)
        blk64 = A.alloc("blk64", [128, 128], F32)
        maskF = A.alloc("maskF", [128, 896], F32)
        maskB = A.alloc("maskB", [128, 896], F32)
        iotaf = A.alloc("iotaf", [128, 256], F32)
        iotap = A.alloc("iotap", [128, 2], F32)
        selfb = A.alloc("selfb", [16, 8, 128], F32)
        sele = A.alloc("sele", [16, 16, 128], F32)
        sel2 = A.alloc("sel2", [2, 2, 128], F32)
        comb = A.alloc("comb", [16, 3, 8], F32)
        g1 = A.alloc("g1", [128, 8], F32)
        convw = A.alloc("convw", [128, 4, 5], F32)
        convb = A.alloc("convb", [128, 4], F32)
        mng = A.alloc("mng", [128, 4], F32)
        msk = A.alloc("msk", [128, 4], F32)
        gbias = A.alloc("gbias", [16, 1], F32)
        gq = A.alloc("gq", [128, 1], F32)
        gk = A.alloc("gk", [128, 1], F32)
        wr = A.alloc("wr", [128, 8, 16], F32)
        brbc = A.alloc("brbc", [128, 16], F32)
        wqb = A.alloc("wqb", [128, 4, 128], BF16)
        wkb = A.alloc("wkb", [128, 4, 128], BF16)
        wvb = A.alloc("wvb", [128, 4, 128], BF16)
        A1 = A.alloc("A1", [128, 2, 8], F32)
        B1 = A.alloc("B1", [128, 2, 8], F32)
        CONST = "const"
        for dst, src in [(ident, ident_d), (jrev, jrev_d), (selab, selab_d), (ones128, ones128_d), (blk64, blk64_d), (maskF, maskF_d),
                         (maskB, maskB_d), (iotaf, iotaf_d), (iotap, iotap_d), (selfb, selfb_d),
                         (sele, sele_d), (sel2, sel2_d), (comb, comb_d), (g1, g1_d), (convw, convw_d),
                         (convb, convb_d), (mng, mng_d), (msk, msk_d), (gbias, gb_d), (gq, gq_d),
                         (gk, gk_d), (wr, wr_d)]:
            S.dma("sp", lambda e, dst=dst, src=src: e.dma_start(out=dst, in_=src), writes=[R("cl")])
        S.dma("sp", lambda e: e.dma_start(out=brbc, in_=br_d.partition_broadcast(128)), writes=[R("cl")])
        for dst, src in [(wqb, wq_d), (wkb, wk_d), (wvb, wv_d)]:
            S.dma("pool", lambda e, dst=dst, src=src: e.dma_start(out=dst, in_=src.rearrange("h p n -> p h n")),
                  writes=[R("cl")])
        S.barrier()
        S.op("act", lambda e: e.activation(out=identb, in_=ident, func=AF.Copy), writes=[R("cl")])
        S.op("pool", lambda e: e.memset(onesb, 1.0), writes=[R("cl")])
        S.barrier()

        m0 = A.mark()
        sc = A.alloc("sc", [128, 8, 2], F32)
        wpiece = A.alloc("wpiece", [128, 8, 1024], F32)
        bfm = A.alloc("bfm", [128, 48], F32)
        brow = A.alloc("brow", [2, 4096], F32)
        mrow = A.alloc("mrow", [2, 4096], F32)
        modfm = A.alloc("modfm", [128, 2, 8, 2], F32)
        load("sp", sc, cT_d, "sc")
        load("sp", bfm, bada_fm_d, "bfm")
        load("sp", brow[0:1, :], bada_row_d[0:1, 2048:6144], "brow0")
        load("sp", brow[1:2, :], bada_row_d[0:1, 2048:6144], "brow1")
        S.op("act", lambda e: e.activation(out=sc, in_=sc, func=AF.Silu), reads=["sc"], writes=["sc"])
        wada_v = wada_d.rearrange("(k p) n -> p k n", p=128)
        for piece in range(6):
            for k in range(8):
                load("sp", wpiece[:, k, :], wada_v[:, k, piece * 1024:(piece + 1) * 1024], ("wpiece", k))
            wp_keys = [("wpiece", k) for k in range(8)]
            if piece < 2:
                for j in range(8):
                    for k in range(8):
                        S.op("pe", lambda e, j=j, k=k: e.matmul(PS[0][:, 2 * j:2 * j + 2], lhsT=wpiece[:, k, j * 128:(j + 1) * 128],
                                                                 rhs=sc[:, k, :], start=(k == 0), stop=(k == 7)),
                             reads=wp_keys + ["sc"], writes=[PB(0)])
                S.op("dve", lambda e, piece=piece: e.tensor_copy(out=modfm[:, piece, :, :], in_=PS[0][:, 0:16].rearrange("p (j b) -> p j b", b=2)),
                     reads=[PB(0)], writes=[("modfm", piece)])
            else:
                for half in range(2):
                    for k in range(8):
                        S.op("pe", lambda e, half=half, k=k: e.matmul(PS[1][0:2, :], lhsT=sc[:, k, :],
                                                                       rhs=wpiece[:, k, half * 512:(half + 1) * 512],
                                                                       start=(k == 0), stop=(k == 7)),
                             reads=wp_keys + ["sc"], writes=[PB(1)])
                    c0 = (piece - 2) * 1024 + half * 512
                    S.op("dve", lambda e, c0=c0: e.tensor_tensor(out=mrow[:, c0:c0 + 512], in0=PS[1][0:2, :], in1=brow[:, c0:c0 + 512], op=ALU.add),
                         reads=[PB(1), "brow0", "brow1"], writes=[("mrow", c0)])
        for b in range(2):
            S.op("dve", lambda e, b=b: e.tensor_tensor(out=B1[:, b, :], in0=modfm[:, 0, :, b], in1=bfm[:, 0:8], op=ALU.add),
                 reads=[("modfm", 0), "bfm"], writes=[("B1", b)])
            S.op("dve", lambda e, b=b: e.tensor_tensor(out=A1[:, b, :], in0=modfm[:, 1, :, b], in1=bfm[:, 8:16], op=ALU.add),
                 reads=[("modfm", 1), "bfm"], writes=[("A1", b)])
            S.op("dve", lambda e, b=b: e.scalar_tensor_tensor(out=A1[:, b, :], in0=A1[:, b, :], scalar=1.0, in1=g1, op0=ALU.add, op1=ALU.mult),
                 reads=[("A1", b)], writes=[("A1", b)])
        S.dma("sp", lambda e: e.dma_start(out=modrow_d, in_=mrow), reads=[("mrow", c) for c in range(0, 4096, 512)], writes=["modrow_d"])
        S.barrier()
        A.release(m0)

        m0 = A.mark()
        relb = A.alloc("relb", [32, 8], F32)
        onehot = A.alloc("onehot", [32, 3, 384], F32)
        ftab = A.alloc("ftab", [8, 3, 384], F32)
        load("sp", relb, relb_d, "relb")
        load("sp", onehot, onehot_d, "onehot")
        for p in range(3):
            S.op("pe", lambda e, p=p: e.matmul(PS[0][0:8, 0:384], lhsT=relb, rhs=onehot[:, p, :], start=True, stop=True),
                 reads=["relb", "onehot"], writes=[PB(0)])
            S.op("act", lambda e, p=p: e.activation(out=ftab[:, p, :], in_=PS[0][0:8, 0:384], func=AF.Exp), reads=[PB(0)], writes=[("ftab", p)])
        for p in range(3):
            S.op("pe", lambda e, p=p: e.matmul(PS[1][0:8, 0:384], lhsT=ones128[0:32, 0:8], rhs=onehot[:, p, :], start=True, stop=True),
                 reads=["onehot"], writes=[PB(1)])
            S.op("dve", lambda e, p=p: e.scalar_tensor_tensor(out=ftab[:, p, :], in0=PS[1][0:8, 0:384], scalar=128.0, in1=ftab[:, p, :],
                                                                op0=ALU.mult, op1=ALU.mult),
                 reads=[PB(1), ("ftab", p)], writes=[("ftab", p)])
        S.dma("sp", lambda e: e.dma_start(out=ftab_d, in_=ftab), reads=[("ftab", p) for p in range(3)], writes=["ftab_d"])
        S.barrier()
        A.release(m0)

        pers_mark = A.mark()
        win_v = win_d.rearrange("(k p) n -> p k n", p=128)
        wout_v = wout_d.rearrange("(k p) n -> p k n", p=128)

        for b in range(nseq):
            A.release(pers_mark)
            A.limit = ARENA_BYTES - 8 * S_LEN * 2
            catT = arena_t[:, (ARENA_BYTES - 8 * S_LEN * 2) // 4: ARENA_BYTES // 4].bitcast(BF16).rearrange("p (a b) -> p a b", a=8)
            hT = A.alloc("hT", [128, 8, S_LEN], BF16)
            seq_mark = A.mark()
            xt = A.alloc("xt", [128, D], F32)
            xs = A.alloc("xs", [128, D], F32)
            junk = A.alloc("junk", [128, D], F32)
            st1 = A.alloc("st1", [128, 4], F32)
            for i in range(NT):
                load("sp", xt, x_d[b, i * 128:(i + 1) * 128, :], "xt")
                S.op("act", lambda e: e.activation(out=junk, in_=xt, func=AF.Square, accum_out=st1[:, 0:1]), reads=["xt"], writes=["junk", "st1"])
                S.op("act", lambda e: e.activation(out=st1[:, 1:2], in_=st1[:, 0:1], func=AF.Ln, scale=1.0 / D, bias=EPS), reads=["st1"], writes=["st1"])
                S.op("act", lambda e: e.activation(out=st1[:, 2:3], in_=st1[:, 1:2], func=AF.Exp, scale=-0.5), reads=["st1"], writes=["st1"])
                S.op("dve", lambda e: e.tensor_scalar(out=xs, in0=xt, scalar1=st1[:, 2:3], scalar2=None, op0=ALU.mult), reads=["xt", "st1"], writes=["xs"])
                for k in range(8):
                    bank = k // 4
                    S.op("pe", lambda e, k=k, bank=bank: e.matmul(PS[bank][:, (k % 4) * 128:(k % 4 + 1) * 128], lhsT=xs[:, k * 128:(k + 1) * 128],
                                                                   rhs=ident, start=True, stop=True),
                         reads=["xs"], writes=[PB(bank)])
                for k in range(8):
                    bank = k // 4
                    eng = "dve" if bank == 0 else "act"
                    if eng == "dve":
                        S.op("dve", lambda e, k=k, bank=bank, i=i: e.tensor_scalar(out=hT[:, k, i * 128:(i + 1) * 128],
                                                                                   in0=PS[bank][:, (k % 4) * 128:(k % 4 + 1) * 128],
                                                                                   scalar1=A1[:, b, k:k + 1], scalar2=B1[:, b, k:k + 1],
                                                                                   op0=ALU.mult, op1=ALU.add),
                             reads=[PB(bank), ("A1", b), ("B1", b)], writes=[("hT", i)])
                    else:
                        S.op("act", lambda e, k=k, bank=bank, i=i: e.activation(out=hT[:, k, i * 128:(i + 1) * 128],
                                                                                in_=PS[bank][:, (k % 4) * 128:(k % 4 + 1) * 128],
                                                                                func=AF.Identity, scale=A1[:, b, k:k + 1], bias=B1[:, b, k:k + 1]),
                             reads=[PB(bank), ("A1", b), ("B1", b)], writes=[("hT", i)])
            hT_keys = [("hT", i) for i in range(NT)]
            if b == 0 and "hT" in dbg:
                S.dma("pool", lambda e: e.dma_start(out=dbg["hT"], in_=hT), reads=hT_keys, writes=["dbg_hT"])
            S.barrier()
            A.release(seq_mark)

            Cg = A.alloc("Cg", [16, S_LEN], F32)
            Dg = A.alloc("Dg", [16, S_LEN], F32)
            utok = A.alloc("utok", [128, 128], F32)
            g_mark = A.mark()
            Zg = A.alloc("Zg", [16, S_LEN], F32)
            Lg = A.alloc("Lg", [16, S_LEN], F32)
            onesr = A.alloc("onesr", [16, S_LEN], F32)
            wgt = A.alloc("wgt", [128, 8, 16], BF16)
            S.dma("pool", lambda e: e.dma_start(out=wgt, in_=win_v[:, :, 1024:1040]), writes=["wgt"])
            S.op("pool", lambda e: e.memset(onesr, 1.0), writes=["onesr"])
            for blk in range(4):
                for k in range(8):
                    S.op("pe", lambda e, k=k, blk=blk: e.matmul(PS[0][0:16, :], lhsT=wgt[:, k, :], rhs=hT[:, k, blk * 512:(blk + 1) * 512],
                                                                 start=(k == 0), stop=(k == 7)),
                         reads=["wgt"] + hT_keys, writes=[PB(0)])
                S.op("dve", lambda e, blk=blk: e.tensor_scalar(out=Zg[:, blk * 512:(blk + 1) * 512], in0=PS[0][0:16, :], scalar1=gbias[:, 0:1],
                                                               scalar2=None, op0=ALU.add),
                     reads=[PB(0)], writes=[("Zg", blk)])
            zk = [("Zg", blk) for blk in range(4)]
            S.op("act", lambda e: e.activation(out=Lg, in_=Zg, func=AF.Exp, scale=-1.0), reads=zk, writes=["Lg"])
            S.op("act", lambda e: e.activation(out=Lg, in_=Lg, func=AF.Ln, bias=1.0), reads=["Lg"], writes=["Lg"])
            S.op("dve", lambda e: e.tensor_tensor_scan(out=Cg, data0=onesr, data1=Lg, initial=0.0, op0=ALU.mult, op1=ALU.add),
                 reads=["onesr", "Lg"], writes=["Cg"])
            S.op("dve", lambda e: e.tensor_tensor(out=Dg, in0=Cg, in1=Lg, op=ALU.subtract), reads=["Cg", "Lg"], writes=["Dg"])
            for i in range(NT):
                sl = slice(i * 128, (i + 1) * 128)
                S.op("pe", lambda e, i=i, sl=sl: e.matmul(PS[1][:, i * 8:(i + 1) * 8], lhsT=Zg[:, sl], rhs=comb[:, 0, :], start=True, stop=False),
                     reads=zk, writes=[PB(1)])
                S.op("pe", lambda e, i=i, sl=sl: e.matmul(PS[1][:, i * 8:(i + 1) * 8], lhsT=Cg[:, sl], rhs=comb[:, 1, :], start=False, stop=False),
                     reads=["Cg"], writes=[PB(1)])
                S.op("pe", lambda e, i=i, sl=sl: e.matmul(PS[1][:, i * 8:(i + 1) * 8], lhsT=Lg[:, sl], rhs=comb[:, 2, :], start=False, stop=True),
                     reads=["Lg"], writes=[PB(1)])
            S.op("dve", lambda e: e.tensor_copy(out=utok, in_=PS[1][:, 0:128]), reads=[PB(1)], writes=["utok"])
            if b == 0:
                dump("Cg", Cg, "Cg")
                dump("utok", utok, "utok")
            S.barrier()
            A.release(g_mark)

            m_mark = A.mark()
            wxm = A.alloc("wxm", [128, 8, 128], BF16)
            wop = A.alloc("wop", [128, 8, 128], BF16)
            xmp = A.alloc("xmp", [128, S_LEN + 4], F32)
            sigo = A.alloc("sigo", [128, S_LEN], BF16)
            cacc = A.alloc("cacc", [128, S_LEN], F32)
            xc = A.alloc("xc", [128, S_LEN], BF16)
            xmb = A.alloc("xmb", [128, S_LEN], BF16)
            qT = A.alloc("qT", [128, S_LEN], BF16)
            kT = A.alloc("kT", [128, S_LEN], BF16)
            vtok = A.alloc("vtok", [128, NT, 128], BF16)
            bcR = Ring("bc", 4, [128, 512], F32)
            tmR = Ring("tmpm", 3, [128, 512], F32)
            wtR = Ring("Wt", 4, [128, 512], F32)
            swR = Ring("STw", 4, [128, 512], BF16)
            fR = Ring("fin", 8, [128, 512], F32)
            stR = Ring("st", 0, banks=[4, 5, 6, 7])
            S.op("pool", lambda e: e.memset(xmp, 0.0), writes=["xmp"])
            mpR = Ring("mpR", 0, banks=[0, 1, 2, 3])
            for h in range(4):
                S.dma("pool", lambda e, h=h: e.dma_start(out=wxm, in_=win_v[:, :, h * 128:(h + 1) * 128]), writes=["wxm"])
                S.dma("pool", lambda e, h=h: e.dma_start(out=wop, in_=win_v[:, :, 512 + h * 128:512 + (h + 1) * 128]), writes=["wop"])
                for blk in range(4):
                    bs = slice(blk * 512, (blk + 1) * 512)
                    pa, pak = mpR.next()
                    for k in range(8):
                        S.op("pe", lambda e, k=k, bs=bs, pa=pa: e.matmul(pa, lhsT=wxm[:, k, :], rhs=hT[:, k, bs], start=(k == 0), stop=(k == 7)),
                             reads=["wxm"] + hT_keys, writes=[pak])
                    S.op("dve", lambda e, blk=blk, pa=pa: e.tensor_copy(out=xmp[:, 2 + blk * 512:2 + (blk + 1) * 512], in_=pa),
                         reads=[pak], writes=["xmp"])
                    pb_, pbk_ = mpR.next()
                    for k in range(8):
                        S.op("pe", lambda e, k=k, bs=bs, pb_=pb_: e.matmul(pb_, lhsT=wop[:, k, :], rhs=hT[:, k, bs], start=(k == 0), stop=(k == 7)),
                             reads=["wop"] + hT_keys, writes=[pbk_])
                    S.op("act", lambda e, bs=bs, pb_=pb_: e.activation(out=sigo[:, bs], in_=pb_, func=AF.Sigmoid), reads=[pbk_], writes=["sigo"])
                S.op("dve", lambda e, h=h: e.tensor_scalar(out=cacc, in0=xmp[:, 0:S_LEN], scalar1=convw[:, h, 0:1], scalar2=None, op0=ALU.mult),
                     reads=["xmp"], writes=["cacc"])
                for j in range(1, 5):
                    S.op("dve", lambda e, h=h, j=j: e.scalar_tensor_tensor(out=cacc, in0=xmp[:, j:j + S_LEN], scalar=convw[:, h, j:j + 1], in1=cacc,
                                                                          op0=ALU.mult, op1=ALU.add),
                         reads=["xmp", "cacc"], writes=["cacc"])
                S.op("act", lambda e, h=h: e.activation(out=xc, in_=cacc, func=AF.Silu, bias=convb[:, h:h + 1]), reads=["cacc"], writes=["xc"])
                S.op("act", lambda e: e.activation(out=xmb, in_=xmp[:, 2:2 + S_LEN], func=AF.Copy), reads=["xmp"], writes=["xmb"])
                for blk in range(4):
                    bs = slice(blk * 512, (blk + 1) * 512)
                    pa, pak = mpR.next()
                    S.op("pe", lambda e, h=h, bs=bs, pa=pa: e.matmul(pa, lhsT=wqb[:, h, :], rhs=xc[:, bs], start=True, stop=True), reads=["xc"], writes=[pak])
                    S.op("act", lambda e, bs=bs, pa=pa: e.activation(out=qT[:, bs], in_=pa, func=AF.Copy), reads=[pak], writes=["qT"])
                    pb_, pbk_ = mpR.next()
                    S.op("pe", lambda e, h=h, bs=bs, pb_=pb_: e.matmul(pb_, lhsT=wkb[:, h, :], rhs=xc[:, bs], start=True, stop=True), reads=["xc"], writes=[pbk_])
                    S.op("dve", lambda e, bs=bs, pb_=pb_: e.tensor_scalar(out=kT[:, bs], in0=pb_, scalar1=1.0 / math.sqrt(128.0), scalar2=None, op0=ALU.mult),
                         reads=[pbk_], writes=["kT"])
                for i4 in range(4):
                    for ii in range(4):
                        i = i4 * 4 + ii
                        if ii == 0:
                            pa, pak = mpR.next()
                        S.op("pe", lambda e, h=h, i=i, ii=ii, pa=pa: e.matmul(pa[:, ii * 128:(ii + 1) * 128], lhsT=xmb[:, i * 128:(i + 1) * 128],
                                                                               rhs=wvb[:, h, :], start=True, stop=True),
                             reads=["xmb"], writes=[pak])
                    S.op("act", lambda e, i4=i4, pa=pa: e.activation(out=vtok[:, i4 * 4:(i4 + 1) * 4, :], in_=pa.rearrange("p (a b) -> p a b", a=4), func=AF.Copy),
                         reads=[pak], writes=["vtok"])
                for T in range(4):
                    bs = slice(T * 512, (T + 1) * 512)
                    pb1, pb1k = stR.next()
                    S.op("pe", lambda e, h=h, bs=bs, pb1=pb1: e.matmul(pb1, lhsT=selfb[:, h, :], rhs=Cg[:, bs], start=True, stop=True), reads=["Cg"], writes=[pb1k])
                    bcf, bcfk = bcR.next()
                    S.op("act", lambda e, bcf=bcf, pb1=pb1: e.activation(out=bcf, in_=pb1, func=AF.Copy), reads=[pb1k], writes=[bcfk])
                    pb2, pb2k = stR.next()
                    S.op("pe", lambda e, h=h, bs=bs, pb2=pb2: e.matmul(pb2, lhsT=selfb[:, 4 + h, :], rhs=Dg[:, bs], start=True, stop=True), reads=["Dg"], writes=[pb2k])
                    bcb, bcbk = bcR.next()
                    S.op("act", lambda e, bcb=bcb, pb2=pb2: e.activation(out=bcb, in_=pb2, func=AF.Copy), reads=[pb2k], writes=[bcbk])
                    nf = 4 * T + 4
                    nb = 16 - 4 * T
                    cf = 0
                    cb = 0
                    sts = {}

                    def emit_st(j, bs=bs, sts=sts):
                        st, stk = stR.next()
                        S.op("pe", lambda e, j=j, bs=bs, st=st: e.matmul(st, lhsT=kT[:, j * 128:(j + 1) * 128], rhs=qT[:, bs], start=True, stop=True),
                             reads=["kT", "qT"], writes=[stk])
                        sts[j] = (st, stk)
                    LOOK = 2
                    for j in range(LOOK):
                        emit_st(j)
                    for j in range(NT):
                        fwd = j <= 4 * T + 3
                        bwd = j >= 4 * T
                        if j + LOOK < NT:
                            emit_st(j + LOOK)
                        st, stk = sts[j]
                        for dirn in (0, 1):
                            if (dirn == 0 and not fwd) or (dirn == 1 and not bwd):
                                continue
                            bc, bck = (bcf, bcfk) if dirn == 0 else (bcb, bcbk)
                            src, srck = bc, bck
                            if 4 * T <= j <= 4 * T + 3:
                                o = (j - 4 * T) * 128
                                mk = maskF if dirn == 0 else maskB
                                tmpm, tmpk = tmR.next()
                                S.op("pool", lambda e, bc=bc, mk=mk, o=o, tmpm=tmpm: e.tensor_tensor(out=tmpm, in0=bc, in1=mk[:, 384 - o:384 - o + 512], op=ALU.add),
                                     reads=[bck], writes=[tmpk])
                                src, srck = tmpm, tmpk
                            ucol = j * 8 + dirn * 4 + h
                            Wt, Wtk = wtR.next()
                            S.op("act", lambda e, src=src, ucol=ucol, Wt=Wt: e.activation(out=Wt, in_=src, func=AF.Exp, bias=utok[:, ucol:ucol + 1]),
                                 reads=[srck, "utok"], writes=[Wtk])
                            STw, STwk = swR.next()
                            S.op("dve", lambda e, st=st, Wt=Wt, STw=STw: e.tensor_tensor(out=STw, in0=st, in1=Wt, op=ALU.mult), reads=[stk, Wtk], writes=[STwk])
                            if dirn == 0:
                                first, last = (cf == 0), (cf == nf - 1)
                                cf += 1
                                nb_, db_ = 0, 1
                            else:
                                first, last = (cb == 0), (cb == nb - 1)
                                cb += 1
                                nb_, db_ = 2, 3
                            S.op("pe", lambda e, j=j, nb_=nb_, first=first, last=last, STw=STw: e.matmul(PS[nb_], lhsT=vtok[:, j, :], rhs=STw, start=first, stop=last),
                                 reads=["vtok", STwk], writes=[PB(nb_)])
                            S.op("pe", lambda e, db_=db_, first=first, last=last, STw=STw: e.matmul(PS[db_], lhsT=onesb, rhs=STw, start=first, stop=last),
                                 reads=[STwk], writes=[PB(db_)])
                    (fa, fak), (fb_, fbk), (fc_, fck), (fd_, fdk) = fR.next(), fR.next(), fR.next(), fR.next()
                    for (nb_, db_, dst, dk) in ((0, 1, fa, fak), (2, 3, fb_, fbk)):
                        S.op("act", lambda e, db_=db_, fd_=fd_: e.activation(out=fd_, in_=PS[db_], func=AF.Copy), reads=[PB(db_)], writes=[fdk])
                        S.op("dve", lambda e, fc_=fc_, fd_=fd_: e.scalar_tensor_tensor(out=fc_, in0=fd_, scalar=-1.0, in1=fd_, op0=ALU.mult, op1=ALU.max),
                             reads=[fdk], writes=[fck])
                        S.op("dve", lambda e, fc_=fc_: e.tensor_scalar(out=fc_, in0=fc_, scalar1=1.0, scalar2=None, op0=ALU.max), reads=[fck], writes=[fck])
                        S.op("dve", lambda e, fc_=fc_: e.reciprocal(out=fc_, in_=fc_), reads=[fck], writes=[fck])
                        S.op("dve", lambda e, nb_=nb_, dst=dst, fc_=fc_: e.tensor_tensor(out=dst, in0=PS[nb_], in1=fc_, op=ALU.mult), reads=[PB(nb_), fck], writes=[dk])
                    S.op("pool", lambda e, fa=fa, fb_=fb_: e.tensor_tensor(out=fa, in0=fa, in1=fb_, op=ALU.add), reads=[fak, fbk], writes=[fak])
                    S.op("act", lambda e, fa=fa, fd_=fd_: e.activation(out=fd_, in_=fa, func=AF.Square), reads=[fak], writes=[fdk])
                    pb3, pb3k = stR.next()
                    S.op("pe", lambda e, fd_=fd_, pb3=pb3: e.matmul(pb3, lhsT=ones128, rhs=fd_, start=True, stop=True), reads=[fdk], writes=[pb3k])
                    S.op("act", lambda e, fd_=fd_, pb3=pb3: e.activation(out=fd_, in_=pb3, func=AF.Ln, bias=EPS), reads=[pb3k], writes=[fdk])
                    S.op("act", lambda e, fd_=fd_: e.activation(out=fd_, in_=fd_, func=AF.Exp, scale=-0.5), reads=[fdk], writes=[fdk])
                    S.op("dve", lambda e, h=h, fa=fa, fd_=fd_: e.scalar_tensor_tensor(out=fa, in0=fa, scalar=mng[:, h:h + 1], in1=fd_, op0=ALU.mult, op1=ALU.mult),
                         reads=[fak, fdk], writes=[fak])
                    S.op("dve", lambda e, h=h, bs=bs, fa=fa: e.scalar_tensor_tensor(out=fa, in0=xc[:, bs], scalar=msk[:, h:h + 1], in1=fa, op0=ALU.mult, op1=ALU.add),
                         reads=[fak, "xc"], writes=[fak])
                    S.op("dve", lambda e, h=h, bs=bs, fa=fa: e.tensor_tensor(out=catT[:, h, bs], in0=fa, in1=sigo[:, bs], op=ALU.mult),
                         reads=[fak, "sigo"], writes=[("catT", h)])
            S.barrier()
            A.release(m_mark)

            a_mark = A.mark()
            wq3 = A.alloc("wq3", [128, 8, 128], BF16)
            wk3 = A.alloc("wk3", [128, 8, 128], BF16)
            wv3 = A.alloc("wv3", [128, 8, 128], BF16)
            a32R = Ring("a32", 3, [128, 512], F32)
            tqR = Ring("tq", 3, [128, 512], F32)
            prR = Ring("prR", 0, banks=[0, 1])
            nrR = Ring("nrR", 0, banks=[2, 3])
            qn = A.alloc("qn", [128, S_LEN], BF16)
            kn = A.alloc("kn", [128, S_LEN], BF16)
            av = A.alloc("av", [128, S_LEN], BF16)
            qd = A.alloc("qd", [128, S_LEN], BF16)
            kd = A.alloc("kd", [128, S_LEN], BF16)
            avd = A.alloc("avd", [128, S_LEN], BF16)
            VpA = A.alloc("VpA", [128, NT, 128], BF16)
            VpB = A.alloc("VpB", [128, NT, 128], BF16)
            tabs = A.alloc("tabs", [128, 3, 2, 256], BF16)
            recb = A.alloc("recb", [128, 512], F32)
            S.op("pool", lambda e: e.memset(VpA, 1.0), writes=["VpA"])
            S.op("pool", lambda e: e.memset(VpB, 1.0), writes=["VpB"])
            tabsR = A.alloc("tabsR", [128, 3, 2, 256], F32)
            accN = A.alloc("accN", [128, S_LEN], F32)
            accD = A.alloc("accD", [128, S_LEN], F32)
            etR = Ring("Et", 4, [128, 2, 256], BF16)
            ptR = Ring("Pt", 4, [128, 2, 256], BF16)
            stA_items = [(PSD[2], 4, 5), (PSD[3], 6, 7)]
            stA_i = [0]
            for c in range(4):
                for (wt, col0, wkey) in ((wq3, 1040, "wq3"), (wk3, 1552, "wk3"), (wv3, 2064, "wv3")):
                    S.dma("pool", lambda e, wt=wt, col0=col0, c=c: e.dma_start(out=wt, in_=win_v[:, :, col0 + c * 128:col0 + (c + 1) * 128]), writes=[wkey])
                for p in range(3):
                    for hh in range(2):
                        hd = 2 * c + hh
                        src = bass.AP(tensor=ftab_d.tensor, offset=(hd * 3 + p) * 384, ap=[[1, 128], [1, 256]])
                        S.dma("sp", lambda e, p=p, hh=hh, src=src: e.dma_start(out=tabsR[:, p, hh, :], in_=src), reads=["ftab_d"], writes=[("tabsR", p, hh)])
                    for hh in range(2):
                        S.op("pe", lambda e, p=p, hh=hh: e.matmul(PS[0][:, hh * 256:(hh + 1) * 256], lhsT=jrev, rhs=tabsR[:, p, hh, :], start=True, stop=True),
                             reads=[("tabsR", p, hh)], writes=[PB(0)])
                    S.op("dve", lambda e, p=p: e.tensor_copy(out=tabs[:, p, :, :], in_=PS[0].rearrange("p (a b) -> p a b", a=2)), reads=[PB(0)], writes=["tabs"])
                pjobs = []
                for (wt, wkey, dst, dkey, gvec) in ((wq3, "wq3", qn, "qn", gq), (wk3, "wk3", kn, "kn", gk)):
                    for blk in range(4):
                        pjobs.append((wt, wkey, dst, dkey, gvec, slice(blk * 512, (blk + 1) * 512)))
                pst = {}

                def emit_P(ji):
                    wt, wkey, dst, dkey, gvec, bs = pjobs[ji]
                    pb, pbk = prR.next()
                    a32_, a32k = a32R.next()
                    tq_, tqk = tqR.next()
                    for k in range(8):
                        S.op("pe", lambda e, k=k, bs=bs, wt=wt, pb=pb: e.matmul(pb, lhsT=wt[:, k, :], rhs=hT[:, k, bs], start=(k == 0), stop=(k == 7)),
                             reads=[wkey] + hT_keys, writes=[pbk])
                    S.op("act", lambda e, pb=pb, a32_=a32_: e.activation(out=a32_, in_=pb, func=AF.Copy), reads=[pbk], writes=[a32k])
                    S.op("act", lambda e, pb=pb, tq_=tq_: e.activation(out=tq_, in_=pb, func=AF.Square), reads=[pbk], writes=[tqk])
                    pst[ji] = (a32_, a32k, tq_, tqk)

                def emit_N(ji):
                    wt, wkey, dst, dkey, gvec, bs = pjobs[ji]
                    a32_, a32k, tq_, tqk = pst[ji]
                    nbk, nbkk = nrR.next()
                    S.op("pe", lambda e, tq_=tq_, nbk=nbk: e.matmul(nbk, lhsT=blk64, rhs=tq_, start=True, stop=True), reads=[tqk], writes=[nbkk])
                    S.op("act", lambda e, tq_=tq_, nbk=nbk: e.activation(out=tq_, in_=nbk, func=AF.Ln, bias=EPS), reads=[nbkk], writes=[tqk])
                    S.op("act", lambda e, tq_=tq_: e.activation(out=tq_, in_=tq_, func=AF.Exp, scale=-0.5), reads=[tqk], writes=[tqk])
                    S.op("dve", lambda e, bs=bs, dst=dst, gvec=gvec, a32_=a32_, tq_=tq_: e.scalar_tensor_tensor(out=dst[:, bs], in0=a32_, scalar=gvec[:, 0:1], in1=tq_,
                                                                                                           op0=ALU.mult, op1=ALU.mult),
                         reads=[a32k, tqk], writes=[dkey])
                emit_P(0)
                for ji in range(len(pjobs)):
                    if ji + 1 < len(pjobs):
                        emit_P(ji + 1)
                    emit_N(ji)
                for blk in range(4):
                    bs = slice(blk * 512, (blk + 1) * 512)
                    pb, pbk = prR.next()
                    for k in range(8):
                        S.op("pe", lambda e, k=k, bs=bs, pb=pb: e.matmul(pb, lhsT=wv3[:, k, :], rhs=hT[:, k, bs], start=(k == 0), stop=(k == 7)),
                             reads=["wv3"] + hT_keys, writes=[pbk])
                    S.op("act", lambda e, bs=bs, pb=pb: e.activation(out=av[:, bs], in_=pb, func=AF.Copy), reads=[pbk], writes=["av"])
                for p, d in enumerate((1, 4, 16)):
                    L = S_LEN // d
                    if d == 1:
                        qv, kv, vv = qn, kn, av
                        qk_, kk_, vk_ = "qn", "kn", "av"
                    else:
                        S.op("pool", lambda e, d=d: e.tensor_copy(out=qd.rearrange("p (d l) -> p d l", d=d), in_=qn.rearrange("p (l d) -> p d l", d=d)),
                             reads=["qn"], writes=["qd"])
                        S.op("pool", lambda e, d=d: e.tensor_copy(out=kd.rearrange("p (d l) -> p d l", d=d), in_=kn.rearrange("p (l d) -> p d l", d=d)),
                             reads=["kn"], writes=["kd"])
                        S.op("pool", lambda e, d=d: e.tensor_copy(out=avd.rearrange("p (d l) -> p d l", d=d), in_=av.rearrange("p (l d) -> p d l", d=d)),
                             reads=["av"], writes=["avd"])
                        qv, kv, vv = qd, kd, avd
                        qk_, kk_, vk_ = "qd", "kd", "avd"
                    for i4 in range(4):
                        for ii in range(4):
                            i = i4 * 4 + ii
                            S.op("pe", lambda e, i=i, ii=ii, vv=vv: e.matmul(PS[0][:, ii * 128:(ii + 1) * 128], lhsT=vv[:, i * 128:(i + 1) * 128], rhs=identb,
                                                                               start=True, stop=True),
                                 reads=[vk_], writes=[PB(0)])
                        S.op("act", lambda e, i4=i4: e.activation(out=VpA[:, i4 * 4:(i4 + 1) * 4, 0:64], in_=PS[0].rearrange("p (a b) -> p a b", a=4)[:, :, 0:64], func=AF.Copy),
                             reads=[PB(0)], writes=["VpA"])
                        S.op("dve", lambda e, i4=i4: e.tensor_copy(out=VpB[:, i4 * 4:(i4 + 1) * 4, 64:128], in_=PS[0].rearrange("p (a b) -> p a b", a=4)[:, :, 64:128]),
                             reads=[PB(0), "VpA"], writes=["VpB"])
                    nkt = L // 128
                    for qb in range(4):
                        S.op("dve", lambda e: e.memset(PS[2], 0.0), writes=[PB(2)])
                        S.op("dve", lambda e: e.memset(PS[3], 0.0), writes=[PB(3)])
                        if L >= 512:
                            phases = [(qb * 512) // L]
                        else:
                            phases = list(range((qb * 512) // L, (qb * 512 + 512) // L))
                        tiles = []
                        for r in phases:
                            base = r * L
                            blo = max(qb * 512, base) - base
                            bhi = min(qb * 512 + 512, base + L) - base
                            for n in range(nkt):
                                qlo = max(blo, 128 * n - 64)
                                qhi = min(bhi, 128 * n + 192)
                                if qhi <= qlo:
                                    continue
                                nq = qhi - qlo
                                toff = qlo - (128 * n - 64)
                                gk0 = base + 128 * n
                                gq0 = base + qlo
                                col0 = gq0 - qb * 512
                                tiles.append((nq, toff, gk0, gq0, col0))
                        stt = {}

                        def emit_qk(t, tiles=tiles, stt=stt, kv=kv, qv=qv, kk_=kk_, qk_=qk_):
                            nq, toff, gk0, gq0, col0 = tiles[t]
                            std, bka, bkb = stA_items[stA_i[0] % 2]
                            stA_i[0] += 1
                            st3 = std.rearrange("p (a b) -> p a b", a=2)
                            for hh in range(2):
                                ps_ = slice(64 * hh, 64 * hh + 64)
                                S.op("pe", lambda e, ps_=ps_, hh=hh, gk0=gk0, gq0=gq0, nq=nq, st3=st3: e.matmul(
                                    st3[:, hh, 0:nq], lhsT=kv[ps_, gk0:gk0 + 128], rhs=qv[ps_, gq0:gq0 + nq], start=True, stop=True),
                                    reads=[kk_, qk_], writes=[PB(bka if hh == 0 else bkb)])
                            stt[t] = (st3, bka, bkb)
                        if tiles:
                            emit_qk(0)
                        for t in range(len(tiles)):
                            if t + 1 < len(tiles):
                                emit_qk(t + 1)
                            nq, toff, gk0, gq0, col0 = tiles[t]
                            st3, bka, bkb = stt[t]
                            Et, Etk = etR.next()
                            Pt, Ptk = ptR.next()
                            S.op("act", lambda e, st3=st3, nq=nq, Et=Et: e.activation(out=Et[:, :, 0:nq], in_=st3[:, :, 0:nq], func=AF.Exp, scale=0.125),
                                 reads=[PB(bka), PB(bkb)], writes=[Etk])
                            S.op("dve", lambda e, nq=nq, p=p, toff=toff, Et=Et, Pt=Pt: e.tensor_tensor(out=Pt[:, :, 0:nq], in0=Et[:, :, 0:nq],
                                                                                                       in1=tabs[:, p, :, toff:toff + nq], op=ALU.mult),
                                 reads=[Etk, "tabs"], writes=[Ptk])
                            ti = gk0 // 128
                            S.op("pe", lambda e, ti=ti, col0=col0, nq=nq, Pt=Pt: e.matmul(PS[2][:, col0:col0 + nq], lhsT=VpA[:, ti, :], rhs=Pt[:, 0, 0:nq],
                                                                                         start=False, stop=False, skip_group_check=True),
                                 reads=["VpA", Ptk], writes=[PB(2)])
                            S.op("pe", lambda e, ti=ti, col0=col0, nq=nq, Pt=Pt: e.matmul(PS[3][:, col0:col0 + nq], lhsT=VpB[:, ti, :], rhs=Pt[:, 1, 0:nq],
                                                                                         start=False, stop=False, skip_group_check=True),
                                 reads=["VpB", Ptk], writes=[PB(3)])
                        if d == 1:
                            S.op("act", lambda e, qb=qb: e.activation(out=accN[:, qb * 512:(qb + 1) * 512], in_=PS[2], func=AF.Copy), reads=[PB(2)], writes=["accN"])
                            S.op("dve", lambda e, qb=qb: e.tensor_copy(out=accD[:, qb * 512:(qb + 1) * 512], in_=PS[3]), reads=[PB(3)], writes=["accD"])
                        else:
                            npb = 512 // L if L < 512 else 1
                            r0 = (qb * 512) // L
                            for (acc, ak, bank) in ((accN, "accN", 2), (accD, "accD", 3)):
                                if npb == 1:
                                    view = acc.rearrange("p (l d) -> p d l", d=d)[:, r0, :]
                                    pin = PS[bank]
                                else:
                                    view = acc.rearrange("p (l d) -> p d l", d=d)[:, r0:r0 + npb, :]
                                    pin = PS[bank].rearrange("p (a b) -> p a b", a=npb)
                                S.op("dve", lambda e, view=view, pin=pin: e.tensor_tensor(out=view, in0=pin, in1=view, op=ALU.add), reads=[PB(bank), ak], writes=[ak])
                if b == 0 and c == 0:
                    dump("tabs", tabs, "tabs")
                    dump("accN", accN, "accN")
                    dump("accD", accD, "accD")
                    if "qn" in dbg:
                        S.dma("pool", lambda e: e.dma_start(out=dbg["qn"], in_=qn), reads=["qn"], writes=["dbg_qn"])
                        S.dma("pool", lambda e: e.dma_start(out=dbg["kn"], in_=kn), reads=["kn"], writes=["dbg_kn"])
                        S.dma("pool", lambda e: e.dma_start(out=dbg["av"], in_=av), reads=["av"], writes=["dbg_av"])
                for blk in range(4):
                    bs = slice(blk * 512, (blk + 1) * 512)
                    S.op("pe", lambda e, bs=bs: e.matmul(PS[0], lhsT=selab[:, 0, :], rhs=accN[:, bs], start=True, stop=False), reads=["accN"], writes=[PB(0)])
                    S.op("pe", lambda e, bs=bs: e.matmul(PS[0], lhsT=selab[:, 1, :], rhs=accD[:, bs], start=False, stop=True), reads=["accD"], writes=[PB(0)])
                    S.op("dve", lambda e: e.reciprocal(out=recb, in_=PS[0]), reads=[PB(0)], writes=["recb"])
                    S.op("dve", lambda e, c=c, bs=bs: e.tensor_tensor(out=catT[0:64, 4 + c, bs], in0=accN[0:64, bs], in1=recb[0:64, :], op=ALU.mult),
                         reads=["accN", "recb"], writes=[("catT", 4 + c)])
                    S.op("dve", lambda e, c=c, bs=bs: e.tensor_tensor(out=catT[64:128, 4 + c, bs], in0=accD[64:128, bs], in1=recb[64:128, :], op=ALU.mult),
                         reads=["accD", "recb"], writes=[("catT", 4 + c)])
            if b == 0 and "catT" in dbg:
                S.dma("pool", lambda e: e.dma_start(out=dbg["catT"], in_=catT), reads=[("catT", k) for k in range(8)], writes=["dbg_catT"])
            if max_phase < 5:
                S.barrier()
                continue
            S.barrier()
            A.release(a_mark)
            A.release(pers_mark)

            h2tok = A.alloc("h2tok", [128, NT, D], BF16)
            afft = A.alloc("afft", [128, NT, 16], F32)
            slott = A.alloc("slott", [128, NT, 16], F32)
            slotv = A.alloc("slotv", [16, S_LEN], F32)
            f_mark = A.mark()
            wo = A.alloc("wo", [128, 8, D], BF16)
            h2 = A.alloc("h2", [128, D], F32)
            h2T = A.alloc("h2T", [128, 8, 128], F32)
            g1bc = A.alloc("g1bc", [128, D], F32)
            a2bc = A.alloc("a2bc", [128, D], F32)
            b2bc = A.alloc("b2bc", [128, D], F32)
            affT = A.alloc("affT", [16, S_LEN], F32)
            work = A.alloc("work", [16, S_LEN], F32)
            mx8 = A.alloc("mx8", [16, 8], F32)
            onesr = A.alloc("onesr2", [16, S_LEN], F32)
            for k in range(8):
                S.dma("pool", lambda e, k=k: e.dma_start(out=wo[:, k, :], in_=wout_v[:, k, :]), writes=[("wo", k)])
            wo_keys = [("wo", k) for k in range(8)]
            S.dma("sp", lambda e: e.dma_start(out=g1bc, in_=modrow_d[b:b + 1, 0:1024].partition_broadcast(128)), reads=["modrow_d"], writes=["g1bc"])
            S.dma("sp", lambda e: e.dma_start(out=b2bc, in_=modrow_d[b:b + 1, 1024:2048].partition_broadcast(128)), reads=["modrow_d"], writes=["b2bc"])
            S.dma("sp", lambda e: e.dma_start(out=a2bc, in_=modrow_d[b:b + 1, 2048:3072].partition_broadcast(128)), reads=["modrow_d"], writes=["a2bc"])
            S.dma("sp", lambda e: e.dma_start(out=h2, in_=g2_d.partition_broadcast(128)), writes=["h2"])
            S.op("dve", lambda e: e.scalar_tensor_tensor(out=a2bc, in0=a2bc, scalar=1.0, in1=h2, op0=ALU.add, op1=ALU.mult), reads=["a2bc", "h2"], writes=["a2bc"])
            S.op("pool", lambda e: e.memset(onesr, 1.0), writes=["onesr2"])
            cat_keys = [("catT", k) for k in range(8)]
            xtR = Ring("xtr", 2, [128, D], F32)
            x1R = Ring("x1r", 2, [128, D], F32)
            h2R = Ring("h2r", 3, [128, D], F32)
            stR5 = Ring("st2r", 3, [128, 8], F32)
            lgR = Ring("lgr", 2, [128, 16], F32)
            opb = [(0, 1), (6, 7)]
            stA5 = {}

            def emit_A(i):
                ts_ = slice(i * 128, (i + 1) * 128)
                xt_, xtk = xtR.next()
                x1_, x1k = x1R.next()
                h2_, h2k_ = h2R.next()
                st_, stk_ = stR5.next()
                load("sp", xt_, x_d[b, ts_, :], xtk)
                for half in range(2):
                    hs = slice(half * 512, (half + 1) * 512)
                    bank = opb[i % 2][half]
                    for k in range(8):
                        S.op("pe", lambda e, k=k, ts_=ts_, hs=hs, bank=bank: e.matmul(PS[bank], lhsT=catT[:, k, ts_], rhs=wo[:, k, hs], start=(k == 0), stop=(k == 7)),
                             reads=cat_keys + wo_keys, writes=[PB(bank)])
                    S.op("dve", lambda e, hs=hs, bank=bank, x1_=x1_: e.tensor_tensor(out=x1_[:, hs], in0=PS[bank], in1=g1bc[:, hs], op=ALU.mult), reads=[PB(bank), "g1bc"], writes=[x1k])
                S.op("pool", lambda e, x1_=x1_, xt_=xt_: e.tensor_tensor(out=x1_, in0=x1_, in1=xt_, op=ALU.add), reads=[x1k, xtk], writes=[x1k])
                S.dma("sp", lambda e, ts_=ts_, x1_=x1_: e.dma_start(out=out_d[b, ts_, :], in_=x1_), reads=[x1k], writes=[("outd", b, i)])
                S.op("act", lambda e, x1_=x1_, h2_=h2_, st_=st_: e.activation(out=h2_, in_=x1_, func=AF.Square, accum_out=st_[:, 0:1]), reads=[x1k], writes=[h2k_, stk_])
                S.op("act", lambda e, st_=st_: e.activation(out=st_[:, 1:2], in_=st_[:, 0:1], func=AF.Ln, scale=1.0 / D, bias=EPS), reads=[stk_], writes=[stk_])
                S.op("act", lambda e, st_=st_: e.activation(out=st_[:, 2:3], in_=st_[:, 1:2], func=AF.Exp, scale=-0.5), reads=[stk_], writes=[stk_])
                S.op("dve", lambda e, x1_=x1_, h2_=h2_, st_=st_: e.scalar_tensor_tensor(out=h2_, in0=x1_, scalar=st_[:, 2:3], in1=a2bc, op0=ALU.mult, op1=ALU.mult),
                     reads=[x1k, stk_, "a2bc"], writes=[h2k_])
                S.op("pool", lambda e, h2_=h2_: e.tensor_tensor(out=h2_, in0=h2_, in1=b2bc, op=ALU.add), reads=[h2k_, "b2bc"], writes=[h2k_])
                S.op("act", lambda e, i=i, h2_=h2_: e.activation(out=h2tok[:, i, :], in_=h2_, func=AF.Copy), reads=[h2k_], writes=[("h2tok", i)])
                stA5[i] = (h2_, h2k_, st_, stk_)

            def emit_B(i):
                ts_ = slice(i * 128, (i + 1) * 128)
                h2_, h2k_, st_, stk_ = stA5[i]
                lg_, lgk = lgR.next()
                for k in range(8):
                    bank = 2 + k // 4
                    S.op("pe", lambda e, k=k, bank=bank, h2_=h2_: e.matmul(PS[bank][:, (k % 4) * 128:(k % 4 + 1) * 128], lhsT=h2_[:, k * 128:(k + 1) * 128], rhs=ident,
                                                                            start=True, stop=True), reads=[h2k_], writes=[PB(bank)])
                S.op("dve", lambda e: e.tensor_copy(out=h2T[:, 0:4, :], in_=PS[2].rearrange("p (a b) -> p a b", a=4)), reads=[PB(2)], writes=["h2T"])
                S.op("act", lambda e: e.activation(out=h2T[:, 4:8, :], in_=PS[3].rearrange("p (a b) -> p a b", a=4), func=AF.Copy), reads=[PB(3)], writes=["h2T"])
                for k in range(8):
                    S.op("pe", lambda e, k=k: e.matmul(PS[4][:, 0:16], lhsT=h2T[:, k, :], rhs=wr[:, k, :], start=(k == 0), stop=(k == 7)), reads=["h2T"], writes=[PB(4)])
                S.op("dve", lambda e, lg_=lg_: e.tensor_tensor(out=lg_, in0=PS[4][:, 0:16], in1=brbc, op=ALU.add), reads=[PB(4)], writes=[lgk])
                S.op("dve", lambda e, lg_=lg_, st_=st_: e.tensor_reduce(out=st_[:, 3:4], in_=lg_, axis=AX.X, op=ALU.max), reads=[lgk], writes=[stk_])
                S.op("dve", lambda e, st_=st_: e.tensor_scalar(out=st_[:, 4:5], in0=st_[:, 3:4], scalar1=-1.0, scalar2=None, op0=ALU.mult), reads=[stk_], writes=[stk_])
                S.op("act", lambda e, lg_=lg_, st_=st_: e.activation(out=lg_, in_=lg_, func=AF.Exp, bias=st_[:, 4:5], accum_out=st_[:, 5:6]), reads=[lgk, stk_], writes=[lgk, stk_])
                S.op("dve", lambda e, st_=st_: e.reciprocal(out=st_[:, 6:7], in_=st_[:, 5:6]), reads=[stk_], writes=[stk_])
                S.op("dve", lambda e, i=i, lg_=lg_, st_=st_: e.tensor_scalar(out=afft[:, i, :], in0=lg_, scalar1=st_[:, 6:7], scalar2=None, op0=ALU.mult), reads=[lgk, stk_], writes=[("afft", i)])
                S.op("pe", lambda e, i=i: e.matmul(PS[5][0:16, 0:128], lhsT=afft[:, i, :], rhs=ident, start=True, stop=True), reads=[("afft", i)], writes=[PB(5)])
                S.op("act", lambda e, ts_=ts_: e.activation(out=affT[:, ts_], in_=PS[5][0:16, 0:128], func=AF.Copy), reads=[PB(5)], writes=["affT"])

            emit_A(0)
            for i in range(NT):
                if i + 1 < NT:
                    emit_A(i + 1)
                emit_B(i)
            S.op("dve", lambda e: e.tensor_copy(out=work, in_=affT), reads=["affT"], writes=["work"])
            for rnd in range(CAP // 8):
                S.op("dve", lambda e: e.max(out=mx8, in_=work), reads=["work"], writes=["mx8"])
                S.op("dve", lambda e: e.match_replace(out=work, in_to_replace=mx8, in_values=work, imm_value=0.0), reads=["work", "mx8"], writes=["work"])
            S.op("dve", lambda e: e.tensor_tensor(out=work, in0=affT, in1=work, op=ALU.subtract), reads=["affT", "work"], writes=["work"])
            S.op("dve", lambda e: e.tensor_scalar(out=work, in0=work, scalar1=0.0, scalar2=None, op0=ALU.is_gt), reads=["work"], writes=["work"])
            S.op("dve", lambda e: e.tensor_tensor_scan(out=slotv, data0=onesr, data1=work, initial=0.0, op0=ALU.mult, op1=ALU.add), reads=["onesr2", "work"], writes=["slotv"])
            S.op("dve", lambda e: e.tensor_tensor(out=slotv, in0=slotv, in1=work, op=ALU.mult), reads=["slotv", "work"], writes=["slotv"])
            S.op("dve", lambda e: e.tensor_scalar(out=slotv, in0=slotv, scalar1=-1.0, scalar2=None, op0=ALU.add), reads=["slotv"], writes=["slotv"])
            for i in range(NT):
                S.op("pe", lambda e, i=i: e.matmul(PS[6][:, i * 16:(i + 1) * 16], lhsT=slotv[:, i * 128:(i + 1) * 128], rhs=ident[0:16, 0:16], start=True, stop=True),
                     reads=["slotv"], writes=[PB(6)])
            S.op("dve", lambda e: e.tensor_copy(out=slott, in_=PS[6][:, 0:256].rearrange("p (a b) -> p a b", a=NT)), reads=[PB(6)], writes=["slott"])
            if b == 0:
                dump("slotv", slotv, "slotv")
                dump("affT", affT, "affT")
                if "h2tok" in dbg:
                    S.dma("pool", lambda e: e.dma_start(out=dbg["h2tok"], in_=h2tok), reads=[("h2tok", i) for i in range(NT)], writes=["dbg_h2tok"])
            if max_phase < 6:
                S.barrier()
                continue
            S.barrier()
            A.release(f_mark)

            A.limit = ARENA_BYTES
            yacc = A.alloc("yacc", [128, NT, D], F32)
            y_mark = A.mark()
            Pm = A.alloc("Pm", [128, NT, CAP], BF16)
            PTm = A.alloc("PTm", [128, 2, S_LEN], BF16)
            xin = A.alloc("xin", [128, 8, CAP], BF16)
            wR = Ring("wbuf", 4, [128, 4096], BF16)
            hid = A.alloc("hid", [128, 16, CAP], BF16)
            yex = A.alloc("yex", [128, 2, D], BF16)
            sgR = Ring("sg", 2, [128, CAP], F32)
            fR2 = Ring("ffps", 0, banks=[0, 1, 2, 3])
            h2k = [("h2tok", i) for i in range(NT)]
            for ex in range(NEXP):
                for i in range(NT):
                    S.op("dve", lambda e, i=i, ex=ex: e.tensor_scalar(out=Pm[:, i, :], in0=iotaf, scalar1=slott[:, i, ex:ex + 1], scalar2=None, op0=ALU.is_equal),
                         reads=["slott"], writes=["Pm"])
                for blk in range(4):
                    bs = slice(blk * 512, (blk + 1) * 512)
                    pb, pbk = fR2.next()
                    S.op("pe", lambda e, ex=ex, bs=bs, pb=pb: e.matmul(pb, lhsT=sele[:, ex, :], rhs=slotv[:, bs], start=True, stop=True), reads=["slotv"], writes=[pbk])
                    for ch in range(2):
                        S.op("dve", lambda e, ch=ch, bs=bs, pb=pb: e.tensor_scalar(out=PTm[:, ch, bs], in0=pb, scalar1=iotap[:, ch:ch + 1], scalar2=None, op0=ALU.is_equal),
                             reads=[pbk], writes=["PTm"])
                for k2 in range(4):
                    pb, pbk = fR2.next()
                    for kk in range(2):
                        k = k2 * 2 + kk
                        for i in range(NT):
                            S.op("pe", lambda e, k=k, kk=kk, i=i, pb=pb: e.matmul(pb[:, kk * 256:(kk + 1) * 256], lhsT=h2tok[:, i, k * 128:(k + 1) * 128], rhs=Pm[:, i, :],
                                                                                  start=(i == 0), stop=(i == NT - 1)),
                                 reads=h2k + ["Pm"], writes=[pbk])
                    S.op("act", lambda e, k2=k2, pb=pb: e.activation(out=xin[:, 2 * k2:2 * k2 + 2, :], in_=pb.rearrange("p (a b) -> p a b", a=2), func=AF.Copy),
                         reads=[pbk], writes=["xin"])
                for fb in range(4):
                    wg, wgk = wR.next()
                    wg = wg.rearrange("p (k f) -> p k f", k=8)
                    S.dma("pool", lambda e, ex=ex, fb=fb, wg=wg: e.dma_start(out=wg, in_=wg_d[ex].rearrange("(k p) f -> p k f", p=128)[:, :, fb * 512:(fb + 1) * 512]), writes=[wgk])
                    wu, wuk = wR.next()
                    wu = wu.rearrange("p (k f) -> p k f", k=8)
                    S.dma("pool", lambda e, ex=ex, fb=fb, wu=wu: e.dma_start(out=wu, in_=wu_d[ex].rearrange("(k p) f -> p k f", p=128)[:, :, fb * 512:(fb + 1) * 512]), writes=[wuk])
                    for fc in range(4):
                        f = fb * 4 + fc
                        pb, pbk = fR2.next()
                        for k in range(8):
                            S.op("pe", lambda e, k=k, fc=fc, pb=pb, wg=wg: e.matmul(pb[:, 0:CAP], lhsT=wg[:, k, fc * 128:(fc + 1) * 128], rhs=xin[:, k, :], start=(k == 0), stop=(k == 7)),
                                 reads=[wgk, "xin"], writes=[pbk])
                        for k in range(8):
                            S.op("pe", lambda e, k=k, fc=fc, pb=pb, wu=wu: e.matmul(pb[:, CAP:2 * CAP], lhsT=wu[:, k, fc * 128:(fc + 1) * 128], rhs=xin[:, k, :], start=(k == 0), stop=(k == 7)),
                                 reads=[wuk, "xin"], writes=[pbk])
                        sg, sgk = sgR.next()
                        S.op("act", lambda e, pb=pb, sg=sg: e.activation(out=sg, in_=pb[:, 0:CAP], func=AF.Silu), reads=[pbk], writes=[sgk])
                        S.op("dve", lambda e, f=f, pb=pb, sg=sg: e.tensor_tensor(out=hid[:, f, :], in0=pb[:, CAP:2 * CAP], in1=sg, op=ALU.mult), reads=[pbk, sgk], writes=["hid"])
                for fb in range(4):
                    wd, wdk = wR.next()
                    wd = wd.rearrange("p (k n) -> p k n", k=4)
                    S.dma("pool", lambda e, ex=ex, fb=fb, wd=wd: e.dma_start(out=wd, in_=wd_d[ex].rearrange("(k p) n -> p k n", p=128)[:, fb * 4:(fb + 1) * 4, :]), writes=[wdk])
                    for fc in range(4):
                        f = fb * 4 + fc
                        for ct in range(2):
                            for dh in range(2):
                                bank = 4 + ct * 2 + dh
                                S.op("pe", lambda e, f=f, fc=fc, ct=ct, dh=dh, bank=bank, wd=wd: e.matmul(PS[bank], lhsT=hid[:, f, ct * 128:(ct + 1) * 128],
                                                                                                          rhs=wd[:, fc, dh * 512:(dh + 1) * 512], start=(f == 0), stop=(f == 15)),
                                     reads=["hid", wdk], writes=[PB(bank)])
                for ct in range(2):
                    for dh in range(2):
                        bank = 4 + ct * 2 + dh
                        if dh == 0:
                            S.op("act", lambda e, ct=ct, dh=dh, bank=bank: e.activation(out=yex[:, ct, dh * 512:(dh + 1) * 512], in_=PS[bank], func=AF.Copy),
                                 reads=[PB(bank)], writes=["yex"])
                        else:
                            S.op("dve", lambda e, ct=ct, dh=dh, bank=bank: e.tensor_copy(out=yex[:, ct, dh * 512:(dh + 1) * 512], in_=PS[bank]), reads=[PB(bank)], writes=["yex"])
                for i in range(NT):
                    for dh in range(2):
                        pb, pbk = fR2.next()
                        for ch in range(2):
                            S.op("pe", lambda e, i=i, dh=dh, ch=ch, pb=pb: e.matmul(pb, lhsT=PTm[:, ch, i * 128:(i + 1) * 128], rhs=yex[:, ch, dh * 512:(dh + 1) * 512],
                                                                                    start=(ch == 0), stop=(ch == 1)),
                                 reads=["PTm", "yex"], writes=[pbk])
                        if ex == 0:
                            S.op("dve", lambda e, i=i, dh=dh, pb=pb, ex=ex: e.tensor_scalar(out=yacc[:, i, dh * 512:(dh + 1) * 512], in0=pb, scalar1=afft[:, i, ex:ex + 1],
                                                                                            scalar2=None, op0=ALU.mult),
                                 reads=[pbk], writes=[("yacc", i, dh)])
                        else:
                            S.op("dve", lambda e, i=i, dh=dh, pb=pb, ex=ex: e.scalar_tensor_tensor(out=yacc[:, i, dh * 512:(dh + 1) * 512], in0=pb, scalar=afft[:, i, ex:ex + 1],
                                                                                                   in1=yacc[:, i, dh * 512:(dh + 1) * 512], op0=ALU.mult, op1=ALU.add),
                                 reads=[pbk, ("yacc", i, dh)], writes=[("yacc", i, dh)])
            S.barrier()
            A.release(y_mark)
            g2bc = A.alloc("g2bc", [128, D], F32)
            xt = A.alloc("xt", [128, D], F32)
            ot = A.alloc("ot", [128, D], F32)
            S.dma("sp", lambda e: e.dma_start(out=g2bc, in_=modrow_d[b:b + 1, 3072:4096].partition_broadcast(128)), reads=["modrow_d"], writes=["g2bc"])
            for i in range(NT):
                ts_ = slice(i * 128, (i + 1) * 128)
                S.dma("sp", lambda e, ts_=ts_: e.dma_start(out=xt, in_=out_d[b, ts_, :]), reads=[("outd", b, i)], writes=["xt"])
                S.op("dve", lambda e, i=i: e.tensor_tensor(out=ot, in0=yacc[:, i, :], in1=g2bc, op=ALU.mult), reads=[("yacc", i, 0), ("yacc", i, 1), "g2bc"], writes=["ot"])
                S.op("pool", lambda e: e.tensor_tensor(out=ot, in0=ot, in1=xt, op=ALU.add), reads=["ot", "xt"], writes=["ot"])
                S.dma("sp", lambda e, ts_=ts_: e.dma_start(out=out_d[b, ts_, :], in_=ot), reads=["ot", ("outd", b, i)], writes=[("outd", b, i)])
            S.barrier()

        S.barrier()
        print("arena peak bytes", A.peak, "instr counts", {e: len(v) for e, v in S.prog.items()})
        with nc.Block() as block:
            S.emit(block)
    return nc


def _t5_bucket(rel):
    half, exact = 16, 8
    n = np.abs(rel)
    log_ratio = np.log(np.maximum(n, 1).astype(np.float32) / exact) / math.log(1024 / exact)
    large = np.minimum(exact + (log_ratio * (half - exact)).astype(np.int32), half - 1)
    return np.where(rel > 0, half, 0) + np.where(n < exact, n, large)


def _consts():
    c = {}
    c["c_ident"] = np.eye(128, dtype=np.float32)
    c["c_jrev"] = np.ascontiguousarray(np.eye(128, dtype=np.float32)[::-1])
    selab = np.zeros((128, 2, 128), np.float32)
    for m in range(64):
        selab[m + 64, 0, m] = 1.0
        selab[m, 1, m + 64] = 1.0
    c["c_selab"] = selab
    x = np.arange(896)[None, :] - 384
    kp = np.arange(128)[:, None]
    c["c_maskF"] = np.where(x >= kp, 0.0, NEG).astype(np.float32)
    c["c_maskB"] = np.where(x <= kp, 0.0, NEG).astype(np.float32)
    c["c_iotaf"] = np.broadcast_to(np.arange(256, dtype=np.float32)[None, :], (128, 256)).copy()
    c["c_iotap"] = np.stack([np.arange(128, dtype=np.float32), np.arange(128, dtype=np.float32) + 128], axis=1)
    selfb = np.zeros((16, 8, 128), np.float32)
    for h in range(4):
        selfb[4 + h, h, :] = -1.0
        selfb[12 + h, 4 + h, :] = 1.0
    c["c_selfb"] = selfb
    sele = np.zeros((16, 16, 128), np.float32)
    for e in range(16):
        sele[e, e, :] = 1.0
    c["c_sele"] = sele
    sel2 = np.zeros((2, 2, 128), np.float32)
    sel2[0, 0, :] = 1.0
    sel2[1, 1, :] = 1.0
    c["c_sel2"] = sel2
    comb = np.zeros((16, 3, 8), np.float32)
    for h in range(4):
        comb[h, 0, h] = 1.0
        comb[4 + h, 1, h] = 1.0
        comb[8 + h, 0, 4 + h] = 1.0
        comb[12 + h, 1, 4 + h] = -1.0
        comb[12 + h, 2, 4 + h] = 1.0
    c["c_comb"] = comb
    c["c_ones128"] = np.full((128, 128), 1.0 / 128.0, np.float32)
    blk = np.zeros((128, 128), np.float32)
    blk[0:64, 0:64] = 1.0 / 64.0
    blk[64:128, 64:128] = 1.0 / 64.0
    c["c_blk64"] = blk
    oh = np.zeros((32, 3, 384), np.float32)
    for p, d in enumerate((1, 4, 16)):
        for y in range(0, 129):
            rel = 64 - y
            bkt = int(_t5_bucket(np.array(rel * d)))
            oh[bkt, p, 127 + y] = 1.0
    c["c_onehot"] = oh
    return c


_NC_CACHE = {}


def _blockdiag(wblk):
    out = np.zeros((4, 128, 128), np.float32)
    for h in range(4):
        for g in range(32):
            out[h, 4 * g:4 * g + 4, 4 * g:4 * g + 4] = wblk[32 * h + g]
    return out


def make_in_maps(inputs, n_cores=8):
    f = lambda a: np.ascontiguousarray(np.asarray(a, dtype=np.float32))
    x = f(inputs["x"]); c = f(inputs["c"])
    shared = {}
    shared["w_ada"] = f(inputs["w_ada"][0])
    shared["b_ada_fm"] = f(inputs["b_ada"][0].reshape(48, 128).T)
    shared["b_ada_row"] = f(inputs["b_ada"][0].reshape(1, 6 * D))
    shared["g1_fm"] = f(inputs["norm1_g"][0].reshape(8, 128).T)
    shared["g2_row"] = f(inputs["norm2_g"][0].reshape(1, D))
    shared["w_in"] = f(inputs["w_in"][0])
    shared["convw_fm"] = f(np.transpose(inputs["conv_w"][0].reshape(5, 4, 128), (2, 1, 0)))
    shared["convb_fm"] = f(inputs["conv_b"][0].reshape(4, 128).T)
    shared["mng_fm"] = f(inputs["mlstm_norm_g"][0].reshape(4, 128).T)
    shared["mskip_fm"] = f(inputs["mlstm_skip"][0].reshape(4, 128).T)
    shared["wq_bd"] = _blockdiag(np.asarray(inputs["w_q_blk"][0]))
    shared["wk_bd"] = _blockdiag(np.asarray(inputs["w_k_blk"][0]))
    shared["wv_bd"] = _blockdiag(np.asarray(inputs["w_v_blk"][0]))
    bi = np.asarray(inputs["b_igate"][0]); bf = np.asarray(inputs["b_fgate"][0])
    shared["gate_bias"] = f(np.concatenate([bi[0], bf[0], bi[1], bf[1]]).reshape(16, 1))
    shared["gq"] = f(np.tile(np.asarray(inputs["q_norm_g"][0]), 2).reshape(128, 1))
    shared["gk"] = f(np.tile(np.asarray(inputs["k_norm_g"][0]), 2).reshape(128, 1))
    shared["rel_bias"] = f(inputs["rel_bias"])
    shared["w_out"] = f(inputs["w_out"][0])
    shared["wr_fm"] = f(np.transpose(np.asarray(inputs["w_router"][0]).reshape(8, 128, 16), (1, 0, 2)))
    shared["b_router"] = f(inputs["b_router"][0].reshape(1, 16))
    shared["w_gate"] = f(inputs["w_gate"][0])
    shared["w_up"] = f(inputs["w_up"][0])
    shared["w_down"] = f(inputs["w_down"][0])
    shared.update(_consts())
    maps = []
    for i in range(n_cores):
        m = dict(shared)
        m["x"] = np.ascontiguousarray(x[2 * i:2 * i + 2])
        cc = c[2 * i:2 * i + 2]
        m["cT"] = np.ascontiguousarray(np.transpose(cc.reshape(2, 8, 128), (2, 1, 0)))
        maps.append(m)
    return maps


def kernel(**inputs):
    if "nc" not in _NC_CACHE:
        _NC_CACHE["nc"] = build_program()
    nc = _NC_CACHE["nc"]
    maps = make_in_maps(inputs, 8)
    res = run_bass_kernel_spmd(nc, maps, core_ids=list(range(8)))
    out = np.concatenate([np.asarray(r["out"]) for r in res.results], axis=0)
    return out.astype(np.float32)
```

```python
import math
from contextlib import ExitStack
import numpy as np
import concourse.bass as bass
import concourse.mybir as mybir
from concourse.bass_utils import run_bass_kernel_spmd

F32 = mybir.dt.float32
BF16 = mybir.dt.bfloat16
AF = mybir.ActivationFunctionType
ALU = mybir.AluOpType
AX = mybir.AxisListType

S_LEN = 2048
D = 1024
NT = 16
NEXP = 16
CAP = 256
DFF = 2048
EPS = 1e-6
NEG = -30000.0
SAME_ENGINE_SYNC = True
N_DMA_SEMS = 24


class _Rec:
    def __init__(self):
        self.call = None

    def __getattr__(self, name):
        def f(*a, **k):
            self.call = (name, a, k)
            return self
        return f


def _bind(fn):
    rec = _Rec()
    fn(rec)
    assert rec.call is not None
    return rec.call


class Sched:
    ENGS = ["pe", "act", "dve", "pool", "sp"]

    def __init__(self, sems):
        self.prog = {e: [] for e in self.ENGS}
        self.cnt = {}
        self.res = {}
        self.waited = {e: {} for e in self.ENGS}
        self.sems = sems
        self.eng_sem = {e: "c_" + e for e in self.ENGS}
        for e in self.ENGS:
            self.cnt["c_" + e] = 0
        self.dma_names = ["d%d" % i for i in range(N_DMA_SEMS)]
        for n in self.dma_names:
            self.cnt[n] = 0
        self.dma_rr = 0
        self.dma_rr_pool = 0

    def _deps(self, reads, writes):
        deps = {}

        def add(tok):
            if tok is None:
                return
            s, v = tok
            if deps.get(s, 0) < v:
                deps[s] = v
        for r in reads:
            st = self.res.get(r)
            if st is not None:
                add(st["w"])
        for w in writes:
            st = self.res.get(w)
            if st is not None:
                add(st["w"])
                for s, v in st["r"].items():
                    add((s, v))
        return deps

    def _commit(self, tok, reads, writes):
        s, v = tok
        for r in reads:
            st = self.res.setdefault(r, {"w": None, "r": {}})
            if st["r"].get(s, 0) < v:
                st["r"][s] = v
        for w in writes:
            self.res[w] = {"w": tok, "r": {}}

    def op(self, eng, fn, reads=(), writes=()):
        deps = self._deps(reads, writes)
        own = self.eng_sem[eng]
        waits = []
        for s, v in deps.items():
            if s == own and (eng == "pe" or not SAME_ENGINE_SYNC):
                continue
            if self.waited[eng].get(s, 0) >= v:
                continue
            self.waited[eng][s] = v
            waits.append((s, v))
        self.cnt[own] += 1
        tok = (own, self.cnt[own])
        self.prog[eng].append((_bind(fn), waits, (own, 1)))
        self._commit(tok, reads, writes)

    def dma(self, eng, fn, reads=(), writes=()):
        deps = self._deps(reads, writes)
        half = len(self.dma_names) // 2
        if eng == "pool":
            name = self.dma_names[half + self.dma_rr_pool % half]
            self.dma_rr_pool += 1
        else:
            name = self.dma_names[self.dma_rr % half]
            self.dma_rr += 1
        prev = self.cnt[name]
        if prev > 0 and deps.get(name, 0) < prev:
            deps[name] = prev
        waits = []
        for s, v in deps.items():
            if self.waited[eng].get(s, 0) >= v:
                continue
            self.waited[eng][s] = v
            waits.append((s, v))
        self.cnt[name] += 16
        tok = (name, self.cnt[name])
        self.prog[eng].append((_bind(fn), waits, (name, 16)))
        self._commit(tok, reads, writes)
        return tok

    def barrier(self):
        for e in self.ENGS:
            waits = []
            for s, v in self.cnt.items():
                if v == 0:
                    continue
                if self.waited[e].get(s, 0) >= v:
                    continue
                self.waited[e][s] = v
                waits.append((s, v))
            if waits:
                self.prog[e].append((None, waits, None))

    def emit(self, block):
        sems = self.sems

        def mk(engname):
            def body(e):
                for fn, waits, inc in self.prog[engname]:
                    for s, v in waits:
                        e.wait_ge(sems[s], v)
                    if fn is not None:
                        name, a, k = fn
                        ins = getattr(e, name)(*a, **k)
                        ins.then_inc(sems[inc[0]], inc[1])
            return body
        block.tensor(mk("pe"))
        block.scalar(mk("act"))
        block.vector(mk("dve"))
        block.gpsimd(mk("pool"))
        block.sync(mk("sp"))


class Arena:
    def __init__(self, ar, nbytes):
        self.ar = ar
        self.top = 0
        self.nbytes = nbytes
        self.limit = nbytes
        self.gen = 0
        self.peak = 0

    def alloc(self, name, shape, dt, parts=128):
        esz = 2 if dt == BF16 else 4
        n = 1
        for s in shape[1:]:
            n *= s
        nb = (n * esz + 31) // 32 * 32
        off = self.top
        self.top += nb
        self.peak = max(self.peak, self.top)
        assert self.top <= self.limit, (name, self.top, self.limit)
        v = self.ar[:, off // 4: (off + nb) // 4]
        if dt == BF16:
            v = v.bitcast(BF16)
        v = v[:, 0:n]
        if len(shape) == 3:
            v = v.rearrange("p (a b) -> p a b", a=shape[1])
        elif len(shape) == 4:
            v = v.rearrange("p (a b c) -> p a b c", a=shape[1], b=shape[2])
        if shape[0] < 128:
            v = v[0:shape[0]]
        self.gen += 1
        return v

    def mark(self):
        return self.top

    def release(self, m):
        self.top = m


def build_program(debug=None, nseq=2, max_phase=9):
    nc = bass.Bass("TRN2", target_bir_lowering=False)
    dr = {}

    def din(name, shape, dt=F32):
        dr[name] = nc.dram_tensor(name, list(shape), dt, kind="ExternalInput").ap()
        return dr[name]
    x_d = din("x", [2, S_LEN, D])
    cT_d = din("cT", [128, 8, 2])
    wada_d = din("w_ada", [D, 6 * D])
    bada_fm_d = din("b_ada_fm", [128, 48])
    bada_row_d = din("b_ada_row", [1, 6 * D])
    g1_d = din("g1_fm", [128, 8])
    g2_d = din("g2_row", [1, D])
    win_d = din("w_in", [D, 2576])
    convw_d = din("convw_fm", [128, 4, 5])
    convb_d = din("convb_fm", [128, 4])
    mng_d = din("mng_fm", [128, 4])
    msk_d = din("mskip_fm", [128, 4])
    wq_d = din("wq_bd", [4, 128, 128])
    wk_d = din("wk_bd", [4, 128, 128])
    wv_d = din("wv_bd", [4, 128, 128])
    gb_d = din("gate_bias", [16, 1])
    gq_d = din("gq", [128, 1])
    gk_d = din("gk", [128, 1])
    relb_d = din("rel_bias", [32, 8])
    wout_d = din("w_out", [D, D])
    wr_d = din("wr_fm", [128, 8, 16])
    br_d = din("b_router", [1, 16])
    wg_d = din("w_gate", [NEXP, D, DFF])
    wu_d = din("w_up", [NEXP, D, DFF])
    wd_d = din("w_down", [NEXP, DFF, D])
    ident_d = din("c_ident", [128, 128])
    jrev_d = din("c_jrev", [128, 128])
    selab_d = din("c_selab", [128, 2, 128])
    maskF_d = din("c_maskF", [128, 896])
    maskB_d = din("c_maskB", [128, 896])
    iotaf_d = din("c_iotaf", [128, 256])
    iotap_d = din("c_iotap", [128, 2])
    selfb_d = din("c_selfb", [16, 8, 128])
    sele_d = din("c_sele", [16, 16, 128])
    sel2_d = din("c_sel2", [2, 2, 128])
    comb_d = din("c_comb", [16, 3, 8])
    ones128_d = din("c_ones128", [128, 128])
    blk64_d = din("c_blk64", [128, 128])
    onehot_d = din("c_onehot", [32, 3, 384])
    out_d = nc.dram_tensor("out", [2, S_LEN, D], F32, kind="ExternalOutput").ap()
    modrow_d = nc.dram_tensor("modrow_s", [2, 4 * D], F32, kind="Internal").ap()
    ftab_d = nc.dram_tensor("ftab_s", [8, 3, 384], F32, kind="Internal").ap()
    dbg = {}
    if debug:
        for nm, shp in debug.items():
            dbg[nm] = nc.dram_tensor("dbg_" + nm, list(shp), F32, kind="ExternalOutput").ap()

    ARENA_BYTES = 206 * 1024
    with ExitStack() as es:
        arena_t = es.enter_context(nc.sbuf_tensor("arena", [128, ARENA_BYTES // 4], F32))
        PSD = [es.enter_context(nc.psum_tensor("psd%d" % i, [128, 1024], F32))[:] for i in range(4)]
        PS = []
        for i in range(4):
            PS.append(PSD[i][:, 0:512])
            PS.append(PSD[i][:, 512:1024])
        names = ["c_pe", "c_act", "c_dve", "c_pool", "c_sp"] + ["d%d" % i for i in range(N_DMA_SEMS)]
        sems = {n: es.enter_context(nc.semaphore(n)) for n in names}
        S = Sched(sems)
        A = Arena(arena_t, ARENA_BYTES)
        uid = [0]

        def R(name):
            uid[0] += 1
            return "%s#%d" % (name, uid[0])

        def PB(i):
            return ("ps", i)

        class Ring:
            def __init__(self, name, n, shape=None, dt=None, banks=None):
                self.n = n
                self.i = 0
                if banks is not None:
                    self.items = [(PS[bk], PB(bk)) for bk in banks]
                    self.n = len(banks)
                else:
                    self.items = [(A.alloc(name + str(j), shape, dt), R(name + str(j))) for j in range(n)]

            def next(self):
                it = self.items[self.i % self.n]
                self.i += 1
                return it

        def load(eng, dst, src, key, reads=()):
            S.dma(eng, lambda e: e.dma_start(out=dst, in_=src), reads=list(reads), writes=[key])

        def dump(name, src, key):
            if name in dbg:
                S.dma("sp", lambda e: e.dma_start(out=dbg[name], in_=src), reads=[key], writes=["dbg_" + name])

        ident = A.alloc("ident", [128, 128], F32)
        identb = A.alloc("identb", [128, 128], BF16)
        jrev = A.alloc("jrev", [128, 128], F32)
        selab = A.alloc("selab", [128, 2, 128], F32)
        onesb = A.alloc("onesb", [128, 128], BF16)
        ones128 = A.alloc("ones128", [128, 128], F32)
        blk64 = A.alloc("blk64", [128, 128], F32)
        maskF = A.alloc("maskF", [128, 896], F32)
        maskB = A.alloc("maskB", [128, 896], F32)
        iotaf = A.alloc("iotaf", [128, 256], F32)
        iotap = A.alloc("iotap", [128, 2], F32)
        selfb = A.alloc("selfb", [16, 8, 128], F32)
        sele = A.alloc("sele", [16, 16, 128], F32)
        sel2 = A.alloc("sel2", [2, 2, 128], F32)
        comb = A.alloc("comb", [16, 3, 8], F32)
        g1 = A.alloc("g1", [128, 8], F32)
        convw = A.alloc("convw", [128, 4, 5], F32)
        convb = A.alloc("convb", [128, 4], F32)
        mng = A.alloc("mng", [128, 4], F32)
        msk = A.alloc("msk", [128, 4], F32)
        gbias = A.alloc("gbias", [16, 1], F32)
        gq = A.alloc("gq", [128, 1], F32)
        gk = A.alloc("gk", [128, 1], F32)
        wr = A.alloc("wr", [128, 8, 16], F32)
        brbc = A.alloc("brbc", [128, 16], F32)
        wqb = A.alloc("wqb", [128, 4, 128], BF16)
        wkb = A.alloc("wkb", [128, 4, 128], BF16)
        wvb = A.alloc("wvb", [128, 4, 128], BF16)
        A1 = A.alloc("A1", [128, 2, 8], F32)
        B1 = A.alloc("B1", [128, 2, 8], F32)
        CONST = "const"
        for dst, src in [(ident, ident_d), (jrev, jrev_d), (selab, selab_d), (ones128, ones128_d), (blk64, blk64_d), (maskF, maskF_d),
                         (maskB, maskB_d), (iotaf, iotaf_d), (iotap, iotap_d), (selfb, selfb_d),
                         (sele, sele_d), (sel2, sel2_d), (comb, comb_d), (g1, g1_d), (convw, convw_d),
                         (convb, convb_d), (mng, mng_d), (msk, msk_d), (gbias, gb_d), (gq, gq_d),
                         (gk, gk_d), (wr, wr_d)]:
            S.dma("sp", lambda e, dst=dst, src=src: e.dma_start(out=dst, in_=src), writes=[R("cl")])
        S.dma("sp", lambda e: e.dma_start(out=brbc, in_=br_d.partition_broadcast(128)), writes=[R("cl")])
        for dst, src in [(wqb, wq_d), (wkb, wk_d), (wvb, wv_d)]:
            S.dma("pool", lambda e, dst=dst, src=src: e.dma_start(out=dst, in_=src.rearrange("h p n -> p h n")),
                  writes=[R("cl")])
        S.barrier()
        S.op("act", lambda e: e.activation(out=identb, in_=ident, func=AF.Copy), writes=[R("cl")])
        S.op("pool", lambda e: e.memset(onesb, 1.0), writes=[R("cl")])
        S.barrier()

        m0 = A.mark()
        sc = A.alloc("sc", [128, 8, 2], F32)
        wpiece = A.alloc("wpiece", [128, 8, 1024], F32)
        bfm = A.alloc("bfm", [128, 48], F32)
        brow = A.alloc("brow", [2, 4096], F32)
        mrow = A.alloc("mrow", [2, 4096], F32)
        modfm = A.alloc("modfm", [128, 2, 8, 2], F32)
        load("sp", sc, cT_d, "sc")
        load("sp", bfm, bada_fm_d, "bfm")
        load("sp", brow[0:1, :], bada_row_d[0:1, 2048:6144], "brow0")
        load("sp", brow[1:2, :], bada_row_d[0:1, 2048:6144], "brow1")
        S.op("act", lambda e: e.activation(out=sc, in_=sc, func=AF.Silu), reads=["sc"], writes=["sc"])
        wada_v = wada_d.rearrange("(k p) n -> p k n", p=128)
        for piece in range(6):
            for k in range(8):
                load("sp", wpiece[:, k, :], wada_v[:, k, piece * 1024:(piece + 1) * 1024], ("wpiece", k))
            wp_keys = [("wpiece", k) for k in range(8)]
            if piece < 2:
                for j in range(8):
                    for k in range(8):
                        S.op("pe", lambda e, j=j, k=k: e.matmul(PS[0][:, 2 * j:2 * j + 2], lhsT=wpiece[:, k, j * 128:(j + 1) * 128],
                                                                 rhs=sc[:, k, :], start=(k == 0), stop=(k == 7)),
                             reads=wp_keys + ["sc"], writes=[PB(0)])
                S.op("dve", lambda e, piece=piece: e.tensor_copy(out=modfm[:, piece, :, :], in_=PS[0][:, 0:16].rearrange("p (j b) -> p j b", b=2)),
                     reads=[PB(0)], writes=[("modfm", piece)])
            else:
                for half in range(2):
                    for k in range(8):
                        S.op("pe", lambda e, half=half, k=k: e.matmul(PS[1][0:2, :], lhsT=sc[:, k, :],
                                                                       rhs=wpiece[:, k, half * 512:(half + 1) * 512],
                                                                       start=(k == 0), stop=(k == 7)),
                             reads=wp_keys + ["sc"], writes=[PB(1)])
                    c0 = (piece - 2) * 1024 + half * 512
                    S.op("dve", lambda e, c0=c0: e.tensor_tensor(out=mrow[:, c0:c0 + 512], in0=PS[1][0:2, :], in1=brow[:, c0:c0 + 512], op=ALU.add),
                         reads=[PB(1), "brow0", "brow1"], writes=[("mrow", c0)])
        for b in range(2):
            S.op("dve", lambda e, b=b: e.tensor_tensor(out=B1[:, b, :], in0=modfm[:, 0, :, b], in1=bfm[:, 0:8], op=ALU.add),
                 reads=[("modfm", 0), "bfm"], writes=[("B1", b)])
            S.op("dve", lambda e, b=b: e.tensor_tensor(out=A1[:, b, :], in0=modfm[:, 1, :, b], in1=bfm[:, 8:16], op=ALU.add),
                 reads=[("modfm", 1), "bfm"], writes=[("A1", b)])
            S.op("dve", lambda e, b=b: e.scalar_tensor_tensor(out=A1[:, b, :], in0=A1[:, b, :], scalar=1.0, in1=g1, op0=ALU.add, op1=ALU.mult),
                 reads=[("A1", b)], writes=[("A1", b)])
        S.dma("sp", lambda e: e.dma_start(out=modrow_d, in_=mrow), reads=[("mrow", c) for c in range(0, 4096, 512)], writes=["modrow_d"])
        S.barrier()
        A.release(m0)

        m0 = A.mark()
        relb = A.alloc("relb", [32, 8], F32)
        onehot = A.alloc("onehot", [32, 3, 384], F32)
        ftab = A.alloc("ftab", [8, 3, 384], F32)
        load("sp", relb, relb_d, "relb")
        load("sp", onehot, onehot_d, "onehot")
        for p in range(3):
            S.op("pe", lambda e, p=p: e.matmul(PS[0][0:8, 0:384], lhsT=relb, rhs=onehot[:, p, :], start=True, stop=True),
                 reads=["relb", "onehot"], writes=[PB(0)])
            S.op("act", lambda e, p=p: e.activation(out=ftab[:, p, :], in_=PS[0][0:8, 0:384], func=AF.Exp), reads=[PB(0)], writes=[("ftab", p)])
        for p in range(3):
            S.op("pe", lambda e, p=p: e.matmul(PS[1][0:8, 0:384], lhsT=ones128[0:32, 0:8], rhs=onehot[:, p, :], start=True, stop=True),
                 reads=["onehot"], writes=[PB(1)])
            S.op("dve", lambda e, p=p: e.scalar_tensor_tensor(out=ftab[:, p, :], in0=PS[1][0:8, 0:384], scalar=128.0, in1=ftab[:, p, :],
                                                                op0=ALU.mult, op1=ALU.mult),
                 reads=[PB(1), ("ftab", p)], writes=[("ftab", p)])
        S.dma("sp", lambda e: e.dma_start(out=ftab_d, in_=ftab), reads=[("ftab", p) for p in range(3)], writes=["ftab_d"])
        S.barrier()
        A.release(m0)

        pers_mark = A.mark()
        win_v = win_d.rearrange("(k p) n -> p k n", p=128)
        wout_v = wout_d.rearrange("(k p) n -> p k n", p=128)

        for b in range(nseq):
            A.release(pers_mark)
            A.limit = ARENA_BYTES - 8 * S_LEN * 2
            catT = arena_t[:, (ARENA_BYTES - 8 * S_LEN * 2) // 4: ARENA_BYTES // 4].bitcast(BF16).rearrange("p (a b) -> p a b", a=8)
            hT = A.alloc("hT", [128, 8, S_LEN], BF16)
            seq_mark = A.mark()
            xt = A.alloc("xt", [128, D], F32)
            xs = A.alloc("xs", [128, D], F32)
            junk = A.alloc("junk", [128, D], F32)
            st1 = A.alloc("st1", [128, 4], F32)
            for i in range(NT):
                load("sp", xt, x_d[b, i * 128:(i + 1) * 128, :], "xt")
                S.op("act", lambda e: e.activation(out=junk, in_=xt, func=AF.Square, accum_out=st1[:, 0:1]), reads=["xt"], writes=["junk", "st1"])
                S.op("act", lambda e: e.activation(out=st1[:, 1:2], in_=st1[:, 0:1], func=AF.Ln, scale=1.0 / D, bias=EPS), reads=["st1"], writes=["st1"])
                S.op("act", lambda e: e.activation(out=st1[:, 2:3], in_=st1[:, 1:2], func=AF.Exp, scale=-0.5), reads=["st1"], writes=["st1"])
                S.op("dve", lambda e: e.tensor_scalar(out=xs, in0=xt, scalar1=st1[:, 2:3], scalar2=None, op0=ALU.mult), reads=["xt", "st1"], writes=["xs"])
                for k in range(8):
                    bank = k // 4
                    S.op("pe", lambda e, k=k, bank=bank: e.matmul(PS[bank][:, (k % 4) * 128:(k % 4 + 1) * 128], lhsT=xs[:, k * 128:(k + 1) * 128],
                                                                   rhs=ident, start=True, stop=True),
                         reads=["xs"], writes=[PB(bank)])
                for k in range(8):
                    bank = k // 4
                    eng = "dve" if bank == 0 else "act"
                    if eng == "dve":
                        S.op("dve", lambda e, k=k, bank=bank, i=i: e.tensor_scalar(out=hT[:, k, i * 128:(i + 1) * 128],
                                                                                   in0=PS[bank][:, (k % 4) * 128:(k % 4 + 1) * 128],
                                                                                   scalar1=A1[:, b, k:k + 1], scalar2=B1[:, b, k:k + 1],
                                                                                   op0=ALU.mult, op1=ALU.add),
                             reads=[PB(bank), ("A1", b), ("B1", b)], writes=[("hT", i)])
                    else:
                        S.op("act", lambda e, k=k, bank=bank, i=i: e.activation(out=hT[:, k, i * 128:(i + 1) * 128],
                                                                                in_=PS[bank][:, (k % 4) * 128:(k % 4 + 1) * 128],
                                                                                func=AF.Identity, scale=A1[:, b, k:k + 1], bias=B1[:, b, k:k + 1]),
                             reads=[PB(bank), ("A1", b), ("B1", b)], writes=[("hT", i)])
            hT_keys = [("hT", i) for i in range(NT)]
            if b == 0 and "hT" in dbg:
                S.dma("pool", lambda e: e.dma_start(out=dbg["hT"], in_=hT), reads=hT_keys, writes=["dbg_hT"])
            S.barrier()
            A.release(seq_mark)

            Cg = A.alloc("Cg", [16, S_LEN], F32)
            Dg = A.alloc("Dg", [16, S_LEN], F32)
            utok = A.alloc("utok", [128, 128], F32)
            g_mark = A.mark()
            Zg = A.alloc("Zg", [16, S_LEN], F32)
            Lg = A.alloc("Lg", [16, S_LEN], F32)
            onesr = A.alloc("onesr", [16, S_LEN], F32)
            wgt = A.alloc("wgt", [128, 8, 16], BF16)
            S.dma("pool", lambda e: e.dma_start(out=wgt, in_=win_v[:, :, 1024:1040]), writes=["wgt"])
            S.op("pool", lambda e: e.memset(onesr, 1.0), writes=["onesr"])
            for blk in range(4):
                for k in range(8):
                    S.op("pe", lambda e, k=k, blk=blk: e.matmul(PS[0][0:16, :], lhsT=wgt[:, k, :], rhs=hT[:, k, blk * 512:(blk + 1) * 512],
                                                                 start=(k == 0), stop=(k == 7)),
                         reads=["wgt"] + hT_keys, writes=[PB(0)])
                S.op("dve", lambda e, blk=blk: e.tensor_scalar(out=Zg[:, blk * 512:(blk + 1) * 512], in0=PS[0][0:16, :], scalar1=gbias[:, 0:1],
                                                               scalar2=None, op0=ALU.add),
                     reads=[PB(0)], writes=[("Zg", blk)])
            zk = [("Zg", blk) for blk in range(4)]
            S.op("act", lambda e: e.activation(out=Lg, in_=Zg, func=AF.Exp, scale=-1.0), reads=zk, writes=["Lg"])
            S.op("act", lambda e: e.activation(out=Lg, in_=Lg, func=AF.Ln, bias=1.0), reads=["Lg"], writes=["Lg"])
            S.op("dve", lambda e: e.tensor_tensor_scan(out=Cg, data0=onesr, data1=Lg, initial=0.0, op0=ALU.mult, op1=ALU.add),
                 reads=["onesr", "Lg"], writes=["Cg"])
            S.op("dve", lambda e: e.tensor_tensor(out=Dg, in0=Cg, in1=Lg, op=ALU.subtract), reads=["Cg", "Lg"], writes=["Dg"])
            for i in range(NT):
                sl = slice(i * 128, (i + 1) * 128)
                S.op("pe", lambda e, i=i, sl=sl: e.matmul(PS[1][:, i * 8:(i + 1) * 8], lhsT=Zg[:, sl], rhs=comb[:, 0, :], start=True, stop=False),
                     reads=zk, writes=[PB(1)])
                S.op("pe", lambda e, i=i, sl=sl: e.matmul(PS[1][:, i * 8:(i + 1) * 8], lhsT=Cg[:, sl], rhs=comb[:, 1, :], start=False, stop=False),
                     reads=["Cg"], writes=[PB(1)])
                S.op("pe", lambda e, i=i, sl=sl: e.matmul(PS[1][:, i * 8:(i + 1) * 8], lhsT=Lg[:, sl], rhs=comb[:, 2, :], start=False, stop=True),
                     reads=["Lg"], writes=[PB(1)])
            S.op("dve", lambda e: e.tensor_copy(out=utok, in_=PS[1][:, 0:128]), reads=[PB(1)], writes=["utok"])
            if b == 0:
                dump("Cg", Cg, "Cg")
                dump("utok", utok, "utok")
            S.barrier()
            A.release(g_mark)

            m_mark = A.mark()
            wxm = A.alloc("wxm", [128, 8, 128], BF16)
            wop = A.alloc("wop", [128, 8, 128], BF16)
            xmp = A.alloc("xmp", [128, S_LEN + 4], F32)
            sigo = A.alloc("sigo", [128, S_LEN], BF16)
            cacc = A.alloc("cacc", [128, S_LEN], F32)
            xc = A.alloc("xc", [128, S_LEN], BF16)
            xmb = A.alloc("xmb", [128, S_LEN], BF16)
            qT = A.alloc("qT", [128, S_LEN], BF16)
            kT = A.alloc("kT", [128, S_LEN], BF16)
            vtok = A.alloc("vtok", [128, NT, 128], BF16)
            bcR = Ring("bc", 4, [128, 512], F32)
            tmR = Ring("tmpm", 3, [128, 512], F32)
            wtR = Ring("Wt", 4, [128, 512], F32)
            swR = Ring("STw", 4, [128, 512], BF16)
            fR = Ring("fin", 8, [128, 512], F32)
            stR = Ring("st", 0, banks=[4, 5, 6, 7])
            S.op("pool", lambda e: e.memset(xmp, 0.0), writes=["xmp"])
            mpR = Ring("mpR", 0, banks=[0, 1, 2, 3])
            for h in range(4):
                S.dma("pool", lambda e, h=h: e.dma_start(out=wxm, in_=win_v[:, :, h * 128:(h + 1) * 128]), writes=["wxm"])
                S.dma("pool", lambda e, h=h: e.dma_start(out=wop, in_=win_v[:, :, 512 + h * 128:512 + (h + 1) * 128]), writes=["wop"])
                for blk in range(4):
                    bs = slice(blk * 512, (blk + 1) * 512)
                    pa, pak = mpR.next()
                    for k in range(8):
                        S.op("pe", lambda e, k=k, bs=bs, pa=pa: e.matmul(pa, lhsT=wxm[:, k, :], rhs=hT[:, k, bs], start=(k == 0), stop=(k == 7)),
                             reads=["wxm"] + hT_keys, writes=[pak])
                    S.op("dve", lambda e, blk=blk, pa=pa: e.tensor_copy(out=xmp[:, 2 + blk * 512:2 + (blk + 1) * 512], in_=pa),
                         reads=[pak], writes=["xmp"])
                    pb_, pbk_ = mpR.next()
                    for k in range(8):
                        S.op("pe", lambda e, k=k, bs=bs, pb_=pb_: e.matmul(pb_, lhsT=wop[:, k, :], rhs=hT[:, k, bs], start=(k == 0), stop=(k == 7)),
                             reads=["wop"] + hT_keys, writes=[pbk_])
                    S.op("act", lambda e, bs=bs, pb_=pb_: e.activation(out=sigo[:, bs], in_=pb_, func=AF.Sigmoid), reads=[pbk_], writes=["sigo"])
                S.op("dve", lambda e, h=h: e.tensor_scalar(out=cacc, in0=xmp[:, 0:S_LEN], scalar1=convw[:, h, 0:1], scalar2=None, op0=ALU.mult),
                     reads=["xmp"], writes=["cacc"])
                for j in range(1, 5):
                    S.op("dve", lambda e, h=h, j=j: e.scalar_tensor_tensor(out=cacc, in0=xmp[:, j:j + S_LEN], scalar=convw[:, h, j:j + 1], in1=cacc,
                                                                          op0=ALU.mult, op1=ALU.add),
                         reads=["xmp", "cacc"], writes=["cacc"])
                S.op("act", lambda e, h=h: e.activation(out=xc, in_=cacc, func=AF.Silu, bias=convb[:, h:h + 1]), reads=["cacc"], writes=["xc"])
                S.op("act", lambda e: e.activation(out=xmb, in_=xmp[:, 2:2 + S_LEN], func=AF.Copy), reads=["xmp"], writes=["xmb"])
                for blk in range(4):
                    bs = slice(blk * 512, (blk + 1) * 512)
                    pa, pak = mpR.next()
                    S.op("pe", lambda e, h=h, bs=bs, pa=pa: e.matmul(pa, lhsT=wqb[:, h, :], rhs=xc[:, bs], start=True, stop=True), reads=["xc"], writes=[pak])
                    S.op("act", lambda e, bs=bs, pa=pa: e.activation(out=qT[:, bs], in_=pa, func=AF.Copy), reads=[pak], writes=["qT"])
                    pb_, pbk_ = mpR.next()
                    S.op("pe", lambda e, h=h, bs=bs, pb_=pb_: e.matmul(pb_, lhsT=wkb[:, h, :], rhs=xc[:, bs], start=True, stop=True), reads=["xc"], writes=[pbk_])
                    S.op("dve", lambda e, bs=bs, pb_=pb_: e.tensor_scalar(out=kT[:, bs], in0=pb_, scalar1=1.0 / math.sqrt(128.0), scalar2=None, op0=ALU.mult),
                         reads=[pbk_], writes=["kT"])
                for i4 in range(4):
                    for ii in range(4):
                        i = i4 * 4 + ii
                        if ii == 0:
                            pa, pak = mpR.next()
                        S.op("pe", lambda e, h=h, i=i, ii=ii, pa=pa: e.matmul(pa[:, ii * 128:(ii + 1) * 128], lhsT=xmb[:, i * 128:(i + 1) * 128],
                                                                               rhs=wvb[:, h, :], start=True, stop=True),
                             reads=["xmb"], writes=[pak])
                    S.op("act", lambda e, i4=i4, pa=pa: e.activation(out=vtok[:, i4 * 4:(i4 + 1) * 4, :], in_=pa.rearrange("p (a b) -> p a b", a=4), func=AF.Copy),
                         reads=[pak], writes=["vtok"])
                for T in range(4):
                    bs = slice(T * 512, (T + 1) * 512)
                    pb1, pb1k = stR.next()
                    S.op("pe", lambda e, h=h, bs=bs, pb1=pb1: e.matmul(pb1, lhsT=selfb[:, h, :], rhs=Cg[:, bs], start=True, stop=True), reads=["Cg"], writes=[pb1k])
                    bcf, bcfk = bcR.next()
                    S.op("act", lambda e, bcf=bcf, pb1=pb1: e.activation(out=bcf, in_=pb1, func=AF.Copy), reads=[pb1k], writes=[bcfk])
                    pb2, pb2k = stR.next()
                    S.op("pe", lambda e, h=h, bs=bs, pb2=pb2: e.matmul(pb2, lhsT=selfb[:, 4 + h, :], rhs=Dg[:, bs], start=True, stop=True), reads=["Dg"], writes=[pb2k])
                    bcb, bcbk = bcR.next()
                    S.op("act", lambda e, bcb=bcb, pb2=pb2: e.activation(out=bcb, in_=pb2, func=AF.Copy), reads=[pb2k], writes=[bcbk])
                    nf = 4 * T + 4
                    nb = 16 - 4 * T
                    cf = 0
                    cb = 0
                    sts = {}

                    def emit_st(j, bs=bs, sts=sts):
                        st, stk = stR.next()
                        S.op("pe", lambda e, j=j, bs=bs, st=st: e.matmul(st, lhsT=kT[:, j * 128:(j + 1) * 128], rhs=qT[:, bs], start=True, stop=True),
                             reads=["kT", "qT"], writes=[stk])
                        sts[j] = (st, stk)
                    LOOK = 2
                    for j in range(LOOK):
                        emit_st(j)
                    for j in range(NT):
                        fwd = j <= 4 * T + 3
                        bwd = j >= 4 * T
                        if j + LOOK < NT:
                            emit_st(j + LOOK)
                        st, stk = sts[j]
                        for dirn in (0, 1):
                            if (dirn == 0 and not fwd) or (dirn == 1 and not bwd):
                                continue
                            bc, bck = (bcf, bcfk) if dirn == 0 else (bcb, bcbk)
                            src, srck = bc, bck
                            if 4 * T <= j <= 4 * T + 3:
                                o = (j - 4 * T) * 128
                                mk = maskF if dirn == 0 else maskB
                                tmpm, tmpk = tmR.next()
                                S.op("pool", lambda e, bc=bc, mk=mk, o=o, tmpm=tmpm: e.tensor_tensor(out=tmpm, in0=bc, in1=mk[:, 384 - o:384 - o + 512], op=ALU.add),
                                     reads=[bck], writes=[tmpk])
                                src, srck = tmpm, tmpk
                            ucol = j * 8 + dirn * 4 + h
                            Wt, Wtk = wtR.next()
                            S.op("act", lambda e, src=src, ucol=ucol, Wt=Wt: e.activation(out=Wt, in_=src, func=AF.Exp, bias=utok[:, ucol:ucol + 1]),
                                 reads=[srck, "utok"], writes=[Wtk])
                            STw, STwk = swR.next()
                            S.op("dve", lambda e, st=st, Wt=Wt, STw=STw: e.tensor_tensor(out=STw, in0=st, in1=Wt, op=ALU.mult), reads=[stk, Wtk], writes=[STwk])
                            if dirn == 0:
                                first, last = (cf == 0), (cf == nf - 1)
                                cf += 1
                                nb_, db_ = 0, 1
                            else:
                                first, last = (cb == 0), (cb == nb - 1)
                                cb += 1
                                nb_, db_ = 2, 3
                            S.op("pe", lambda e, j=j, nb_=nb_, first=first, last=last, STw=STw: e.matmul(PS[nb_], lhsT=vtok[:, j, :], rhs=STw, start=first, stop=last),
                                 reads=["vtok", STwk], writes=[PB(nb_)])
                            S.op("pe", lambda e, db_=db_, first=first, last=last, STw=STw: e.matmul(PS[db_], lhsT=onesb, rhs=STw, start=first, stop=last),
                                 reads=[STwk], writes=[PB(db_)])
                    (fa, fak), (fb_, fbk), (fc_, fck), (fd_, fdk) = fR.next(), fR.next(), fR.next(), fR.next()
                    for (nb_, db_, dst, dk) in ((0, 1, fa, fak), (2, 3, fb_, fbk)):
                        S.op("act", lambda e, db_=db_, fd_=fd_: e.activation(out=fd_, in_=PS[db_], func=AF.Copy), reads=[PB(db_)], writes=[fdk])
                        S.op("dve", lambda e, fc_=fc_, fd_=fd_: e.scalar_tensor_tensor(out=fc_, in0=fd_, scalar=-1.0, in1=fd_, op0=ALU.mult, op1=ALU.max),
                             reads=[fdk], writes=[fck])
                        S.op("dve", lambda e, fc_=fc_: e.tensor_scalar(out=fc_, in0=fc_, scalar1=1.0, scalar2=None, op0=ALU.max), reads=[fck], writes=[fck])
                        S.op("dve", lambda e, fc_=fc_: e.reciprocal(out=fc_, in_=fc_), reads=[fck], writes=[fck])
                        S.op("dve", lambda e, nb_=nb_, dst=dst, fc_=fc_: e.tensor_tensor(out=dst, in0=PS[nb_], in1=fc_, op=ALU.mult), reads=[PB(nb_), fck], writes=[dk])
                    S.op("pool", lambda e, fa=fa, fb_=fb_: e.tensor_tensor(out=fa, in0=fa, in1=fb_, op=ALU.add), reads=[fak, fbk], writes=[fak])
                    S.op("act", lambda e, fa=fa, fd_=fd_: e.activation(out=fd_, in_=fa, func=AF.Square), reads=[fak], writes=[fdk])
                    pb3, pb3k = stR.next()
                    S.op("pe", lambda e, fd_=fd_, pb3=pb3: e.matmul(pb3, lhsT=ones128, rhs=fd_, start=True, stop=True), reads=[fdk], writes=[pb3k])
                    S.op("act", lambda e, fd_=fd_, pb3=pb3: e.activation(out=fd_, in_=pb3, func=AF.Ln, bias=EPS), reads=[pb3k], writes=[fdk])
                    S.op("act", lambda e, fd_=fd_: e.activation(out=fd_, in_=fd_, func=AF.Exp, scale=-0.5), reads=[fdk], writes=[fdk])
                    S.op("dve", lambda e, h=h, fa=fa, fd_=fd_: e.scalar_tensor_tensor(out=fa, in0=fa, scalar=mng[:, h:h + 1], in1=fd_, op0=ALU.mult, op1=ALU.mult),
                         reads=[fak, fdk], writes=[fak])
                    S.op("dve", lambda e, h=h, bs=bs, fa=fa: e.scalar_tensor_tensor(out=fa, in0=xc[:, bs], scalar=msk[:, h:h + 1], in1=fa, op0=ALU.mult, op1=ALU.add),
                         reads=[fak, "xc"], writes=[fak])
                    S.op("dve", lambda e, h=h, bs=bs, fa=fa: e.tensor_tensor(out=catT[:, h, bs], in0=fa, in1=sigo[:, bs], op=ALU.mult),
                         reads=[fak, "sigo"], writes=[("catT", h)])
            S.barrier()
            A.release(m_mark)

            a_mark = A.mark()
            wq3 = A.alloc("wq3", [128, 8, 128], BF16)
            wk3 = A.alloc("wk3", [128, 8, 128], BF16)
            wv3 = A.alloc("wv3", [128, 8, 128], BF16)
            a32R = Ring("a32", 3, [128, 512], F32)
            tqR = Ring("tq", 3, [128, 512], F32)
            prR = Ring("prR", 0, banks=[0, 1])
            nrR = Ring("nrR", 0, banks=[2, 3])
            qn = A.alloc("qn", [128, S_LEN], BF16)
            kn = A.alloc("kn", [128, S_LEN], BF16)
            av = A.alloc("av", [128, S_LEN], BF16)
            qd = A.alloc("qd", [128, S_LEN], BF16)
            kd = A.alloc("kd", [128, S_LEN], BF16)
            avd = A.alloc("avd", [128, S_LEN], BF16)
            VpA = A.alloc("VpA", [128, NT, 128], BF16)
            VpB = A.alloc("VpB", [128, NT, 128], BF16)
            tabs = A.alloc("tabs", [128, 3, 2, 256], BF16)
            recb = A.alloc("recb", [128, 512], F32)
            S.op("pool", lambda e: e.memset(VpA, 1.0), writes=["VpA"])
            S.op("pool", lambda e: e.memset(VpB, 1.0), writes=["VpB"])
            tabsR = A.alloc("tabsR", [128, 3, 2, 256], F32)
            accN = A.alloc("accN", [128, S_LEN], F32)
            accD = A.alloc("accD", [128, S_LEN], F32)
            etR = Ring("Et", 4, [128, 2, 256], BF16)
            ptR = Ring("Pt", 4, [128, 2, 256], BF16)
            stA_items = [(PSD[2], 4, 5), (PSD[3], 6, 7)]
            stA_i = [0]
            for c in range(4):
                for (wt, col0, wkey) in ((wq3, 1040, "wq3"), (wk3, 1552, "wk3"), (wv3, 2064, "wv3")):
                    S.dma("pool", lambda e, wt=wt, col0=col0, c=c: e.dma_start(out=wt, in_=win_v[:, :, col0 + c * 128:col0 + (c + 1) * 128]), writes=[wkey])
                for p in range(3):
                    for hh in range(2):
                        hd = 2 * c + hh
                        src = bass.AP(tensor=ftab_d.tensor, offset=(hd * 3 + p) * 384, ap=[[1, 128], [1, 256]])
                        S.dma("sp", lambda e, p=p, hh=hh, src=src: e.dma_start(out=tabsR[:, p, hh, :], in_=src), reads=["ftab_d"], writes=[("tabsR", p, hh)])
                    for hh in range(2):
                        S.op("pe", lambda e, p=p, hh=hh: e.matmul(PS[0][:, hh * 256:(hh + 1) * 256], lhsT=jrev, rhs=tabsR[:, p, hh, :], start=True, stop=True),
                             reads=[("tabsR", p, hh)], writes=[PB(0)])
                    S.op("dve", lambda e, p=p: e.tensor_copy(out=tabs[:, p, :, :], in_=PS[0].rearrange("p (a b) -> p a b", a=2)), reads=[PB(0)], writes=["tabs"])
                pjobs = []
                for (wt, wkey, dst, dkey, gvec) in ((wq3, "wq3", qn, "qn", gq), (wk3, "wk3", kn, "kn", gk)):
                    for blk in range(4):
                        pjobs.append((wt, wkey, dst, dkey, gvec, slice(blk * 512, (blk + 1) * 512)))
                pst = {}

                def emit_P(ji):
                    wt, wkey, dst, dkey, gvec, bs = pjobs[ji]
                    pb, pbk = prR.next()
                    a32_, a32k = a32R.next()
                    tq_, tqk = tqR.next()
                    for k in range(8):
                        S.op("pe", lambda e, k=k, bs=bs, wt=wt, pb=pb: e.matmul(pb, lhsT=wt[:, k, :], rhs=hT[:, k, bs], start=(k == 0), stop=(k == 7)),
                             reads=[wkey] + hT_keys, writes=[pbk])
                    S.op("act", lambda e, pb=pb, a32_=a32_: e.activation(out=a32_, in_=pb, func=AF.Copy), reads=[pbk], writes=[a32k])
                    S.op("act", lambda e, pb=pb, tq_=tq_: e.activation(out=tq_, in_=pb, func=AF.Square), reads=[pbk], writes=[tqk])
                    pst[ji] = (a32_, a32k, tq_, tqk)

                def emit_N(ji):
                    wt, wkey, dst, dkey, gvec, bs = pjobs[ji]
                    a32_, a32k, tq_, tqk = pst[ji]
                    nbk, nbkk = nrR.next()
                    S.op("pe", lambda e, tq_=tq_, nbk=nbk: e.matmul(nbk, lhsT=blk64, rhs=tq_, start=True, stop=True), reads=[tqk], writes=[nbkk])
                    S.op("act", lambda e, tq_=tq_, nbk=nbk: e.activation(out=tq_, in_=nbk, func=AF.Ln, bias=EPS), reads=[nbkk], writes=[tqk])
                    S.op("act", lambda e, tq_=tq_: e.activation(out=tq_, in_=tq_, func=AF.Exp, scale=-0.5), reads=[tqk], writes=[tqk])
                    S.op("dve", lambda e, bs=bs, dst=dst, gvec=gvec, a32_=a32_, tq_=tq_: e.scalar_tensor_tensor(out=dst[:, bs], in0=a32_, scalar=gvec[:, 0:1], in1=tq_,
                                                                                                           op0=ALU.mult, op1=ALU.mult),
                         reads=[a32k, tqk], writes=[dkey])
                emit_P(0)
                for ji in range(len(pjobs)):
                    if ji + 1 < len(pjobs):
                        emit_P(ji + 1)
                    emit_N(ji)
                for blk in range(4):
                    bs = slice(blk * 512, (blk + 1) * 512)
                    pb, pbk = prR.next()
                    for k in range(8):
                        S.op("pe", lambda e, k=k, bs=bs, pb=pb: e.matmul(pb, lhsT=wv3[:, k, :], rhs=hT[:, k, bs], start=(k == 0), stop=(k == 7)),
                             reads=["wv3"] + hT_keys, writes=[pbk])
                    S.op("act", lambda e, bs=bs, pb=pb: e.activation(out=av[:, bs], in_=pb, func=AF.Copy), reads=[pbk], writes=["av"])
                for p, d in enumerate((1, 4, 16)):
                    L = S_LEN // d
                    if d == 1:
                        qv, kv, vv = qn, kn, av
                        qk_, kk_, vk_ = "qn", "kn", "av"
                    else:
                        S.op("pool", lambda e, d=d: e.tensor_copy(out=qd.rearrange("p (d l) -> p d l", d=d), in_=qn.rearrange("p (l d) -> p d l", d=d)),
                             reads=["qn"], writes=["qd"])
                        S.op("pool", lambda e, d=d: e.tensor_copy(out=kd.rearrange("p (d l) -> p d l", d=d), in_=kn.rearrange("p (l d) -> p d l", d=d)),
                             reads=["kn"], writes=["kd"])
                        S.op("pool", lambda e, d=d: e.tensor_copy(out=avd.rearrange("p (d l) -> p d l", d=d), in_=av.rearrange("p (l d) -> p d l", d=d)),
                             reads=["av"], writes=["avd"])
                        qv, kv, vv = qd, kd, avd
                        qk_, kk_, vk_ = "qd", "kd", "avd"
                    for i4 in range(4):
                        for ii in range(4):
                            i = i4 * 4 + ii
                            S.op("pe", lambda e, i=i, ii=ii, vv=vv: e.matmul(PS[0][:, ii * 128:(ii + 1) * 128], lhsT=vv[:, i * 128:(i + 1) * 128], rhs=identb,
                                                                               start=True, stop=True),
                                 reads=[vk_], writes=[PB(0)])
                        S.op("act", lambda e, i4=i4: e.activation(out=VpA[:, i4 * 4:(i4 + 1) * 4, 0:64], in_=PS[0].rearrange("p (a b) -> p a b", a=4)[:, :, 0:64], func=AF.Copy),
                             reads=[PB(0)], writes=["VpA"])
                        S.op("dve", lambda e, i4=i4: e.tensor_copy(out=VpB[:, i4 * 4:(i4 + 1) * 4, 64:128], in_=PS[0].rearrange("p (a b) -> p a b", a=4)[:, :, 64:128]),
                             reads=[PB(0), "VpA"], writes=["VpB"])
                    nkt = L // 128
                    for qb in range(4):
                        S.op("dve", lambda e: e.memset(PS[2], 0.0), writes=[PB(2)])
                        S.op("dve", lambda e: e.memset(PS[3], 0.0), writes=[PB(3)])
                        if L >= 512:
                            phases = [(qb * 512) // L]
                        else:
                            phases = list(range((qb * 512) // L, (qb * 512 + 512) // L))
                        tiles = []
                        for r in phases:
                            base = r * L
                            blo = max(qb * 512, base) - base
                            bhi = min(qb * 512 + 512, base + L) - base
                            for n in range(nkt):
                                qlo = max(blo, 128 * n - 64)
                                qhi = min(bhi, 128 * n + 192)
                                if qhi <= qlo:
                                    continue
                                nq = qhi - qlo
                                toff = qlo - (128 * n - 64)
                                gk0 = base + 128 * n
                                gq0 = base + qlo
                                col0 = gq0 - qb * 512
                                tiles.append((nq, toff, gk0, gq0, col0))
                        stt = {}

                        def emit_qk(t, tiles=tiles, stt=stt, kv=kv, qv=qv, kk_=kk_, qk_=qk_):
                            nq, toff, gk0, gq0, col0 = tiles[t]
                            std, bka, bkb = stA_items[stA_i[0] % 2]
                            stA_i[0] += 1
                            st3 = std.rearrange("p (a b) -> p a b", a=2)
                            for hh in range(2):
                                ps_ = slice(64 * hh, 64 * hh + 64)
                                S.op("pe", lambda e, ps_=ps_, hh=hh, gk0=gk0, gq0=gq0, nq=nq, st3=st3: e.matmul(
                                    st3[:, hh, 0:nq], lhsT=kv[ps_, gk0:gk0 + 128], rhs=qv[ps_, gq0:gq0 + nq], start=True, stop=True),
                                    reads=[kk_, qk_], writes=[PB(bka if hh == 0 else bkb)])
                            stt[t] = (st3, bka, bkb)
                        if tiles:
                            emit_qk(0)
                        for t in range(len(tiles)):
                            if t + 1 < len(tiles):
                                emit_qk(t + 1)
                            nq, toff, gk0, gq0, col0 = tiles[t]
                            st3, bka, bkb = stt[t]
                            Et, Etk = etR.next()
                            Pt, Ptk = ptR.next()
                            S.op("act", lambda e, st3=st3, nq=nq, Et=Et: e.activation(out=Et[:, :, 0:nq], in_=st3[:, :, 0:nq], func=AF.Exp, scale=0.125),
                                 reads=[PB(bka), PB(bkb)], writes=[Etk])
                            S.op("dve", lambda e, nq=nq, p=p, toff=toff, Et=Et, Pt=Pt: e.tensor_tensor(out=Pt[:, :, 0:nq], in0=Et[:, :, 0:nq],
                                                                                                       in1=tabs[:, p, :, toff:toff + nq], op=ALU.mult),
                                 reads=[Etk, "tabs"], writes=[Ptk])
                            ti = gk0 // 128
                            S.op("pe", lambda e, ti=ti, col0=col0, nq=nq, Pt=Pt: e.matmul(PS[2][:, col0:col0 + nq], lhsT=VpA[:, ti, :], rhs=Pt[:, 0, 0:nq],
                                                                                         start=False, stop=False, skip_group_check=True),
                                 reads=["VpA", Ptk], writes=[PB(2)])
                            S.op("pe", lambda e, ti=ti, col0=col0, nq=nq, Pt=Pt: e.matmul(PS[3][:, col0:col0 + nq], lhsT=VpB[:, ti, :], rhs=Pt[:, 1, 0:nq],
                                                                                         start=False, stop=False, skip_group_check=True),
                                 reads=["VpB", Ptk], writes=[PB(3)])
                        if d == 1:
                            S.op("act", lambda e, qb=qb: e.activation(out=accN[:, qb * 512:(qb + 1) * 512], in_=PS[2], func=AF.Copy), reads=[PB(2)], writes=["accN"])
                            S.op("dve", lambda e, qb=qb: e.tensor_copy(out=accD[:, qb * 512:(qb + 1) * 512], in_=PS[3]), reads=[PB(3)], writes=["accD"])
                        else:
                            npb = 512 // L if L < 512 else 1
                            r0 = (qb * 512) // L
                            for (acc, ak, bank) in ((accN, "accN", 2), (accD, "accD", 3)):
                                if npb == 1:
                                    view = acc.rearrange("p (l d) -> p d l", d=d)[:, r0, :]
                                    pin = PS[bank]
                                else:
                                    view = acc.rearrange("p (l d) -> p d l", d=d)[:, r0:r0 + npb, :]
                                    pin = PS[bank].rearrange("p (a b) -> p a b", a=npb)
                                S.op("dve", lambda e, view=view, pin=pin: e.tensor_tensor(out=view, in0=pin, in1=view, op=ALU.add), reads=[PB(bank), ak], writes=[ak])
                if b == 0 and c == 0:
                    dump("tabs", tabs, "tabs")
                    dump("accN", accN, "accN")
                    dump("accD", accD, "accD")
                    if "qn" in dbg:
                        S.dma("pool", lambda e: e.dma_start(out=dbg["qn"], in_=qn), reads=["qn"], writes=["dbg_qn"])
                        S.dma("pool", lambda e: e.dma_start(out=dbg["kn"], in_=kn), reads=["kn"], writes=["dbg_kn"])
                        S.dma("pool", lambda e: e.dma_start(out=dbg["av"], in_=av), reads=["av"], writes=["dbg_av"])
                for blk in range(4):
                    bs = slice(blk * 512, (blk + 1) * 512)
                    S.op("pe", lambda e, bs=bs: e.matmul(PS[0], lhsT=selab[:, 0, :], rhs=accN[:, bs], start=True, stop=False), reads=["accN"], writes=[PB(0)])
                    S.op("pe", lambda e, bs=bs: e.matmul(PS[0], lhsT=selab[:, 1, :], rhs=accD[:, bs], start=False, stop=True), reads=["accD"], writes=[PB(0)])
                    S.op("dve", lambda e: e.reciprocal(out=recb, in_=PS[0]), reads=[PB(0)], writes=["recb"])
                    S.op("dve", lambda e, c=c, bs=bs: e.tensor_tensor(out=catT[0:64, 4 + c, bs], in0=accN[0:64, bs], in1=recb[0:64, :], op=ALU.mult),
                         reads=["accN", "recb"], writes=[("catT", 4 + c)])
                    S.op("dve", lambda e, c=c, bs=bs: e.tensor_tensor(out=catT[64:128, 4 + c, bs], in0=accD[64:128, bs], in1=recb[64:128, :], op=ALU.mult),
                         reads=["accD", "recb"], writes=[("catT", 4 + c)])
            if b == 0 and "catT" in dbg:
                S.dma("pool", lambda e: e.dma_start(out=dbg["catT"], in_=catT), reads=[("catT", k) for k in range(8)], writes=["dbg_catT"])
            if max_phase < 5:
                S.barrier()
                continue
            S.barrier()
            A.release(a_mark)
            A.release(pers_mark)

            h2tok = A.alloc("h2tok", [128, NT, D], BF16)
            afft = A.alloc("afft", [128, NT, 16], F32)
            slott = A.alloc("slott", [128, NT, 16], F32)
            slotv = A.alloc("slotv", [16, S_LEN], F32)
            f_mark = A.mark()
            wo = A.alloc("wo", [128, 8, D], BF16)
            h2 = A.alloc("h2", [128, D], F32)
            h2T = A.alloc("h2T", [128, 8, 128], F32)
            g1bc = A.alloc("g1bc", [128, D], F32)
            a2bc = A.alloc("a2bc", [128, D], F32)
            b2bc = A.alloc("b2bc", [128, D], F32)
            affT = A.alloc("affT", [16, S_LEN], F32)
            work = A.alloc("work", [16, S_LEN], F32)
            mx8 = A.alloc("mx8", [16, 8], F32)
            onesr = A.alloc("onesr2", [16, S_LEN], F32)
            for k in range(8):
                S.dma("pool", lambda e, k=k: e.dma_start(out=wo[:, k, :], in_=wout_v[:, k, :]), writes=[("wo", k)])
            wo_keys = [("wo", k) for k in range(8)]
            S.dma("sp", lambda e: e.dma_start(out=g1bc, in_=modrow_d[b:b + 1, 0:1024].partition_broadcast(128)), reads=["modrow_d"], writes=["g1bc"])
            S.dma("sp", lambda e: e.dma_start(out=b2bc, in_=modrow_d[b:b + 1, 1024:2048].partition_broadcast(128)), reads=["modrow_d"], writes=["b2bc"])
            S.dma("sp", lambda e: e.dma_start(out=a2bc, in_=modrow_d[b:b + 1, 2048:3072].partition_broadcast(128)), reads=["modrow_d"], writes=["a2bc"])
            S.dma("sp", lambda e: e.dma_start(out=h2, in_=g2_d.partition_broadcast(128)), writes=["h2"])
            S.op("dve", lambda e: e.scalar_tensor_tensor(out=a2bc, in0=a2bc, scalar=1.0, in1=h2, op0=ALU.add, op1=ALU.mult), reads=["a2bc", "h2"], writes=["a2bc"])
            S.op("pool", lambda e: e.memset(onesr, 1.0), writes=["onesr2"])
            cat_keys = [("catT", k) for k in range(8)]
            xtR = Ring("xtr", 2, [128, D], F32)
            x1R = Ring("x1r", 2, [128, D], F32)
            h2R = Ring("h2r", 3, [128, D], F32)
            stR5 = Ring("st2r", 3, [128, 8], F32)
            lgR = Ring("lgr", 2, [128, 16], F32)
            opb = [(0, 1), (0, 1)]
            h2T_b = A.alloc("h2Tb", [128, 8, 128], F32)
            tpb = [(2, 3), (6, 7)]
            stA5 = {}

            def emit_A(i):
                ts_ = slice(i * 128, (i + 1) * 128)
                xt_, xtk = xtR.next()
                x1_, x1k = x1R.next()
                h2_, h2k_ = h2R.next()
                st_, stk_ = stR5.next()
                load("sp", xt_, x_d[b, ts_, :], xtk)
                for half in range(2):
                    hs = slice(half * 512, (half + 1) * 512)
                    bank = opb[i % 2][half]
                    for k in range(8):
                        S.op("pe", lambda e, k=k, ts_=ts_, hs=hs, bank=bank: e.matmul(PS[bank], lhsT=catT[:, k, ts_], rhs=wo[:, k, hs], start=(k == 0), stop=(k == 7)),
                             reads=cat_keys + wo_keys, writes=[PB(bank)])
                    S.op("dve", lambda e, hs=hs, bank=bank, x1_=x1_: e.tensor_tensor(out=x1_[:, hs], in0=PS[bank], in1=g1bc[:, hs], op=ALU.mult), reads=[PB(bank), "g1bc"], writes=[x1k])
                S.op("pool", lambda e, x1_=x1_, xt_=xt_: e.tensor_tensor(out=x1_, in0=x1_, in1=xt_, op=ALU.add), reads=[x1k, xtk], writes=[x1k])
                S.dma("sp", lambda e, ts_=ts_, x1_=x1_: e.dma_start(out=out_d[b, ts_, :], in_=x1_), reads=[x1k], writes=[("outd", b, i)])
                S.op("act", lambda e, x1_=x1_, h2_=h2_, st_=st_: e.activation(out=h2_, in_=x1_, func=AF.Square, accum_out=st_[:, 0:1]), reads=[x1k], writes=[h2k_, stk_])
                S.op("act", lambda e, st_=st_: e.activation(out=st_[:, 1:2], in_=st_[:, 0:1], func=AF.Ln, scale=1.0 / D, bias=EPS), reads=[stk_], writes=[stk_])
                S.op("act", lambda e, st_=st_: e.activation(out=st_[:, 2:3], in_=st_[:, 1:2], func=AF.Exp, scale=-0.5), reads=[stk_], writes=[stk_])
                S.op("dve", lambda e, x1_=x1_, h2_=h2_, st_=st_: e.scalar_tensor_tensor(out=h2_, in0=x1_, scalar=st_[:, 2:3], in1=a2bc, op0=ALU.mult, op1=ALU.mult),
                     reads=[x1k, stk_, "a2bc"], writes=[h2k_])
                S.op("pool", lambda e, h2_=h2_: e.tensor_tensor(out=h2_, in0=h2_, in1=b2bc, op=ALU.add), reads=[h2k_, "b2bc"], writes=[h2k_])
                S.op("act", lambda e, i=i, h2_=h2_: e.activation(out=h2tok[:, i, :], in_=h2_, func=AF.Copy), reads=[h2k_], writes=[("h2tok", i)])
                stA5[i] = (h2_, h2k_, st_, stk_)

            def emit_B(i):
                ts_ = slice(i * 128, (i + 1) * 128)
                h2_, h2k_, st_, stk_ = stA5[i]
                lg_, lgk = lgR.next()
                tb0, tb1 = tpb[i % 2]
                hT_ = h2T if i % 2 == 0 else h2T_b
                hTk = "h2T" if i % 2 == 0 else "h2Tb"
                for k in range(8):
                    bank = tb0 if k < 4 else tb1
                    S.op("pe", lambda e, k=k, bank=bank, h2_=h2_: e.matmul(PS[bank][:, (k % 4) * 128:(k % 4 + 1) * 128], lhsT=h2_[:, k * 128:(k + 1) * 128], rhs=ident,
                                                                            start=True, stop=True), reads=[h2k_], writes=[PB(bank)])
                S.op("dve", lambda e, hT_=hT_, tb0=tb0: e.tensor_copy(out=hT_[:, 0:4, :], in_=PS[tb0].rearrange("p (a b) -> p a b", a=4)), reads=[PB(tb0)], writes=[hTk])
                S.op("act", lambda e, hT_=hT_, tb1=tb1: e.activation(out=hT_[:, 4:8, :], in_=PS[tb1].rearrange("p (a b) -> p a b", a=4), func=AF.Copy), reads=[PB(tb1)], writes=[hTk])
                for k in range(8):
                    S.op("pe", lambda e, k=k, hT_=hT_: e.matmul(PS[4][:, 0:16], lhsT=hT_[:, k, :], rhs=wr[:, k, :], start=(k == 0), stop=(k == 7)), reads=[hTk], writes=[PB(4)])
                S.op("dve", lambda e, lg_=lg_: e.tensor_tensor(out=lg_, in0=PS[4][:, 0:16], in1=brbc, op=ALU.add), reads=[PB(4)], writes=[lgk])
                S.op("dve", lambda e, lg_=lg_, st_=st_: e.tensor_reduce(out=st_[:, 3:4], in_=lg_, axis=AX.X, op=ALU.max), reads=[lgk], writes=[stk_])
                S.op("dve", lambda e, st_=st_: e.tensor_scalar(out=st_[:, 4:5], in0=st_[:, 3:4], scalar1=-1.0, scalar2=None, op0=ALU.mult), reads=[stk_], writes=[stk_])
                S.op("act", lambda e, lg_=lg_, st_=st_: e.activation(out=lg_, in_=lg_, func=AF.Exp, bias=st_[:, 4:5], accum_out=st_[:, 5:6]), reads=[lgk, stk_], writes=[lgk, stk_])
                S.op("dve", lambda e, st_=st_: e.reciprocal(out=st_[:, 6:7], in_=st_[:, 5:6]), reads=[stk_], writes=[stk_])
                S.op("dve", lambda e, i=i, lg_=lg_, st_=st_: e.tensor_scalar(out=afft[:, i, :], in0=lg_, scalar1=st_[:, 6:7], scalar2=None, op0=ALU.mult), reads=[lgk, stk_], writes=[("afft", i)])
                S.op("pe", lambda e, i=i: e.matmul(PS[5][0:16, 0:128], lhsT=afft[:, i, :], rhs=ident, start=True, stop=True), reads=[("afft", i)], writes=[PB(5)])
                S.op("act", lambda e, ts_=ts_: e.activation(out=affT[:, ts_], in_=PS[5][0:16, 0:128], func=AF.Copy), reads=[PB(5)], writes=["affT"])

            emit_A(0)
            for i in range(NT):
                if i + 1 < NT:
                    emit_A(i + 1)
                emit_B(i)
            S.op("dve", lambda e: e.tensor_copy(out=work, in_=affT), reads=["affT"], writes=["work"])
            for rnd in range(CAP // 8):
                S.op("dve", lambda e: e.max(out=mx8, in_=work), reads=["work"], writes=["mx8"])
                S.op("dve", lambda e: e.match_replace(out=work, in_to_replace=mx8, in_values=work, imm_value=0.0), reads=["work", "mx8"], writes=["work"])
            S.op("dve", lambda e: e.tensor_tensor(out=work, in0=affT, in1=work, op=ALU.subtract), reads=["affT", "work"], writes=["work"])
            S.op("dve", lambda e: e.tensor_scalar(out=work, in0=work, scalar1=0.0, scalar2=None, op0=ALU.is_gt), reads=["work"], writes=["work"])
            S.op("dve", lambda e: e.tensor_tensor_scan(out=slotv, data0=onesr, data1=work, initial=0.0, op0=ALU.mult, op1=ALU.add), reads=["onesr2", "work"], writes=["slotv"])
            S.op("dve", lambda e: e.tensor_tensor(out=slotv, in0=slotv, in1=work, op=ALU.mult), reads=["slotv", "work"], writes=["slotv"])
            S.op("dve", lambda e: e.tensor_scalar(out=slotv, in0=slotv, scalar1=-1.0, scalar2=None, op0=ALU.add), reads=["slotv"], writes=["slotv"])
            for i in range(NT):
                S.op("pe", lambda e, i=i: e.matmul(PS[6][:, i * 16:(i + 1) * 16], lhsT=slotv[:, i * 128:(i + 1) * 128], rhs=ident[0:16, 0:16], start=True, stop=True),
                     reads=["slotv"], writes=[PB(6)])
            S.op("dve", lambda e: e.tensor_copy(out=slott, in_=PS[6][:, 0:256].rearrange("p (a b) -> p a b", a=NT)), reads=[PB(6)], writes=["slott"])
            if b == 0:
                dump("slotv", slotv, "slotv")
                dump("affT", affT, "affT")
                if "h2tok" in dbg:
                    S.dma("pool", lambda e: e.dma_start(out=dbg["h2tok"], in_=h2tok), reads=[("h2tok", i) for i in range(NT)], writes=["dbg_h2tok"])
            if max_phase < 6:
                S.barrier()
                continue
            S.barrier()
            A.release(f_mark)

            A.limit = ARENA_BYTES
            yacc = A.alloc("yacc", [128, NT, D], F32)
            y_mark = A.mark()
            Pm = A.alloc("Pm", [128, NT, CAP], BF16)
            PTm = A.alloc("PTm", [128, 2, S_LEN], BF16)
            xin = A.alloc("xin", [128, 8, CAP], BF16)
            wR = Ring("wbuf", 4, [128, 4096], BF16)
            hid = A.alloc("hid", [128, 16, CAP], BF16)
            yex = A.alloc("yex", [128, 2, D], BF16)
            sgR = Ring("sg", 2, [128, CAP], F32)
            fR2 = Ring("ffps", 0, banks=[0, 1, 2, 3])
            h2k = [("h2tok", i) for i in range(NT)]
            for ex in range(NEXP):
                for i in range(NT):
                    S.op("dve", lambda e, i=i, ex=ex: e.tensor_scalar(out=Pm[:, i, :], in0=iotaf, scalar1=slott[:, i, ex:ex + 1], scalar2=None, op0=ALU.is_equal),
                         reads=["slott"], writes=["Pm"])
                for blk in range(4):
                    bs = slice(blk * 512, (blk + 1) * 512)
                    pb, pbk = fR2.next()
                    S.op("pe", lambda e, ex=ex, bs=bs, pb=pb: e.matmul(pb, lhsT=sele[:, ex, :], rhs=slotv[:, bs], start=True, stop=True), reads=["slotv"], writes=[pbk])
                    for ch in range(2):
                        S.op("dve", lambda e, ch=ch, bs=bs, pb=pb: e.tensor_scalar(out=PTm[:, ch, bs], in0=pb, scalar1=iotap[:, ch:ch + 1], scalar2=None, op0=ALU.is_equal),
                             reads=[pbk], writes=["PTm"])
                for k2 in range(4):
                    pb, pbk = fR2.next()
                    for kk in range(2):
                        k = k2 * 2 + kk
                        for i in range(NT):
                            S.op("pe", lambda e, k=k, kk=kk, i=i, pb=pb: e.matmul(pb[:, kk * 256:(kk + 1) * 256], lhsT=h2tok[:, i, k * 128:(k + 1) * 128], rhs=Pm[:, i, :],
                                                                                  start=(i == 0), stop=(i == NT - 1)),
                                 reads=h2k + ["Pm"], writes=[pbk])
                    S.op("act", lambda e, k2=k2, pb=pb: e.activation(out=xin[:, 2 * k2:2 * k2 + 2, :], in_=pb.rearrange("p (a b) -> p a b", a=2), func=AF.Copy),
                         reads=[pbk], writes=["xin"])
                for fb in range(4):
                    wg, wgk = wR.next()
                    wg = wg.rearrange("p (k f) -> p k f", k=8)
                    S.dma("pool", lambda e, ex=ex, fb=fb, wg=wg: e.dma_start(out=wg, in_=wg_d[ex].rearrange("(k p) f -> p k f", p=128)[:, :, fb * 512:(fb + 1) * 512]), writes=[wgk])
                    wu, wuk = wR.next()
                    wu = wu.rearrange("p (k f) -> p k f", k=8)
                    S.dma("pool", lambda e, ex=ex, fb=fb, wu=wu: e.dma_start(out=wu, in_=wu_d[ex].rearrange("(k p) f -> p k f", p=128)[:, :, fb * 512:(fb + 1) * 512]), writes=[wuk])
                    for fc in range(4):
                        f = fb * 4 + fc
                        pb, pbk = fR2.next()
                        for k in range(8):
                            S.op("pe", lambda e, k=k, fc=fc, pb=pb, wg=wg: e.matmul(pb[:, 0:CAP], lhsT=wg[:, k, fc * 128:(fc + 1) * 128], rhs=xin[:, k, :], start=(k == 0), stop=(k == 7)),
                                 reads=[wgk, "xin"], writes=[pbk])
                        for k in range(8):
                            S.op("pe", lambda e, k=k, fc=fc, pb=pb, wu=wu: e.matmul(pb[:, CAP:2 * CAP], lhsT=wu[:, k, fc * 128:(fc + 1) * 128], rhs=xin[:, k, :], start=(k == 0), stop=(k == 7)),
                                 reads=[wuk, "xin"], writes=[pbk])
                        sg, sgk = sgR.next()
                        S.op("act", lambda e, pb=pb, sg=sg: e.activation(out=sg, in_=pb[:, 0:CAP], func=AF.Silu), reads=[pbk], writes=[sgk])
                        S.op("dve", lambda e, f=f, pb=pb, sg=sg: e.tensor_tensor(out=hid[:, f, :], in0=pb[:, CAP:2 * CAP], in1=sg, op=ALU.mult), reads=[pbk, sgk], writes=["hid"])
                for fb in range(4):
                    wd, wdk = wR.next()
                    wd = wd.rearrange("p (k n) -> p k n", k=4)
                    S.dma("pool", lambda e, ex=ex, fb=fb, wd=wd: e.dma_start(out=wd, in_=wd_d[ex].rearrange("(k p) n -> p k n", p=128)[:, fb * 4:(fb + 1) * 4, :]), writes=[wdk])
                    for fc in range(4):
                        f = fb * 4 + fc
                        for ct in range(2):
                            for dh in range(2):
                                bank = 4 + ct * 2 + dh
                                S.op("pe", lambda e, f=f, fc=fc, ct=ct, dh=dh, bank=bank, wd=wd: e.matmul(PS[bank], lhsT=hid[:, f, ct * 128:(ct + 1) * 128],
                                                                                                          rhs=wd[:, fc, dh * 512:(dh + 1) * 512], start=(f == 0), stop=(f == 15)),
                                     reads=["hid", wdk], writes=[PB(bank)])
                for ct in range(2):
                    for dh in range(2):
                        bank = 4 + ct * 2 + dh
                        if dh == 0:
                            S.op("act", lambda e, ct=ct, dh=dh, bank=bank: e.activation(out=yex[:, ct, dh * 512:(dh + 1) * 512], in_=PS[bank], func=AF.Copy),
                                 reads=[PB(bank)], writes=["yex"])
                        else:
                            S.op("dve", lambda e, ct=ct, dh=dh, bank=bank: e.tensor_copy(out=yex[:, ct, dh * 512:(dh + 1) * 512], in_=PS[bank]), reads=[PB(bank)], writes=["yex"])
                for i in range(NT):
                    for dh in range(2):
                        pb, pbk = fR2.next()
                        for ch in range(2):
                            S.op("pe", lambda e, i=i, dh=dh, ch=ch, pb=pb: e.matmul(pb, lhsT=PTm[:, ch, i * 128:(i + 1) * 128], rhs=yex[:, ch, dh * 512:(dh + 1) * 512],
                                                                                    start=(ch == 0), stop=(ch == 1)),
                                 reads=["PTm", "yex"], writes=[pbk])
                        if ex == 0:
                            S.op("dve", lambda e, i=i, dh=dh, pb=pb, ex=ex: e.tensor_scalar(out=yacc[:, i, dh * 512:(dh + 1) * 512], in0=pb, scalar1=afft[:, i, ex:ex + 1],
                                                                                            scalar2=None, op0=ALU.mult),
                                 reads=[pbk], writes=[("yacc", i, dh)])
                        else:
                            S.op("dve", lambda e, i=i, dh=dh, pb=pb, ex=ex: e.scalar_tensor_tensor(out=yacc[:, i, dh * 512:(dh + 1) * 512], in0=pb, scalar=afft[:, i, ex:ex + 1],
                                                                                                   in1=yacc[:, i, dh * 512:(dh + 1) * 512], op0=ALU.mult, op1=ALU.add),
                                 reads=[pbk, ("yacc", i, dh)], writes=[("yacc", i, dh)])
            S.barrier()
            A.release(y_mark)
            g2bc = A.alloc("g2bc", [128, D], F32)
            xt = A.alloc("xt", [128, D], F32)
            ot = A.alloc("ot", [128, D], F32)
            S.dma("sp", lambda e: e.dma_start(out=g2bc, in_=modrow_d[b:b + 1, 3072:4096].partition_broadcast(128)), reads=["modrow_d"], writes=["g2bc"])
            for i in range(NT):
                ts_ = slice(i * 128, (i + 1) * 128)
                S.dma("sp", lambda e, ts_=ts_: e.dma_start(out=xt, in_=out_d[b, ts_, :]), reads=[("outd", b, i)], writes=["xt"])
                S.op("dve", lambda e, i=i: e.tensor_tensor(out=ot, in0=yacc[:, i, :], in1=g2bc, op=ALU.mult), reads=[("yacc", i, 0), ("yacc", i, 1), "g2bc"], writes=["ot"])
                S.op("pool", lambda e: e.tensor_tensor(out=ot, in0=ot, in1=xt, op=ALU.add), reads=["ot", "xt"], writes=["ot"])
                S.dma("sp", lambda e, ts_=ts_: e.dma_start(out=out_d[b, ts_, :], in_=ot), reads=["ot", ("outd", b, i)], writes=[("outd", b, i)])
            S.barrier()

        S.barrier()
        print("arena peak bytes", A.peak, "instr counts", {e: len(v) for e, v in S.prog.items()})
        with nc.Block() as block:
            S.emit(block)
    return nc


def _t5_bucket(rel):
    half, exact = 16, 8
    n = np.abs(rel)
    log_ratio = np.log(np.maximum(n, 1).astype(np.float32) / exact) / math.log(1024 / exact)
    large = np.minimum(exact + (log_ratio * (half - exact)).astype(np.int32), half - 1)
    return np.where(rel > 0, half, 0) + np.where(n < exact, n, large)


def _consts():
    c = {}
    c["c_ident"] = np.eye(128, dtype=np.float32)
    c["c_jrev"] = np.ascontiguousarray(np.eye(128, dtype=np.float32)[::-1])
    selab = np.zeros((128, 2, 128), np.float32)
    for m in range(64):
        selab[m + 64, 0, m] = 1.0
        selab[m, 1, m + 64] = 1.0
    c["c_selab"] = selab
    x = np.arange(896)[None, :] - 384
    kp = np.arange(128)[:, None]
    c["c_maskF"] = np.where(x >= kp, 0.0, NEG).astype(np.float32)
    c["c_maskB"] = np.where(x <= kp, 0.0, NEG).astype(np.float32)
    c["c_iotaf"] = np.broadcast_to(np.arange(256, dtype=np.float32)[None, :], (128, 256)).copy()
    c["c_iotap"] = np.stack([np.arange(128, dtype=np.float32), np.arange(128, dtype=np.float32) + 128], axis=1)
    selfb = np.zeros((16, 8, 128), np.float32)
    for h in range(4):
        selfb[4 + h, h, :] = -1.0
        selfb[12 + h, 4 + h, :] = 1.0
    c["c_selfb"] = selfb
    sele = np.zeros((16, 16, 128), np.float32)
    for e in range(16):
        sele[e, e, :] = 1.0
    c["c_sele"] = sele
    sel2 = np.zeros((2, 2, 128), np.float32)
    sel2[0, 0, :] = 1.0
    sel2[1, 1, :] = 1.0
    c["c_sel2"] = sel2
    comb = np.zeros((16, 3, 8), np.float32)
    for h in range(4):
        comb[h, 0, h] = 1.0
        comb[4 + h, 1, h] = 1.0
        comb[8 + h, 0, 4 + h] = 1.0
        comb[12 + h, 1, 4 + h] = -1.0
        comb[12 + h, 2, 4 + h] = 1.0
    c["c_comb"] = comb
    c["c_ones128"] = np.full((128, 128), 1.0 / 128.0, np.float32)
    blk = np.zeros((128, 128), np.float32)
    blk[0:64, 0:64] = 1.0 / 64.0
    blk[64:128, 64:128] = 1.0 / 64.0
    c["c_blk64"] = blk
    oh = np.zeros((32, 3, 384), np.float32)
    for p, d in enumerate((1, 4, 16)):
        for y in range(0, 129):
            rel = 64 - y
            bkt = int(_t5_bucket(np.array(rel * d)))
            oh[bkt, p, 127 + y] = 1.0
    c["c_onehot"] = oh
    return c


_NC_CACHE = {}


def _blockdiag(wblk):
    out = np.zeros((4, 128, 128), np.float32)
    for h in range(4):
        for g in range(32):
            out[h, 4 * g:4 * g + 4, 4 * g:4 * g + 4] = wblk[32 * h + g]
    return out


def make_in_maps(inputs, n_cores=8):
    f = lambda a: np.ascontiguousarray(np.asarray(a, dtype=np.float32))
    x = f(inputs["x"]); c = f(inputs["c"])
    shared = {}
    shared["w_ada"] = f(inputs["w_ada"][0])
    shared["b_ada_fm"] = f(inputs["b_ada"][0].reshape(48, 128).T)
    shared["b_ada_row"] = f(inputs["b_ada"][0].reshape(1, 6 * D))
    shared["g1_fm"] = f(inputs["norm1_g"][0].reshape(8, 128).T)
    shared["g2_row"] = f(inputs["norm2_g"][0].reshape(1, D))
    shared["w_in"] = f(inputs["w_in"][0])
    shared["convw_fm"] = f(np.transpose(inputs["conv_w"][0].reshape(5, 4, 128), (2, 1, 0)))
    shared["convb_fm"] = f(inputs["conv_b"][0].reshape(4, 128).T)
    shared["mng_fm"] = f(inputs["mlstm_norm_g"][0].reshape(4, 128).T)
    shared["mskip_fm"] = f(inputs["mlstm_skip"][0].reshape(4, 128).T)
    shared["wq_bd"] = _blockdiag(np.asarray(inputs["w_q_blk"][0]))
    shared["wk_bd"] = _blockdiag(np.asarray(inputs["w_k_blk"][0]))
    shared["wv_bd"] = _blockdiag(np.asarray(inputs["w_v_blk"][0]))
    bi = np.asarray(inputs["b_igate"][0]); bf = np.asarray(inputs["b_fgate"][0])
    shared["gate_bias"] = f(np.concatenate([bi[0], bf[0], bi[1], bf[1]]).reshape(16, 1))
    shared["gq"] = f(np.tile(np.asarray(inputs["q_norm_g"][0]), 2).reshape(128, 1))
    shared["gk"] = f(np.tile(np.asarray(inputs["k_norm_g"][0]), 2).reshape(128, 1))
    shared["rel_bias"] = f(inputs["rel_bias"])
    shared["w_out"] = f(inputs["w_out"][0])
    shared["wr_fm"] = f(np.transpose(np.asarray(inputs["w_router"][0]).reshape(8, 128, 16), (1, 0, 2)))
    shared["b_router"] = f(inputs["b_router"][0].reshape(1, 16))
    shared["w_gate"] = f(inputs["w_gate"][0])
    shared["w_up"] = f(inputs["w_up"][0])
    shared["w_down"] = f(inputs["w_down"][0])
    shared.update(_consts())
    maps = []
    for i in range(n_cores):
        m = dict(shared)
        m["x"] = np.ascontiguousarray(x[2 * i:2 * i + 2])
        cc = c[2 * i:2 * i + 2]
        m["cT"] = np.ascontiguousarray(np.transpose(cc.reshape(2, 8, 128), (2, 1, 0)))
        maps.append(m)
    return maps


def kernel(**inputs):
    if "nc" not in _NC_CACHE:
        _NC_CACHE["nc"] = build_program()
    nc = _NC_CACHE["nc"]
    maps = make_in_maps(inputs, 8)
    res = run_bass_kernel_spmd(nc, maps, core_ids=list(range(8)))
    out = np.concatenate([np.asarray(r["out"]) for r in res.results], axis=0)
    return out.astype(np.float32)
```

```python
import math
from contextlib import ExitStack
import numpy as np
import concourse.bass as bass
import concourse.mybir as mybir
from concourse.bass_utils import run_bass_kernel_spmd

F32 = mybir.dt.float32
BF16 = mybir.dt.bfloat16
AF = mybir.ActivationFunctionType
ALU = mybir.AluOpType
AX = mybir.AxisListType

S_LEN = 2048
D = 1024
NT = 16
NEXP = 16
CAP = 256
DFF = 2048
EPS = 1e-6
NEG = -30000.0
SAME_ENGINE_SYNC = True
N_DMA_SEMS = 24


class _Rec:
    def __init__(self):
        self.call = None

    def __getattr__(self, name):
        def f(*a, **k):
            self.call = (name, a, k)
            return self
        return f


def _bind(fn):
    rec = _Rec()
    fn(rec)
    assert rec.call is not None
    return rec.call


class Sched:
    ENGS = ["pe", "act", "dve", "pool", "sp"]

    def __init__(self, sems):
        self.prog = {e: [] for e in self.ENGS}
        self.cnt = {}
        self.res = {}
        self.waited = {e: {} for e in self.ENGS}
        self.sems = sems
        self.eng_sem = {e: "c_" + e for e in self.ENGS}
        for e in self.ENGS:
            self.cnt["c_" + e] = 0
        self.dma_names = ["d%d" % i for i in range(N_DMA_SEMS)]
        for n in self.dma_names:
            self.cnt[n] = 0
        self.dma_rr = 0
        self.dma_rr_pool = 0

    def _deps(self, reads, writes):
        deps = {}

        def add(tok):
            if tok is None:
                return
            s, v = tok
            if deps.get(s, 0) < v:
                deps[s] = v
        for r in reads:
            st = self.res.get(r)
            if st is not None:
                add(st["w"])
        for w in writes:
            st = self.res.get(w)
            if st is not None:
                add(st["w"])
                for s, v in st["r"].items():
                    add((s, v))
        return deps

    def _commit(self, tok, reads, writes):
        s, v = tok
        for r in reads:
            st = self.res.setdefault(r, {"w": None, "r": {}})
            if st["r"].get(s, 0) < v:
                st["r"][s] = v
        for w in writes:
            self.res[w] = {"w": tok, "r": {}}

    def op(self, eng, fn, reads=(), writes=()):
        deps = self._deps(reads, writes)
        own = self.eng_sem[eng]
        waits = []
        for s, v in deps.items():
            if s == own and (eng == "pe" or not SAME_ENGINE_SYNC):
                continue
            if self.waited[eng].get(s, 0) >= v:
                continue
            self.waited[eng][s] = v
            waits.append((s, v))
        self.cnt[own] += 1
        tok = (own, self.cnt[own])
        self.prog[eng].append((_bind(fn), waits, (own, 1)))
        self._commit(tok, reads, writes)

    def dma(self, eng, fn, reads=(), writes=()):
        deps = self._deps(reads, writes)
        half = len(self.dma_names) // 2
        if eng == "pool":
            name = self.dma_names[half + self.dma_rr_pool % half]
            self.dma_rr_pool += 1
        else:
            name = self.dma_names[self.dma_rr % half]
            self.dma_rr += 1
        prev = self.cnt[name]
        if prev > 0 and deps.get(name, 0) < prev:
            deps[name] = prev
        waits = []
        for s, v in deps.items():
            if self.waited[eng].get(s, 0) >= v:
                continue
            self.waited[eng][s] = v
            waits.append((s, v))
        self.cnt[name] += 16
        tok = (name, self.cnt[name])
        self.prog[eng].append((_bind(fn), waits, (name, 16)))
        self._commit(tok, reads, writes)
        return tok

    def barrier(self):
        for e in self.ENGS:
            waits = []
            for s, v in self.cnt.items():
                if v == 0:
                    continue
                if self.waited[e].get(s, 0) >= v:
                    continue
                self.waited[e][s] = v
                waits.append((s, v))
            if waits:
                self.prog[e].append((None, waits, None))

    def emit(self, block):
        sems = self.sems

        def mk(engname):
            def body(e):
                for fn, waits, inc in self.prog[engname]:
                    for s, v in waits:
                        e.wait_ge(sems[s], v)
                    if fn is not None:
                        name, a, k = fn
                        ins = getattr(e, name)(*a, **k)
                        ins.then_inc(sems[inc[0]], inc[1])
            return body
        block.tensor(mk("pe"))
        block.scalar(mk("act"))
        block.vector(mk("dve"))
        block.gpsimd(mk("pool"))
        block.sync(mk("sp"))


class Arena:
    def __init__(self, ar, nbytes):
        self.ar = ar
        self.top = 0
        self.nbytes = nbytes
        self.limit = nbytes
        self.gen = 0
        self.peak = 0

    def alloc(self, name, shape, dt, parts=128):
        esz = 2 if dt == BF16 else 4
        n = 1
        for s in shape[1:]:
            n *= s
        nb = (n * esz + 31) // 32 * 32
        off = self.top
        self.top += nb
        self.peak = max(self.peak, self.top)
        assert self.top <= self.limit, (name, self.top, self.limit)
        v = self.ar[:, off // 4: (off + nb) // 4]
        if dt == BF16:
            v = v.bitcast(BF16)
        v = v[:, 0:n]
        if len(shape) == 3:
            v = v.rearrange("p (a b) -> p a b", a=shape[1])
        elif len(shape) == 4:
            v = v.rearrange("p (a b c) -> p a b c", a=shape[1], b=shape[2])
        if shape[0] < 128:
            v = v[0:shape[0]]
        self.gen += 1
        return v

    def mark(self):
        return self.top

    def release(self, m):
        self.top = m


def build_program(debug=None, nseq=2, max_phase=9):
    nc = bass.Bass("TRN2", target_bir_lowering=False)
    dr = {}

    def din(name, shape, dt=F32):
        dr[name] = nc.dram_tensor(name, list(shape), dt, kind="ExternalInput").ap()
        return dr[name]
    x_d = din("x", [2, S_LEN, D])
    cT_d = din("cT", [128, 8, 2])
    wada_d = din("w_ada", [D, 6 * D])
    bada_fm_d = din("b_ada_fm", [128, 48])
    bada_row_d = din("b_ada_row", [1, 6 * D])
    g1_d = din("g1_fm", [128, 8])
    g2_d = din("g2_row", [1, D])
    win_d = din("w_in", [D, 2576])
    convw_d = din("convw_fm", [128, 4, 5])
    convb_d = din("convb_fm", [128, 4])
    mng_d = din("mng_fm", [128, 4])
    msk_d = din("mskip_fm", [128, 4])
    wq_d = din("wq_bd", [4, 128, 128])
    wk_d = din("wk_bd", [4, 128, 128])
    wv_d = din("wv_bd", [4, 128, 128])
    gb_d = din("gate_bias", [16, 1])
    gq_d = din("gq", [128, 1])
    gk_d = din("gk", [128, 1])
    relb_d = din("rel_bias", [32, 8])
    wout_d = din("w_out", [D, D])
    wr_d = din("wr_fm", [128, 8, 16])
    br_d = din("b_router", [1, 16])
    wg_d = din("w_gate", [NEXP, D, DFF])
    wu_d = din("w_up", [NEXP, D, DFF])
    wd_d = din("w_down", [NEXP, DFF, D])
    ident_d = din("c_ident", [128, 128])
    jrev_d = din("c_jrev", [128, 128])
    selab_d = din("c_selab", [128, 2, 128])
    maskF_d = din("c_maskF", [128, 896])
    maskB_d = din("c_maskB", [128, 896])
    iotaf_d = din("c_iotaf", [128, 256])
    iotap_d = din("c_iotap", [128, 2])
    selfb_d = din("c_selfb", [16, 8, 128])
    sele_d = din("c_sele", [16, 16, 128])
    sel2_d = din("c_sel2", [2, 2, 128])
    comb_d = din("c_comb", [16, 3, 8])
    ones128_d = din("c_ones128", [128, 128])
    blk64_d = din("c_blk64", [128, 128])
    onehot_d = din("c_onehot", [32, 3, 384])
    out_d = nc.dram_tensor("out", [2, S_LEN, D], F32, kind="ExternalOutput").ap()
    modrow_d = nc.dram_tensor("modrow_s", [2, 4 * D], F32, kind="Internal").ap()
    ftab_d = nc.dram_tensor("ftab_s", [8, 3, 384], F32, kind="Internal").ap()
    dbg = {}
    if debug:
        for nm, shp in debug.items():
            dbg[nm] = nc.dram_tensor("dbg_" + nm, list(shp), F32, kind="ExternalOutput").ap()

    ARENA_BYTES = 206 * 1024
    with ExitStack() as es:
        arena_t = es.enter_context(nc.sbuf_tensor("arena", [128, ARENA_BYTES // 4], F32))
        PSD = [es.enter_context(nc.psum_tensor("psd%d" % i, [128, 1024], F32))[:] for i in range(4)]
        PS = []
        for i in range(4):
            PS.append(PSD[i][:, 0:512])
            PS.append(PSD[i][:, 512:1024])
        names = ["c_pe", "c_act", "c_dve", "c_pool", "c_sp"] + ["d%d" % i for i in range(N_DMA_SEMS)]
        sems = {n: es.enter_context(nc.semaphore(n)) for n in names}
        S = Sched(sems)
        A = Arena(arena_t, ARENA_BYTES)
        uid = [0]

        def R(name):
            uid[0] += 1
            return "%s#%d" % (name, uid[0])

        def PB(i):
            return ("ps", i)

        class Ring:
            def __init__(self, name, n, shape=None, dt=None, banks=None):
                self.n = n
                self.i = 0
                if banks is not None:
                    self.items = [(PS[bk], PB(bk)) for bk in banks]
                    self.n = len(banks)
                else:
                    self.items = [(A.alloc(name + str(j), shape, dt), R(name + str(j))) for j in range(n)]

            def next(self):
                it = self.items[self.i % self.n]
                self.i += 1
                return it

        def load(eng, dst, src, key, reads=()):
            S.dma(eng, lambda e: e.dma_start(out=dst, in_=src), reads=list(reads), writes=[key])

        def dump(name, src, key):
            if name in dbg:
                S.dma("sp", lambda e: e.dma_start(out=dbg[name], in_=src), reads=[key], writes=["dbg_" + name])

        ident = A.alloc("ident", [128, 128], F32)
        identb = A.alloc("identb", [128, 128], BF16)
        jrev = A.alloc("jrev", [128, 128], F32)
        selab = A.alloc("selab", [128, 2, 128], F32)
        onesb = A.alloc("onesb", [128, 128], BF16)
        ones128 = A.alloc("ones128", [128, 128], F32)
        blk64 = A.alloc("blk64", [128, 128], F32)
        maskF = A.alloc("maskF", [128, 896], F32)
        maskB = A.alloc("maskB", [128, 896], F32)
        iotaf = A.alloc("iotaf", [128, 256], F32)
        iotap = A.alloc("iotap", [128, 2], F32)
        selfb = A.alloc("selfb", [16, 8, 128], F32)
        sele = A.alloc("sele", [16, 16, 128], F32)
        sel2 = A.alloc("sel2", [2, 2, 128], F32)
        comb = A.alloc("comb", [16, 3, 8], F32)
        g1 = A.alloc("g1", [128, 8], F32)
        convw = A.alloc("convw", [128, 4, 5], F32)
        convb = A.alloc("convb", [128, 4], F32)
        mng = A.alloc("mng", [128, 4], F32)
        msk = A.alloc("msk", [128, 4], F32)
        gbias = A.alloc("gbias", [16, 1], F32)
        gq = A.alloc("gq", [128, 1], F32)
        gk = A.alloc("gk", [128, 1], F32)
        wr = A.alloc("wr", [128, 8, 16], F32)
        brbc = A.alloc("brbc", [128, 16], F32)
        wqb = A.alloc("wqb", [128, 4, 128], BF16)
        wkb = A.alloc("wkb", [128, 4, 128], BF16)
        wvb = A.alloc("wvb", [128, 4, 128], BF16)
        A1 = A.alloc("A1", [128, 2, 8], F32)
        B1 = A.alloc("B1", [128, 2, 8], F32)
        CONST = "const"
        for dst, src in [(ident, ident_d), (jrev, jrev_d), (selab, selab_d), (ones128, ones128_d), (blk64, blk64_d), (maskF, maskF_d),
                         (maskB, maskB_d), (iotaf, iotaf_d), (iotap, iotap_d), (selfb, selfb_d),
                         (sele, sele_d), (sel2, sel2_d), (comb, comb_d), (g1, g1_d), (convw, convw_d),
                         (convb, convb_d), (mng, mng_d), (msk, msk_d), (gbias, gb_d), (gq, gq_d),
                         (gk, gk_d), (wr, wr_d)]:
            S.dma("sp", lambda e, dst=dst, src=src: e.dma_start(out=dst, in_=src), writes=[R("cl")])
        S.dma("sp", lambda e: e.dma_start(out=brbc, in_=br_d.partition_broadcast(128)), writes=[R("cl")])
        for dst, src in [(wqb, wq_d), (wkb, wk_d), (wvb, wv_d)]:
            S.dma("pool", lambda e, dst=dst, src=src: e.dma_start(out=dst, in_=src.rearrange("h p n -> p h n")),
                  writes=[R("cl")])
        S.barrier()
        S.op("act", lambda e: e.activation(out=identb, in_=ident, func=AF.Copy), writes=[R("cl")])
        S.op("pool", lambda e: e.memset(onesb, 1.0), writes=[R("cl")])
        S.barrier()

        m0 = A.mark()
        sc = A.alloc("sc", [128, 8, 2], F32)
        wpiece = A.alloc("wpiece", [128, 8, 1024], F32)
        bfm = A.alloc("bfm", [128, 48], F32)
        brow = A.alloc("brow", [2, 4096], F32)
        mrow = A.alloc("mrow", [2, 4096], F32)
        modfm = A.alloc("modfm", [128, 2, 8, 2], F32)
        load("sp", sc, cT_d, "sc")
        load("sp", bfm, bada_fm_d, "bfm")
        load("sp", brow[0:1, :], bada_row_d[0:1, 2048:6144], "brow0")
        load("sp", brow[1:2, :], bada_row_d[0:1, 2048:6144], "brow1")
        S.op("act", lambda e: e.activation(out=sc, in_=sc, func=AF.Silu), reads=["sc"], writes=["sc"])
        wada_v = wada_d.rearrange("(k p) n -> p k n", p=128)
        for piece in range(6):
            for k in range(8):
                load("sp", wpiece[:, k, :], wada_v[:, k, piece * 1024:(piece + 1) * 1024], ("wpiece", k))
            wp_keys = [("wpiece", k) for k in range(8)]
            if piece < 2:
                for j in range(8):
                    for k in range(8):
                        S.op("pe", lambda e, j=j, k=k: e.matmul(PS[0][:, 2 * j:2 * j + 2], lhsT=wpiece[:, k, j * 128:(j + 1) * 128],
                                                                 rhs=sc[:, k, :], start=(k == 0), stop=(k == 7)),
                             reads=wp_keys + ["sc"], writes=[PB(0)])
                S.op("dve", lambda e, piece=piece: e.tensor_copy(out=modfm[:, piece, :, :], in_=PS[0][:, 0:16].rearrange("p (j b) -> p j b", b=2)),
                     reads=[PB(0)], writes=[("modfm", piece)])
            else:
                for half in range(2):
                    for k in range(8):
                        S.op("pe", lambda e, half=half, k=k: e.matmul(PS[1][0:2, :], lhsT=sc[:, k, :],
                                                                       rhs=wpiece[:, k, half * 512:(half + 1) * 512],
                                                                       start=(k == 0), stop=(k == 7)),
                             reads=wp_keys + ["sc"], writes=[PB(1)])
                    c0 = (piece - 2) * 1024 + half * 512
                    S.op("dve", lambda e, c0=c0: e.tensor_tensor(out=mrow[:, c0:c0 + 512], in0=PS[1][0:2, :], in1=brow[:, c0:c0 + 512], op=ALU.add),
                         reads=[PB(1), "brow0", "brow1"], writes=[("mrow", c0)])
        for b in range(2):
            S.op("dve", lambda e, b=b: e.tensor_tensor(out=B1[:, b, :], in0=modfm[:, 0, :, b], in1=bfm[:, 0:8], op=ALU.add),
                 reads=[("modfm", 0), "bfm"], writes=[("B1", b)])
            S.op("dve", lambda e, b=b: e.tensor_tensor(out=A1[:, b, :], in0=modfm[:, 1, :, b], in1=bfm[:, 8:16], op=ALU.add),
                 reads=[("modfm", 1), "bfm"], writes=[("A1", b)])
            S.op("dve", lambda e, b=b: e.scalar_tensor_tensor(out=A1[:, b, :], in0=A1[:, b, :], scalar=1.0, in1=g1, op0=ALU.add, op1=ALU.mult),
                 reads=[("A1", b)], writes=[("A1", b)])
        S.dma("sp", lambda e: e.dma_start(out=modrow_d, in_=mrow), reads=[("mrow", c) for c in range(0, 4096, 512)], writes=["modrow_d"])
        S.barrier()
        A.release(m0)

        m0 = A.mark()
        relb = A.alloc("relb", [32, 8], F32)
        onehot = A.alloc("onehot", [32, 3, 384], F32)
        ftab = A.alloc("ftab", [8, 3, 384], F32)
        load("sp", relb, relb_d, "relb")
        load("sp", onehot, onehot_d, "onehot")
        for p in range(3):
            S.op("pe", lambda e, p=p: e.matmul(PS[0][0:8, 0:384], lhsT=relb, rhs=onehot[:, p, :], start=True, stop=True),
                 reads=["relb", "onehot"], writes=[PB(0)])
            S.op("act", lambda e, p=p: e.activation(out=ftab[:, p, :], in_=PS[0][0:8, 0:384], func=AF.Exp), reads=[PB(0)], writes=[("ftab", p)])
        for p in range(3):
            S.op("pe", lambda e, p=p: e.matmul(PS[1][0:8, 0:384], lhsT=ones128[0:32, 0:8], rhs=onehot[:, p, :], start=True, stop=True),
                 reads=["onehot"], writes=[PB(1)])
            S.op("dve", lambda e, p=p: e.scalar_tensor_tensor(out=ftab[:, p, :], in0=PS[1][0:8, 0:384], scalar=128.0, in1=ftab[:, p, :],
                                                                op0=ALU.mult, op1=ALU.mult),
                 reads=[PB(1), ("ftab", p)], writes=[("ftab", p)])
        S.dma("sp", lambda e: e.dma_start(out=ftab_d, in_=ftab), reads=[("ftab", p) for p in range(3)], writes=["ftab_d"])
        S.barrier()
        A.release(m0)

        pers_mark = A.mark()
        win_v = win_d.rearrange("(k p) n -> p k n", p=128)
        wout_v = wout_d.rearrange("(k p) n -> p k n", p=128)

        for b in range(nseq):
            A.release(pers_mark)
            A.limit = ARENA_BYTES - 8 * S_LEN * 2
            catT = arena_t[:, (ARENA_BYTES - 8 * S_LEN * 2) // 4: ARENA_BYTES // 4].bitcast(BF16).rearrange("p (a b) -> p a b", a=8)
            hT = A.alloc("hT", [128, 8, S_LEN], BF16)
            seq_mark = A.mark()
            xt = A.alloc("xt", [128, D], F32)
            xs = A.alloc("xs", [128, D], F32)
            junk = A.alloc("junk", [128, D], F32)
            st1 = A.alloc("st1", [128, 4], F32)
            for i in range(NT):
                load("sp", xt, x_d[b, i * 128:(i + 1) * 128, :], "xt")
                S.op("act", lambda e: e.activation(out=junk, in_=xt, func=AF.Square, accum_out=st1[:, 0:1]), reads=["xt"], writes=["junk", "st1"])
                S.op("act", lambda e: e.activation(out=st1[:, 1:2], in_=st1[:, 0:1], func=AF.Ln, scale=1.0 / D, bias=EPS), reads=["st1"], writes=["st1"])
                S.op("act", lambda e: e.activation(out=st1[:, 2:3], in_=st1[:, 1:2], func=AF.Exp, scale=-0.5), reads=["st1"], writes=["st1"])
                S.op("dve", lambda e: e.tensor_scalar(out=xs, in0=xt, scalar1=st1[:, 2:3], scalar2=None, op0=ALU.mult), reads=["xt", "st1"], writes=["xs"])
                for k in range(8):
                    bank = k // 4
                    S.op("pe", lambda e, k=k, bank=bank: e.matmul(PS[bank][:, (k % 4) * 128:(k % 4 + 1) * 128], lhsT=xs[:, k * 128:(k + 1) * 128],
                                                                   rhs=ident, start=True, stop=True),
                         reads=["xs"], writes=[PB(bank)])
                for k in range(8):
                    bank = k // 4
                    eng = "dve" if bank == 0 else "act"
                    if eng == "dve":
                        S.op("dve", lambda e, k=k, bank=bank, i=i: e.tensor_scalar(out=hT[:, k, i * 128:(i + 1) * 128],
                                                                                   in0=PS[bank][:, (k % 4) * 128:(k % 4 + 1) * 128],
                                                                                   scalar1=A1[:, b, k:k + 1], scalar2=B1[:, b, k:k + 1],
                                                                                   op0=ALU.mult, op1=ALU.add),
                             reads=[PB(bank), ("A1", b), ("B1", b)], writes=[("hT", i)])
                    else:
                        S.op("act", lambda e, k=k, bank=bank, i=i: e.activation(out=hT[:, k, i * 128:(i + 1) * 128],
                                                                                in_=PS[bank][:, (k % 4) * 128:(k % 4 + 1) * 128],
                                                                                func=AF.Identity, scale=A1[:, b, k:k + 1], bias=B1[:, b, k:k + 1]),
                             reads=[PB(bank), ("A1", b), ("B1", b)], writes=[("hT", i)])
            hT_keys = [("hT", i) for i in range(NT)]
            if b == 0 and "hT" in dbg:
                S.dma("pool", lambda e: e.dma_start(out=dbg["hT"], in_=hT), reads=hT_keys, writes=["dbg_hT"])
            S.barrier()
            A.release(seq_mark)

            Cg = A.alloc("Cg", [16, S_LEN], F32)
            Dg = A.alloc("Dg", [16, S_LEN], F32)
            utok = A.alloc("utok", [128, 128], F32)
            g_mark = A.mark()
            Zg = A.alloc("Zg", [16, S_LEN], F32)
            Lg = A.alloc("Lg", [16, S_LEN], F32)
            onesr = A.alloc("onesr", [16, S_LEN], F32)
            wgt = A.alloc("wgt", [128, 8, 16], BF16)
            S.dma("pool", lambda e: e.dma_start(out=wgt, in_=win_v[:, :, 1024:1040]), writes=["wgt"])
            S.op("pool", lambda e: e.memset(onesr, 1.0), writes=["onesr"])
            for blk in range(4):
                for k in range(8):
                    S.op("pe", lambda e, k=k, blk=blk: e.matmul(PS[0][0:16, :], lhsT=wgt[:, k, :], rhs=hT[:, k, blk * 512:(blk + 1) * 512],
                                                                 start=(k == 0), stop=(k == 7)),
                         reads=["wgt"] + hT_keys, writes=[PB(0)])
                S.op("dve", lambda e, blk=blk: e.tensor_scalar(out=Zg[:, blk * 512:(blk + 1) * 512], in0=PS[0][0:16, :], scalar1=gbias[:, 0:1],
                                                               scalar2=None, op0=ALU.add),
                     reads=[PB(0)], writes=[("Zg", blk)])
            zk = [("Zg", blk) for blk in range(4)]
            S.op("act", lambda e: e.activation(out=Lg, in_=Zg, func=AF.Exp, scale=-1.0), reads=zk, writes=["Lg"])
            S.op("act", lambda e: e.activation(out=Lg, in_=Lg, func=AF.Ln, bias=1.0), reads=["Lg"], writes=["Lg"])
            S.op("dve", lambda e: e.tensor_tensor_scan(out=Cg, data0=onesr, data1=Lg, initial=0.0, op0=ALU.mult, op1=ALU.add),
                 reads=["onesr", "Lg"], writes=["Cg"])
            S.op("dve", lambda e: e.tensor_tensor(out=Dg, in0=Cg, in1=Lg, op=ALU.subtract), reads=["Cg", "Lg"], writes=["Dg"])
            for i in range(NT):
                sl = slice(i * 128, (i + 1) * 128)
                S.op("pe", lambda e, i=i, sl=sl: e.matmul(PS[1][:, i * 8:(i + 1) * 8], lhsT=Zg[:, sl], rhs=comb[:, 0, :], start=True, stop=False),
                     reads=zk, writes=[PB(1)])
                S.op("pe", lambda e, i=i, sl=sl: e.matmul(PS[1][:, i * 8:(i + 1) * 8], lhsT=Cg[:, sl], rhs=comb[:, 1, :], start=False, stop=False),
                     reads=["Cg"], writes=[PB(1)])
                S.op("pe", lambda e, i=i, sl=sl: e.matmul(PS[1][:, i * 8:(i + 1) * 8], lhsT=Lg[:, sl], rhs=comb[:, 2, :], start=False, stop=True),
                     reads=["Lg"], writes=[PB(1)])
            S.op("dve", lambda e: e.tensor_copy(out=utok, in_=PS[1][:, 0:128]), reads=[PB(1)], writes=["utok"])
            if b == 0:
                dump("Cg", Cg, "Cg")
                dump("utok", utok, "utok")
            S.barrier()
            A.release(g_mark)

            m_mark = A.mark()
            wxm = A.alloc("wxm", [128, 8, 128], BF16)
            wop = A.alloc("wop", [128, 8, 128], BF16)
            xmp = A.alloc("xmp", [128, S_LEN + 4], F32)
            sigo = A.alloc("sigo", [128, S_LEN], BF16)
            cacc = A.alloc("cacc", [128, S_LEN], F32)
            xc = A.alloc("xc", [128, S_LEN], BF16)
            xmb = A.alloc("xmb", [128, S_LEN], BF16)
            qT = A.alloc("qT", [128, S_LEN], BF16)
            kT = A.alloc("kT", [128, S_LEN], BF16)
            vtok = A.alloc("vtok", [128, NT, 128], BF16)
            bcR = Ring("bc", 4, [128, 512], F32)
            tmR = Ring("tmpm", 3, [128, 512], F32)
            wtR = Ring("Wt", 4, [128, 512], F32)
            swR = Ring("STw", 4, [128, 512], BF16)
            fR = Ring("fin", 8, [128, 512], F32)
            stR = Ring("st", 0, banks=[4, 5, 6, 7])
            S.op("pool", lambda e: e.memset(xmp, 0.0), writes=["xmp"])
            mpR = Ring("mpR", 0, banks=[0, 1, 2, 3])
            for h in range(4):
                S.dma("pool", lambda e, h=h: e.dma_start(out=wxm, in_=win_v[:, :, h * 128:(h + 1) * 128]), writes=["wxm"])
                S.dma("pool", lambda e, h=h: e.dma_start(out=wop, in_=win_v[:, :, 512 + h * 128:512 + (h + 1) * 128]), writes=["wop"])
                for blk in range(4):
                    bs = slice(blk * 512, (blk + 1) * 512)
                    pa, pak = mpR.next()
                    for k in range(8):
                        S.op("pe", lambda e, k=k, bs=bs, pa=pa: e.matmul(pa, lhsT=wxm[:, k, :], rhs=hT[:, k, bs], start=(k == 0), stop=(k == 7)),
                             reads=["wxm"] + hT_keys, writes=[pak])
                    S.op("dve", lambda e, blk=blk, pa=pa: e.tensor_copy(out=xmp[:, 2 + blk * 512:2 + (blk + 1) * 512], in_=pa),
                         reads=[pak], writes=["xmp"])
                    pb_, pbk_ = mpR.next()
                    for k in range(8):
                        S.op("pe", lambda e, k=k, bs=bs, pb_=pb_: e.matmul(pb_, lhsT=wop[:, k, :], rhs=hT[:, k, bs], start=(k == 0), stop=(k == 7)),
                             reads=["wop"] + hT_keys, writes=[pbk_])
                    S.op("act", lambda e, bs=bs, pb_=pb_: e.activation(out=sigo[:, bs], in_=pb_, func=AF.Sigmoid), reads=[pbk_], writes=["sigo"])
                S.op("dve", lambda e, h=h: e.tensor_scalar(out=cacc, in0=xmp[:, 0:S_LEN], scalar1=convw[:, h, 0:1], scalar2=None, op0=ALU.mult),
                     reads=["xmp"], writes=["cacc"])
                for j in range(1, 5):
                    S.op("dve", lambda e, h=h, j=j: e.scalar_tensor_tensor(out=cacc, in0=xmp[:, j:j + S_LEN], scalar=convw[:, h, j:j + 1], in1=cacc,
                                                                          op0=ALU.mult, op1=ALU.add),
                         reads=["xmp", "cacc"], writes=["cacc"])
                S.op("act", lambda e, h=h: e.activation(out=xc, in_=cacc, func=AF.Silu, bias=convb[:, h:h + 1]), reads=["cacc"], writes=["xc"])
                S.op("act", lambda e: e.activation(out=xmb, in_=xmp[:, 2:2 + S_LEN], func=AF.Copy), reads=["xmp"], writes=["xmb"])
                for blk in range(4):
                    bs = slice(blk * 512, (blk + 1) * 512)
                    pa, pak = mpR.next()
                    S.op("pe", lambda e, h=h, bs=bs, pa=pa: e.matmul(pa, lhsT=wqb[:, h, :], rhs=xc[:, bs], start=True, stop=True), reads=["xc"], writes=[pak])
                    S.op("act", lambda e, bs=bs, pa=pa: e.activation(out=qT[:, bs], in_=pa, func=AF.Copy), reads=[pak], writes=["qT"])
                    pb_, pbk_ = mpR.next()
                    S.op("pe", lambda e, h=h, bs=bs, pb_=pb_: e.matmul(pb_, lhsT=wkb[:, h, :], rhs=xc[:, bs], start=True, stop=True), reads=["xc"], writes=[pbk_])
                    S.op("dve", lambda e, bs=bs, pb_=pb_: e.tensor_scalar(out=kT[:, bs], in0=pb_, scalar1=1.0 / math.sqrt(128.0), scalar2=None, op0=ALU.mult),
                         reads=[pbk_], writes=["kT"])
                for i4 in range(4):
                    for ii in range(4):
                        i = i4 * 4 + ii
                        if ii == 0:
                            pa, pak = mpR.next()
                        S.op("pe", lambda e, h=h, i=i, ii=ii, pa=pa: e.matmul(pa[:, ii * 128:(ii + 1) * 128], lhsT=xmb[:, i * 128:(i + 1) * 128],
                                                                               rhs=wvb[:, h, :], start=True, stop=True),
                             reads=["xmb"], writes=[pak])
                    S.op("act", lambda e, i4=i4, pa=pa: e.activation(out=vtok[:, i4 * 4:(i4 + 1) * 4, :], in_=pa.rearrange("p (a b) -> p a b", a=4), func=AF.Copy),
                         reads=[pak], writes=["vtok"])
                for T in range(4):
                    bs = slice(T * 512, (T + 1) * 512)
                    pb1, pb1k = stR.next()
                    S.op("pe", lambda e, h=h, bs=bs, pb1=pb1: e.matmul(pb1, lhsT=selfb[:, h, :], rhs=Cg[:, bs], start=True, stop=True), reads=["Cg"], writes=[pb1k])
                    bcf, bcfk = bcR.next()
                    S.op("act", lambda e, bcf=bcf, pb1=pb1: e.activation(out=bcf, in_=pb1, func=AF.Copy), reads=[pb1k], writes=[bcfk])
                    pb2, pb2k = stR.next()
                    S.op("pe", lambda e, h=h, bs=bs, pb2=pb2: e.matmul(pb2, lhsT=selfb[:, 4 + h, :], rhs=Dg[:, bs], start=True, stop=True), reads=["Dg"], writes=[pb2k])
                    bcb, bcbk = bcR.next()
                    S.op("act", lambda e, bcb=bcb, pb2=pb2: e.activation(out=bcb, in_=pb2, func=AF.Copy), reads=[pb2k], writes=[bcbk])
                    nf = 4 * T + 4
                    nb = 16 - 4 * T
                    cf = 0
                    cb = 0
                    sts = {}

                    def emit_st(j, bs=bs, sts=sts):
                        st, stk = stR.next()
                        S.op("pe", lambda e, j=j, bs=bs, st=st: e.matmul(st, lhsT=kT[:, j * 128:(j + 1) * 128], rhs=qT[:, bs], start=True, stop=True),
                             reads=["kT", "qT"], writes=[stk])
                        sts[j] = (st, stk)
                    LOOK = 2
                    for j in range(LOOK):
                        emit_st(j)
                    for j in range(NT):
                        fwd = j <= 4 * T + 3
                        bwd = j >= 4 * T
                        if j + LOOK < NT:
                            emit_st(j + LOOK)
                        st, stk = sts[j]
                        for dirn in (0, 1):
                            if (dirn == 0 and not fwd) or (dirn == 1 and not bwd):
                                continue
                            bc, bck = (bcf, bcfk) if dirn == 0 else (bcb, bcbk)
                            src, srck = bc, bck
                            if 4 * T <= j <= 4 * T + 3:
                                o = (j - 4 * T) * 128
                                mk = maskF if dirn == 0 else maskB
                                tmpm, tmpk = tmR.next()
                                S.op("pool", lambda e, bc=bc, mk=mk, o=o, tmpm=tmpm: e.tensor_tensor(out=tmpm, in0=bc, in1=mk[:, 384 - o:384 - o + 512], op=ALU.add),
                                     reads=[bck], writes=[tmpk])
                                src, srck = tmpm, tmpk
                            ucol = j * 8 + dirn * 4 + h
                            Wt, Wtk = wtR.next()
                            S.op("act", lambda e, src=src, ucol=ucol, Wt=Wt: e.activation(out=Wt, in_=src, func=AF.Exp, bias=utok[:, ucol:ucol + 1]),
                                 reads=[srck, "utok"], writes=[Wtk])
                            STw, STwk = swR.next()
                            S.op("dve", lambda e, st=st, Wt=Wt, STw=STw: e.tensor_tensor(out=STw, in0=st, in1=Wt, op=ALU.mult), reads=[stk, Wtk], writes=[STwk])
                            if dirn == 0:
                                first, last = (cf == 0), (cf == nf - 1)
                                cf += 1
                                nb_, db_ = 0, 1
                            else:
                                first, last = (cb == 0), (cb == nb - 1)
                                cb += 1
                                nb_, db_ = 2, 3
                            S.op("pe", lambda e, j=j, nb_=nb_, first=first, last=last, STw=STw: e.matmul(PS[nb_], lhsT=vtok[:, j, :], rhs=STw, start=first, stop=last),
                                 reads=["vtok", STwk], writes=[PB(nb_)])
                            S.op("pe", lambda e, db_=db_, first=first, last=last, STw=STw: e.matmul(PS[db_], lhsT=onesb, rhs=STw, start=first, stop=last),
                                 reads=[STwk], writes=[PB(db_)])
                    (fa, fak), (fb_, fbk), (fc_, fck), (fd_, fdk) = fR.next(), fR.next(), fR.next(), fR.next()
                    for (nb_, db_, dst, dk) in ((0, 1, fa, fak), (2, 3, fb_, fbk)):
                        S.op("act", lambda e, db_=db_, fd_=fd_: e.activation(out=fd_, in_=PS[db_], func=AF.Copy), reads=[PB(db_)], writes=[fdk])
                        S.op("dve", lambda e, fc_=fc_, fd_=fd_: e.scalar_tensor_tensor(out=fc_, in0=fd_, scalar=-1.0, in1=fd_, op0=ALU.mult, op1=ALU.max),
                             reads=[fdk], writes=[fck])
                        S.op("dve", lambda e, fc_=fc_: e.tensor_scalar(out=fc_, in0=fc_, scalar1=1.0, scalar2=None, op0=ALU.max), reads=[fck], writes=[fck])
                        S.op("act", lambda e, fc_=fc_: e.activation(out=fc_, in_=fc_, func=AF.Ln), reads=[fck], writes=[fck])
                        S.op("act", lambda e, fc_=fc_: e.activation(out=fc_, in_=fc_, func=AF.Exp, scale=-1.0), reads=[fck], writes=[fck])
                        S.op("dve", lambda e, nb_=nb_, dst=dst, fc_=fc_: e.tensor_tensor(out=dst, in0=PS[nb_], in1=fc_, op=ALU.mult), reads=[PB(nb_), fck], writes=[dk])
                    S.op("pool", lambda e, fa=fa, fb_=fb_: e.tensor_tensor(out=fa, in0=fa, in1=fb_, op=ALU.add), reads=[fak, fbk], writes=[fak])
                    S.op("act", lambda e, fa=fa, fd_=fd_: e.activation(out=fd_, in_=fa, func=AF.Square), reads=[fak], writes=[fdk])
                    pb3, pb3k = stR.next()
                    S.op("pe", lambda e, fd_=fd_, pb3=pb3: e.matmul(pb3, lhsT=ones128, rhs=fd_, start=True, stop=True), reads=[fdk], writes=[pb3k])
                    S.op("act", lambda e, fd_=fd_, pb3=pb3: e.activation(out=fd_, in_=pb3, func=AF.Ln, bias=EPS), reads=[pb3k], writes=[fdk])
                    S.op("act", lambda e, fd_=fd_: e.activation(out=fd_, in_=fd_, func=AF.Exp, scale=-0.5), reads=[fdk], writes=[fdk])
                    S.op("dve", lambda e, h=h, fa=fa, fd_=fd_: e.scalar_tensor_tensor(out=fa, in0=fa, scalar=mng[:, h:h + 1], in1=fd_, op0=ALU.mult, op1=ALU.mult),
                         reads=[fak, fdk], writes=[fak])
                    S.op("dve", lambda e, h=h, bs=bs, fa=fa: e.scalar_tensor_tensor(out=fa, in0=xc[:, bs], scalar=msk[:, h:h + 1], in1=fa, op0=ALU.mult, op1=ALU.add),
                         reads=[fak, "xc"], writes=[fak])
                    S.op("dve", lambda e, h=h, bs=bs, fa=fa: e.tensor_tensor(out=catT[:, h, bs], in0=fa, in1=sigo[:, bs], op=ALU.mult),
                         reads=[fak, "sigo"], writes=[("catT", h)])
            S.barrier()
            A.release(m_mark)

            a_mark = A.mark()
            wq3 = A.alloc("wq3", [128, 8, 128], BF16)
            wk3 = A.alloc("wk3", [128, 8, 128], BF16)
            wv3 = A.alloc("wv3", [128, 8, 128], BF16)
            a32R = Ring("a32", 3, [128, 512], F32)
            tqR = Ring("tq", 3, [128, 512], F32)
            prR = Ring("prR", 0, banks=[0, 1])
            nrR = Ring("nrR", 0, banks=[2, 3])
            qn = A.alloc("qn", [128, S_LEN], BF16)
            kn = A.alloc("kn", [128, S_LEN], BF16)
            av = A.alloc("av", [128, S_LEN], BF16)
            qd = A.alloc("qd", [128, S_LEN], BF16)
            kd = A.alloc("kd", [128, S_LEN], BF16)
            avd = A.alloc("avd", [128, S_LEN], BF16)
            VpA = A.alloc("VpA", [128, NT, 128], BF16)
            VpB = A.alloc("VpB", [128, NT, 128], BF16)
            tabs = A.alloc("tabs", [128, 3, 2, 256], BF16)
            recb = A.alloc("recb", [128, 512], F32)
            S.op("pool", lambda e: e.memset(VpA, 1.0), writes=["VpA"])
            S.op("pool", lambda e: e.memset(VpB, 1.0), writes=["VpB"])
            tabsR = A.alloc("tabsR", [128, 3, 2, 256], F32)
            accN = A.alloc("accN", [128, S_LEN], F32)
            accD = A.alloc("accD", [128, S_LEN], F32)
            etR = Ring("Et", 4, [128, 2, 256], BF16)
            ptR = Ring("Pt", 4, [128, 2, 256], BF16)
            stA_items = [(PSD[2], 4, 5), (PSD[3], 6, 7)]
            stA_i = [0]
            for c in range(4):
                for (wt, col0, wkey) in ((wq3, 1040, "wq3"), (wk3, 1552, "wk3"), (wv3, 2064, "wv3")):
                    S.dma("pool", lambda e, wt=wt, col0=col0, c=c: e.dma_start(out=wt, in_=win_v[:, :, col0 + c * 128:col0 + (c + 1) * 128]), writes=[wkey])
                for p in range(3):
                    for hh in range(2):
                        hd = 2 * c + hh
                        src = bass.AP(tensor=ftab_d.tensor, offset=(hd * 3 + p) * 384, ap=[[1, 128], [1, 256]])
                        S.dma("sp", lambda e, p=p, hh=hh, src=src: e.dma_start(out=tabsR[:, p, hh, :], in_=src), reads=["ftab_d"], writes=[("tabsR", p, hh)])
                    for hh in range(2):
                        S.op("pe", lambda e, p=p, hh=hh: e.matmul(PS[0][:, hh * 256:(hh + 1) * 256], lhsT=jrev, rhs=tabsR[:, p, hh, :], start=True, stop=True),
                             reads=[("tabsR", p, hh)], writes=[PB(0)])
                    S.op("dve", lambda e, p=p: e.tensor_copy(out=tabs[:, p, :, :], in_=PS[0].rearrange("p (a b) -> p a b", a=2)), reads=[PB(0)], writes=["tabs"])
                pjobs = []
                for (wt, wkey, dst, dkey, gvec) in ((wq3, "wq3", qn, "qn", gq), (wk3, "wk3", kn, "kn", gk)):
                    for blk in range(4):
                        pjobs.append((wt, wkey, dst, dkey, gvec, slice(blk * 512, (blk + 1) * 512)))
                pst = {}

                def emit_P(ji):
                    wt, wkey, dst, dkey, gvec, bs = pjobs[ji]
                    pb, pbk = prR.next()
                    a32_, a32k = a32R.next()
                    tq_, tqk = tqR.next()
                    for k in range(8):
                        S.op("pe", lambda e, k=k, bs=bs, wt=wt, pb=pb: e.matmul(pb, lhsT=wt[:, k, :], rhs=hT[:, k, bs], start=(k == 0), stop=(k == 7)),
                             reads=[wkey] + hT_keys, writes=[pbk])
                    S.op("act", lambda e, pb=pb, a32_=a32_: e.activation(out=a32_, in_=pb, func=AF.Copy), reads=[pbk], writes=[a32k])
                    S.op("act", lambda e, pb=pb, tq_=tq_: e.activation(out=tq_, in_=pb, func=AF.Square), reads=[pbk], writes=[tqk])
                    pst[ji] = (a32_, a32k, tq_, tqk)

                def emit_N(ji):
                    wt, wkey, dst, dkey, gvec, bs = pjobs[ji]
                    a32_, a32k, tq_, tqk = pst[ji]
                    nbk, nbkk = nrR.next()
                    S.op("pe", lambda e, tq_=tq_, nbk=nbk: e.matmul(nbk, lhsT=blk64, rhs=tq_, start=True, stop=True), reads=[tqk], writes=[nbkk])
                    S.op("act", lambda e, tq_=tq_, nbk=nbk: e.activation(out=tq_, in_=nbk, func=AF.Ln, bias=EPS), reads=[nbkk], writes=[tqk])
                    S.op("act", lambda e, tq_=tq_: e.activation(out=tq_, in_=tq_, func=AF.Exp, scale=-0.5), reads=[tqk], writes=[tqk])
                    S.op("dve", lambda e, bs=bs, dst=dst, gvec=gvec, a32_=a32_, tq_=tq_: e.scalar_tensor_tensor(out=dst[:, bs], in0=a32_, scalar=gvec[:, 0:1], in1=tq_,
                                                                                                           op0=ALU.mult, op1=ALU.mult),
                         reads=[a32k, tqk], writes=[dkey])
                emit_P(0)
                for ji in range(len(pjobs)):
                    if ji + 1 < len(pjobs):
                        emit_P(ji + 1)
                    emit_N(ji)
                for blk in range(4):
                    bs = slice(blk * 512, (blk + 1) * 512)
                    pb, pbk = prR.next()
                    for k in range(8):
                        S.op("pe", lambda e, k=k, bs=bs, pb=pb: e.matmul(pb, lhsT=wv3[:, k, :], rhs=hT[:, k, bs], start=(k == 0), stop=(k == 7)),
                             reads=["wv3"] + hT_keys, writes=[pbk])
                    S.op("act", lambda e, bs=bs, pb=pb: e.activation(out=av[:, bs], in_=pb, func=AF.Copy), reads=[pbk], writes=["av"])
                for p, d in enumerate((1, 4, 16)):
                    L = S_LEN // d
                    if d == 1:
                        qv, kv, vv = qn, kn, av
                        qk_, kk_, vk_ = "qn", "kn", "av"
                    else:
                        S.op("pool", lambda e, d=d: e.tensor_copy(out=qd.rearrange("p (d l) -> p d l", d=d), in_=qn.rearrange("p (l d) -> p d l", d=d)),
                             reads=["qn"], writes=["qd"])
                        S.op("pool", lambda e, d=d: e.tensor_copy(out=kd.rearrange("p (d l) -> p d l", d=d), in_=kn.rearrange("p (l d) -> p d l", d=d)),
                             reads=["kn"], writes=["kd"])
                        S.op("pool", lambda e, d=d: e.tensor_copy(out=avd.rearrange("p (d l) -> p d l", d=d), in_=av.rearrange("p (l d) -> p d l", d=d)),
                             reads=["av"], writes=["avd"])
                        qv, kv, vv = qd, kd, avd
                        qk_, kk_, vk_ = "qd", "kd", "avd"
                    for i4 in range(4):
                        for ii in range(4):
                            i = i4 * 4 + ii
                            S.op("pe", lambda e, i=i, ii=ii, vv=vv: e.matmul(PS[0][:, ii * 128:(ii + 1) * 128], lhsT=vv[:, i * 128:(i + 1) * 128], rhs=identb,
                                                                               start=True, stop=True),
                                 reads=[vk_], writes=[PB(0)])
                        S.op("act", lambda e, i4=i4: e.activation(out=VpA[:, i4 * 4:(i4 + 1) * 4, 0:64], in_=PS[0].rearrange("p (a b) -> p a b", a=4)[:, :, 0:64], func=AF.Copy),
                             reads=[PB(0)], writes=["VpA"])
                        S.op("dve", lambda e, i4=i4: e.tensor_copy(out=VpB[:, i4 * 4:(i4 + 1) * 4, 64:128], in_=PS[0].rearrange("p (a b) -> p a b", a=4)[:, :, 64:128]),
                             reads=[PB(0), "VpA"], writes=["VpB"])
                    nkt = L // 128
                    for qb in range(4):
                        S.op("dve", lambda e: e.memset(PS[2], 0.0), writes=[PB(2)])
                        S.op("dve", lambda e: e.memset(PS[3], 0.0), writes=[PB(3)])
                        if L >= 512:
                            phases = [(qb * 512) // L]
                        else:
                            phases = list(range((qb * 512) // L, (qb * 512 + 512) // L))
                        tiles = []
                        for r in phases:
                            base = r * L
                            blo = max(qb * 512, base) - base
                            bhi = min(qb * 512 + 512, base + L) - base
                            for n in range(nkt):
                                qlo = max(blo, 128 * n - 64)
                                qhi = min(bhi, 128 * n + 192)
                                if qhi <= qlo:
                                    continue
                                nq = qhi - qlo
                                toff = qlo - (128 * n - 64)
                                gk0 = base + 128 * n
                                gq0 = base + qlo
                                col0 = gq0 - qb * 512
                                tiles.append((nq, toff, gk0, gq0, col0))
                        stt = {}

                        def emit_qk(t, tiles=tiles, stt=stt, kv=kv, qv=qv, kk_=kk_, qk_=qk_):
                            nq, toff, gk0, gq0, col0 = tiles[t]
                            std, bka, bkb = stA_items[stA_i[0] % 2]
                            stA_i[0] += 1
                            st3 = std.rearrange("p (a b) -> p a b", a=2)
                            for hh in range(2):
                                ps_ = slice(64 * hh, 64 * hh + 64)
                                S.op("pe", lambda e, ps_=ps_, hh=hh, gk0=gk0, gq0=gq0, nq=nq, st3=st3: e.matmul(
                                    st3[:, hh, 0:nq], lhsT=kv[ps_, gk0:gk0 + 128], rhs=qv[ps_, gq0:gq0 + nq], start=True, stop=True),
                                    reads=[kk_, qk_], writes=[PB(bka if hh == 0 else bkb)])
                            stt[t] = (st3, bka, bkb)
                        if tiles:
                            emit_qk(0)
                        for t in range(len(tiles)):
                            if t + 1 < len(tiles):
                                emit_qk(t + 1)
                            nq, toff, gk0, gq0, col0 = tiles[t]
                            st3, bka, bkb = stt[t]
                            Et, Etk = etR.next()
                            Pt, Ptk = ptR.next()
                            S.op("act", lambda e, st3=st3, nq=nq, Et=Et: e.activation(out=Et[:, :, 0:nq], in_=st3[:, :, 0:nq], func=AF.Exp, scale=0.125),
                                 reads=[PB(bka), PB(bkb)], writes=[Etk])
                            S.op("dve", lambda e, nq=nq, p=p, toff=toff, Et=Et, Pt=Pt: e.tensor_tensor(out=Pt[:, :, 0:nq], in0=Et[:, :, 0:nq],
                                                                                                       in1=tabs[:, p, :, toff:toff + nq], op=ALU.mult),
                                 reads=[Etk, "tabs"], writes=[Ptk])
                            ti = gk0 // 128
                            S.op("pe", lambda e, ti=ti, col0=col0, nq=nq, Pt=Pt: e.matmul(PS[2][:, col0:col0 + nq], lhsT=VpA[:, ti, :], rhs=Pt[:, 0, 0:nq],
                                                                                         start=False, stop=False, skip_group_check=True),
                                 reads=["VpA", Ptk], writes=[PB(2)])
                            S.op("pe", lambda e, ti=ti, col0=col0, nq=nq, Pt=Pt: e.matmul(PS[3][:, col0:col0 + nq], lhsT=VpB[:, ti, :], rhs=Pt[:, 1, 0:nq],
                                                                                         start=False, stop=False, skip_group_check=True),
                                 reads=["VpB", Ptk], writes=[PB(3)])
                        if d == 1:
                            S.op("act", lambda e, qb=qb: e.activation(out=accN[:, qb * 512:(qb + 1) * 512], in_=PS[2], func=AF.Copy), reads=[PB(2)], writes=["accN"])
                            S.op("dve", lambda e, qb=qb: e.tensor_copy(out=accD[:, qb * 512:(qb + 1) * 512], in_=PS[3]), reads=[PB(3)], writes=["accD"])
                        else:
                            npb = 512 // L if L < 512 else 1
                            r0 = (qb * 512) // L
                            for (acc, ak, bank) in ((accN, "accN", 2), (accD, "accD", 3)):
                                if npb == 1:
                                    view = acc.rearrange("p (l d) -> p d l", d=d)[:, r0, :]
                                    pin = PS[bank]
                                else:
                                    view = acc.rearrange("p (l d) -> p d l", d=d)[:, r0:r0 + npb, :]
                                    pin = PS[bank].rearrange("p (a b) -> p a b", a=npb)
                                S.op("dve", lambda e, view=view, pin=pin: e.tensor_tensor(out=view, in0=pin, in1=view, op=ALU.add), reads=[PB(bank), ak], writes=[ak])
                if b == 0 and c == 0:
                    dump("tabs", tabs, "tabs")
                    dump("accN", accN, "accN")
                    dump("accD", accD, "accD")
                    if "qn" in dbg:
                        S.dma("pool", lambda e: e.dma_start(out=dbg["qn"], in_=qn), reads=["qn"], writes=["dbg_qn"])
                        S.dma("pool", lambda e: e.dma_start(out=dbg["kn"], in_=kn), reads=["kn"], writes=["dbg_kn"])
                        S.dma("pool", lambda e: e.dma_start(out=dbg["av"], in_=av), reads=["av"], writes=["dbg_av"])
                for blk in range(4):
                    bs = slice(blk * 512, (blk + 1) * 512)
                    S.op("pe", lambda e, bs=bs: e.matmul(PS[0], lhsT=selab[:, 0, :], rhs=accN[:, bs], start=True, stop=False), reads=["accN"], writes=[PB(0)])
                    S.op("pe", lambda e, bs=bs: e.matmul(PS[0], lhsT=selab[:, 1, :], rhs=accD[:, bs], start=False, stop=True), reads=["accD"], writes=[PB(0)])
                    S.op("act", lambda e: e.activation(out=recb, in_=PS[0], func=AF.Ln), reads=[PB(0)], writes=["recb"])
                    S.op("act", lambda e: e.activation(out=recb, in_=recb, func=AF.Exp, scale=-1.0), reads=["recb"], writes=["recb"])
                    S.op("dve", lambda e, c=c, bs=bs: e.tensor_tensor(out=catT[0:64, 4 + c, bs], in0=accN[0:64, bs], in1=recb[0:64, :], op=ALU.mult),
                         reads=["accN", "recb"], writes=[("catT", 4 + c)])
                    S.op("dve", lambda e, c=c, bs=bs: e.tensor_tensor(out=catT[64:128, 4 + c, bs], in0=accD[64:128, bs], in1=recb[64:128, :], op=ALU.mult),
                         reads=["accD", "recb"], writes=[("catT", 4 + c)])
            if b == 0 and "catT" in dbg:
                S.dma("pool", lambda e: e.dma_start(out=dbg["catT"], in_=catT), reads=[("catT", k) for k in range(8)], writes=["dbg_catT"])
            if max_phase < 5:
                S.barrier()
                continue
            S.barrier()
            A.release(a_mark)
            A.release(pers_mark)

            h2tok = A.alloc("h2tok", [128, NT, D], BF16)
            afft = A.alloc("afft", [128, NT, 16], F32)
            slott = A.alloc("slott", [128, NT, 16], F32)
            slotv = A.alloc("slotv", [16, S_LEN], F32)
            f_mark = A.mark()
            wo = A.alloc("wo", [128, 8, D], BF16)
            h2 = A.alloc("h2", [128, D], F32)
            h2T = A.alloc("h2T", [128, 8, 128], F32)
            g1bc = A.alloc("g1bc", [128, D], F32)
            a2bc = A.alloc("a2bc", [128, D], F32)
            b2bc = A.alloc("b2bc", [128, D], F32)
            affT = A.alloc("affT", [16, S_LEN], F32)
            work = A.alloc("work", [16, S_LEN], F32)
            mx8 = A.alloc("mx8", [16, 8], F32)
            onesr = A.alloc("onesr2", [16, S_LEN], F32)
            for k in range(8):
                S.dma("pool", lambda e, k=k: e.dma_start(out=wo[:, k, :], in_=wout_v[:, k, :]), writes=[("wo", k)])
            wo_keys = [("wo", k) for k in range(8)]
            S.dma("sp", lambda e: e.dma_start(out=g1bc, in_=modrow_d[b:b + 1, 0:1024].partition_broadcast(128)), reads=["modrow_d"], writes=["g1bc"])
            S.dma("sp", lambda e: e.dma_start(out=b2bc, in_=modrow_d[b:b + 1, 1024:2048].partition_broadcast(128)), reads=["modrow_d"], writes=["b2bc"])
            S.dma("sp", lambda e: e.dma_start(out=a2bc, in_=modrow_d[b:b + 1, 2048:3072].partition_broadcast(128)), reads=["modrow_d"], writes=["a2bc"])
            S.dma("sp", lambda e: e.dma_start(out=h2, in_=g2_d.partition_broadcast(128)), writes=["h2"])
            S.op("dve", lambda e: e.scalar_tensor_tensor(out=a2bc, in0=a2bc, scalar=1.0, in1=h2, op0=ALU.add, op1=ALU.mult), reads=["a2bc", "h2"], writes=["a2bc"])
            S.op("pool", lambda e: e.memset(onesr, 1.0), writes=["onesr2"])
            cat_keys = [("catT", k) for k in range(8)]
            xtR = Ring("xtr", 2, [128, D], F32)
            x1R = Ring("x1r", 2, [128, D], F32)
            h2R = Ring("h2r", 3, [128, D], F32)
            stR5 = Ring("st2r", 3, [128, 8], F32)
            lgR = Ring("lgr", 2, [128, 16], F32)
            opb = [(0, 1), (0, 1)]
            h2T_b = A.alloc("h2Tb", [128, 8, 128], F32)
            tpb = [(2, 3), (6, 7)]
            stA5 = {}

            def emit_A(i):
                ts_ = slice(i * 128, (i + 1) * 128)
                xt_, xtk = xtR.next()
                x1_, x1k = x1R.next()
                h2_, h2k_ = h2R.next()
                st_, stk_ = stR5.next()
                load("sp", xt_, x_d[b, ts_, :], xtk)
                for half in range(2):
                    hs = slice(half * 512, (half + 1) * 512)
                    bank = opb[i % 2][half]
                    for k in range(8):
                        S.op("pe", lambda e, k=k, ts_=ts_, hs=hs, bank=bank: e.matmul(PS[bank], lhsT=catT[:, k, ts_], rhs=wo[:, k, hs], start=(k == 0), stop=(k == 7)),
                             reads=cat_keys + wo_keys, writes=[PB(bank)])
                    S.op("dve", lambda e, hs=hs, bank=bank, x1_=x1_: e.tensor_tensor(out=x1_[:, hs], in0=PS[bank], in1=g1bc[:, hs], op=ALU.mult), reads=[PB(bank), "g1bc"], writes=[x1k])
                S.op("pool", lambda e, x1_=x1_, xt_=xt_: e.tensor_tensor(out=x1_, in0=x1_, in1=xt_, op=ALU.add), reads=[x1k, xtk], writes=[x1k])
                S.dma("sp", lambda e, ts_=ts_, x1_=x1_: e.dma_start(out=out_d[b, ts_, :], in_=x1_), reads=[x1k], writes=[("outd", b, i)])
                S.op("act", lambda e, x1_=x1_, h2_=h2_, st_=st_: e.activation(out=h2_, in_=x1_, func=AF.Square, accum_out=st_[:, 0:1]), reads=[x1k], writes=[h2k_, stk_])
                S.op("act", lambda e, st_=st_: e.activation(out=st_[:, 1:2], in_=st_[:, 0:1], func=AF.Ln, scale=1.0 / D, bias=EPS), reads=[stk_], writes=[stk_])
                S.op("act", lambda e, st_=st_: e.activation(out=st_[:, 2:3], in_=st_[:, 1:2], func=AF.Exp, scale=-0.5), reads=[stk_], writes=[stk_])
                S.op("dve", lambda e, x1_=x1_, h2_=h2_, st_=st_: e.scalar_tensor_tensor(out=h2_, in0=x1_, scalar=st_[:, 2:3], in1=a2bc, op0=ALU.mult, op1=ALU.mult),
                     reads=[x1k, stk_, "a2bc"], writes=[h2k_])
                S.op("pool", lambda e, h2_=h2_: e.tensor_tensor(out=h2_, in0=h2_, in1=b2bc, op=ALU.add), reads=[h2k_, "b2bc"], writes=[h2k_])
                S.op("act", lambda e, i=i, h2_=h2_: e.activation(out=h2tok[:, i, :], in_=h2_, func=AF.Copy), reads=[h2k_], writes=[("h2tok", i)])
                stA5[i] = (h2_, h2k_, st_, stk_)

            def emit_B(i):
                ts_ = slice(i * 128, (i + 1) * 128)
                h2_, h2k_, st_, stk_ = stA5[i]
                lg_, lgk = lgR.next()
                tb0, tb1 = tpb[i % 2]
                hT_ = h2T if i % 2 == 0 else h2T_b
                hTk = "h2T" if i % 2 == 0 else "h2Tb"
                for k in range(8):
                    bank = tb0 if k < 4 else tb1
                    S.op("pe", lambda e, k=k, bank=bank, h2_=h2_: e.matmul(PS[bank][:, (k % 4) * 128:(k % 4 + 1) * 128], lhsT=h2_[:, k * 128:(k + 1) * 128], rhs=ident,
                                                                            start=True, stop=True), reads=[h2k_], writes=[PB(bank)])
                S.op("dve", lambda e, hT_=hT_, tb0=tb0: e.tensor_copy(out=hT_[:, 0:4, :], in_=PS[tb0].rearrange("p (a b) -> p a b", a=4)), reads=[PB(tb0)], writes=[hTk])
                S.op("act", lambda e, hT_=hT_, tb1=tb1: e.activation(out=hT_[:, 4:8, :], in_=PS[tb1].rearrange("p (a b) -> p a b", a=4), func=AF.Copy), reads=[PB(tb1)], writes=[hTk])
                for k in range(8):
                    S.op("pe", lambda e, k=k, hT_=hT_: e.matmul(PS[4][:, 0:16], lhsT=hT_[:, k, :], rhs=wr[:, k, :], start=(k == 0), stop=(k == 7)), reads=[hTk], writes=[PB(4)])
                S.op("dve", lambda e, lg_=lg_: e.tensor_tensor(out=lg_, in0=PS[4][:, 0:16], in1=brbc, op=ALU.add), reads=[PB(4)], writes=[lgk])
                S.op("dve", lambda e, lg_=lg_, st_=st_: e.tensor_reduce(out=st_[:, 3:4], in_=lg_, axis=AX.X, op=ALU.max), reads=[lgk], writes=[stk_])
                S.op("dve", lambda e, st_=st_: e.tensor_scalar(out=st_[:, 4:5], in0=st_[:, 3:4], scalar1=-1.0, scalar2=None, op0=ALU.mult), reads=[stk_], writes=[stk_])
                S.op("act", lambda e, lg_=lg_, st_=st_: e.activation(out=lg_, in_=lg_, func=AF.Exp, bias=st_[:, 4:5], accum_out=st_[:, 5:6]), reads=[lgk, stk_], writes=[lgk, stk_])
                S.op("dve", lambda e, st_=st_: e.reciprocal(out=st_[:, 6:7], in_=st_[:, 5:6]), reads=[stk_], writes=[stk_])
                S.op("dve", lambda e, i=i, lg_=lg_, st_=st_: e.tensor_scalar(out=afft[:, i, :], in0=lg_, scalar1=st_[:, 6:7], scalar2=None, op0=ALU.mult), reads=[lgk, stk_], writes=[("afft", i)])
                S.op("pe", lambda e, i=i: e.matmul(PS[5][0:16, 0:128], lhsT=afft[:, i, :], rhs=ident, start=True, stop=True), reads=[("afft", i)], writes=[PB(5)])
                S.op("act", lambda e, ts_=ts_: e.activation(out=affT[:, ts_], in_=PS[5][0:16, 0:128], func=AF.Copy), reads=[PB(5)], writes=["affT"])

            emit_A(0)
            for i in range(NT):
                if i + 1 < NT:
                    emit_A(i + 1)
                emit_B(i)
            S.op("dve", lambda e: e.tensor_copy(out=work, in_=affT), reads=["affT"], writes=["work"])
            for rnd in range(CAP // 8):
                S.op("dve", lambda e: e.max(out=mx8, in_=work), reads=["work"], writes=["mx8"])
                S.op("dve", lambda e: e.match_replace(out=work, in_to_replace=mx8, in_values=work, imm_value=0.0), reads=["work", "mx8"], writes=["work"])
            S.op("dve", lambda e: e.tensor_tensor(out=work, in0=affT, in1=work, op=ALU.subtract), reads=["affT", "work"], writes=["work"])
            S.op("dve", lambda e: e.tensor_scalar(out=work, in0=work, scalar1=0.0, scalar2=None, op0=ALU.is_gt), reads=["work"], writes=["work"])
            S.op("dve", lambda e: e.tensor_tensor_scan(out=slotv, data0=onesr, data1=work, initial=0.0, op0=ALU.mult, op1=ALU.add), reads=["onesr2", "work"], writes=["slotv"])
            S.op("dve", lambda e: e.tensor_tensor(out=slotv, in0=slotv, in1=work, op=ALU.mult), reads=["slotv", "work"], writes=["slotv"])
            S.op("dve", lambda e: e.tensor_scalar(out=slotv, in0=slotv, scalar1=-1.0, scalar2=None, op0=ALU.add), reads=["slotv"], writes=["slotv"])
            for i in range(NT):
                S.op("pe", lambda e, i=i: e.matmul(PS[6][:, i * 16:(i + 1) * 16], lhsT=slotv[:, i * 128:(i + 1) * 128], rhs=ident[0:16, 0:16], start=True, stop=True),
                     reads=["slotv"], writes=[PB(6)])
            S.op("dve", lambda e: e.tensor_copy(out=slott, in_=PS[6][:, 0:256].rearrange("p (a b) -> p a b", a=NT)), reads=[PB(6)], writes=["slott"])
            if b == 0:
                dump("slotv", slotv, "slotv")
                dump("affT", affT, "affT")
                if "h2tok" in dbg:
                    S.dma("pool", lambda e: e.dma_start(out=dbg["h2tok"], in_=h2tok), reads=[("h2tok", i) for i in range(NT)], writes=["dbg_h2tok"])
            if max_phase < 6:
                S.barrier()
                continue
            S.barrier()
            A.release(f_mark)

            A.limit = ARENA_BYTES
            yacc = A.alloc("yacc", [128, NT, D], F32)
            y_mark = A.mark()
            Pm = A.alloc("Pm", [128, NT, CAP], BF16)
            PTm = A.alloc("PTm", [128, 2, S_LEN], BF16)
            xin = A.alloc("xin", [128, 8, CAP], BF16)
            wR = Ring("wbuf", 4, [128, 4096], BF16)
            hid = A.alloc("hid", [128, 16, CAP], BF16)
            yex = A.alloc("yex", [128, 2, D], BF16)
            sgR = Ring("sg", 2, [128, CAP], F32)
            fR2 = Ring("ffps", 0, banks=[0, 1, 2, 3])
            h2k = [("h2tok", i) for i in range(NT)]
            for ex in range(NEXP):
                for i in range(NT):
                    S.op("dve", lambda e, i=i, ex=ex: e.tensor_scalar(out=Pm[:, i, :], in0=iotaf, scalar1=slott[:, i, ex:ex + 1], scalar2=None, op0=ALU.is_equal),
                         reads=["slott"], writes=["Pm"])
                for blk in range(4):
                    bs = slice(blk * 512, (blk + 1) * 512)
                    pb, pbk = fR2.next()
                    S.op("pe", lambda e, ex=ex, bs=bs, pb=pb: e.matmul(pb, lhsT=sele[:, ex, :], rhs=slotv[:, bs], start=True, stop=True), reads=["slotv"], writes=[pbk])
                    for ch in range(2):
                        S.op("dve", lambda e, ch=ch, bs=bs, pb=pb: e.tensor_scalar(out=PTm[:, ch, bs], in0=pb, scalar1=iotap[:, ch:ch + 1], scalar2=None, op0=ALU.is_equal),
                             reads=[pbk], writes=["PTm"])
                for k2 in range(4):
                    pb, pbk = fR2.next()
                    for kk in range(2):
                        k = k2 * 2 + kk
                        for i in range(NT):
                            S.op("pe", lambda e, k=k, kk=kk, i=i, pb=pb: e.matmul(pb[:, kk * 256:(kk + 1) * 256], lhsT=h2tok[:, i, k * 128:(k + 1) * 128], rhs=Pm[:, i, :],
                                                                                  start=(i == 0), stop=(i == NT - 1)),
                                 reads=h2k + ["Pm"], writes=[pbk])
                    S.op("act", lambda e, k2=k2, pb=pb: e.activation(out=xin[:, 2 * k2:2 * k2 + 2, :], in_=pb.rearrange("p (a b) -> p a b", a=2), func=AF.Copy),
                         reads=[pbk], writes=["xin"])
                for fb in range(4):
                    wg, wgk = wR.next()
                    wg = wg.rearrange("p (k f) -> p k f", k=8)
                    S.dma("pool", lambda e, ex=ex, fb=fb, wg=wg: e.dma_start(out=wg, in_=wg_d[ex].rearrange("(k p) f -> p k f", p=128)[:, :, fb * 512:(fb + 1) * 512]), writes=[wgk])
                    wu, wuk = wR.next()
                    wu = wu.rearrange("p (k f) -> p k f", k=8)
                    S.dma("pool", lambda e, ex=ex, fb=fb, wu=wu: e.dma_start(out=wu, in_=wu_d[ex].rearrange("(k p) f -> p k f", p=128)[:, :, fb * 512:(fb + 1) * 512]), writes=[wuk])
                    for fc in range(4):
                        f = fb * 4 + fc
                        pb, pbk = fR2.next()
                        for k in range(8):
                            S.op("pe", lambda e, k=k, fc=fc, pb=pb, wg=wg: e.matmul(pb[:, 0:CAP], lhsT=wg[:, k, fc * 128:(fc + 1) * 128], rhs=xin[:, k, :], start=(k == 0), stop=(k == 7)),
                                 reads=[wgk, "xin"], writes=[pbk])
                        for k in range(8):
                            S.op("pe", lambda e, k=k, fc=fc, pb=pb, wu=wu: e.matmul(pb[:, CAP:2 * CAP], lhsT=wu[:, k, fc * 128:(fc + 1) * 128], rhs=xin[:, k, :], start=(k == 0), stop=(k == 7)),
                                 reads=[wuk, "xin"], writes=[pbk])
                        sg, sgk = sgR.next()
                        S.op("act", lambda e, pb=pb, sg=sg: e.activation(out=sg, in_=pb[:, 0:CAP], func=AF.Silu), reads=[pbk], writes=[sgk])
                        S.op("dve", lambda e, f=f, pb=pb, sg=sg: e.tensor_tensor(out=hid[:, f, :], in0=pb[:, CAP:2 * CAP], in1=sg, op=ALU.mult), reads=[pbk, sgk], writes=["hid"])
                for fb in range(4):
                    wd, wdk = wR.next()
                    wd = wd.rearrange("p (k n) -> p k n", k=4)
                    S.dma("pool", lambda e, ex=ex, fb=fb, wd=wd: e.dma_start(out=wd, in_=wd_d[ex].rearrange("(k p) n -> p k n", p=128)[:, fb * 4:(fb + 1) * 4, :]), writes=[wdk])
                    for fc in range(4):
                        f = fb * 4 + fc
                        for ct in range(2):
                            for dh in range(2):
                                bank = 4 + ct * 2 + dh
                                S.op("pe", lambda e, f=f, fc=fc, ct=ct, dh=dh, bank=bank, wd=wd: e.matmul(PS[bank], lhsT=hid[:, f, ct * 128:(ct + 1) * 128],
                                                                                                          rhs=wd[:, fc, dh * 512:(dh + 1) * 512], start=(f == 0), stop=(f == 15)),
                                     reads=["hid", wdk], writes=[PB(bank)])
                for ct in range(2):
                    for dh in range(2):
                        bank = 4 + ct * 2 + dh
                        if dh == 0:
                            S.op("act", lambda e, ct=ct, dh=dh, bank=bank: e.activation(out=yex[:, ct, dh * 512:(dh + 1) * 512], in_=PS[bank], func=AF.Copy),
                                 reads=[PB(bank)], writes=["yex"])
                        else:
                            S.op("dve", lambda e, ct=ct, dh=dh, bank=bank: e.tensor_copy(out=yex[:, ct, dh * 512:(dh + 1) * 512], in_=PS[bank]), reads=[PB(bank)], writes=["yex"])
                for i in range(NT):
                    for dh in range(2):
                        pb, pbk = fR2.next()
                        for ch in range(2):
                            S.op("pe", lambda e, i=i, dh=dh, ch=ch, pb=pb: e.matmul(pb, lhsT=PTm[:, ch, i * 128:(i + 1) * 128], rhs=yex[:, ch, dh * 512:(dh + 1) * 512],
                                                                                    start=(ch == 0), stop=(ch == 1)),
                                 reads=["PTm", "yex"], writes=[pbk])
                        if ex == 0:
                            S.op("dve", lambda e, i=i, dh=dh, pb=pb, ex=ex: e.tensor_scalar(out=yacc[:, i, dh * 512:(dh + 1) * 512], in0=pb, scalar1=afft[:, i, ex:ex + 1],
                                                                                            scalar2=None, op0=ALU.mult),
                                 reads=[pbk], writes=[("yacc", i, dh)])
                        else:
                            S.op("dve", lambda e, i=i, dh=dh, pb=pb, ex=ex: e.scalar_tensor_tensor(out=yacc[:, i, dh * 512:(dh + 1) * 512], in0=pb, scalar=afft[:, i, ex:ex + 1],
                                                                                                   in1=yacc[:, i, dh * 512:(dh + 1) * 512], op0=ALU.mult, op1=ALU.add),
                                 reads=[pbk, ("yacc", i, dh)], writes=[("yacc", i, dh)])
            S.barrier()
            A.release(y_mark)
            g2bc = A.alloc("g2bc", [128, D], F32)
            xt = A.alloc("xt", [128, D], F32)
            ot = A.alloc("ot", [128, D], F32)
            S.dma("sp", lambda e: e.dma_start(out=g2bc, in_=modrow_d[b:b + 1, 3072:4096].partition_broadcast(128)), reads=["modrow_d"], writes=["g2bc"])
            for i in range(NT):
                ts_ = slice(i * 128, (i + 1) * 128)
                S.dma("sp", lambda e, ts_=ts_: e.dma_start(out=xt, in_=out_d[b, ts_, :]), reads=[("outd", b, i)], writes=["xt"])
                S.op("dve", lambda e, i=i: e.tensor_tensor(out=ot, in0=yacc[:, i, :], in1=g2bc, op=ALU.mult), reads=[("yacc", i, 0), ("yacc", i, 1), "g2bc"], writes=["ot"])
                S.op("pool", lambda e: e.tensor_tensor(out=ot, in0=ot, in1=xt, op=ALU.add), reads=["ot", "xt"], writes=["ot"])
                S.dma("sp", lambda e, ts_=ts_: e.dma_start(out=out_d[b, ts_, :], in_=ot), reads=["ot", ("outd", b, i)], writes=[("outd", b, i)])
            S.barrier()

        S.barrier()
        print("arena peak bytes", A.peak, "instr counts", {e: len(v) for e, v in S.prog.items()})
        with nc.Block() as block:
            S.emit(block)
    return nc


def _t5_bucket(rel):
    half, exact = 16, 8
    n = np.abs(rel)
    log_ratio = np.log(np.maximum(n, 1).astype(np.float32) / exact) / math.log(1024 / exact)
    large = np.minimum(exact + (log_ratio * (half - exact)).astype(np.int32), half - 1)
    return np.where(rel > 0, half, 0) + np.where(n < exact, n, large)


def _consts():
    c = {}
    c["c_ident"] = np.eye(128, dtype=np.float32)
    c["c_jrev"] = np.ascontiguousarray(np.eye(128, dtype=np.float32)[::-1])
    selab = np.zeros((128, 2, 128), np.float32)
    for m in range(64):
        selab[m + 64, 0, m] = 1.0
        selab[m, 1, m + 64] = 1.0
    c["c_selab"] = selab
    x = np.arange(896)[None, :] - 384
    kp = np.arange(128)[:, None]
    c["c_maskF"] = np.where(x >= kp, 0.0, NEG).astype(np.float32)
    c["c_maskB"] = np.where(x <= kp, 0.0, NEG).astype(np.float32)
    c["c_iotaf"] = np.broadcast_to(np.arange(256, dtype=np.float32)[None, :], (128, 256)).copy()
    c["c_iotap"] = np.stack([np.arange(128, dtype=np.float32), np.arange(128, dtype=np.float32) + 128], axis=1)
    selfb = np.zeros((16, 8, 128), np.float32)
    for h in range(4):
        selfb[4 + h, h, :] = -1.0
        selfb[12 + h, 4 + h, :] = 1.0
    c["c_selfb"] = selfb
    sele = np.zeros((16, 16, 128), np.float32)
    for e in range(16):
        sele[e, e, :] = 1.0
    c["c_sele"] = sele
    sel2 = np.zeros((2, 2, 128), np.float32)
    sel2[0, 0, :] = 1.0
    sel2[1, 1, :] = 1.0
    c["c_sel2"] = sel2
    comb = np.zeros((16, 3, 8), np.float32)
    for h in range(4):
        comb[h, 0, h] = 1.0
        comb[4 + h, 1, h] = 1.0
        comb[8 + h, 0, 4 + h] = 1.0
        comb[12 + h, 1, 4 + h] = -1.0
        comb[12 + h, 2, 4 + h] = 1.0
    c["c_comb"] = comb
    c["c_ones128"] = np.full((128, 128), 1.0 / 128.0, np.float32)
    blk = np.zeros((128, 128), np.float32)
    blk[0:64, 0:64] = 1.0 / 64.0
    blk[64:128, 64:128] = 1.0 / 64.0
    c["c_blk64"] = blk
    oh = np.zeros((32, 3, 384), np.float32)
    for p, d in enumerate((1, 4, 16)):
        for y in range(0, 129):
            rel = 64 - y
            bkt = int(_t5_bucket(np.array(rel * d)))
            oh[bkt, p, 127 + y] = 1.0
    c["c_onehot"] = oh
    return c


_NC_CACHE = {}


def _blockdiag(wblk):
    out = np.zeros((4, 128, 128), np.float32)
    for h in range(4):
        for g in range(32):
            out[h, 4 * g:4 * g + 4, 4 * g:4 * g + 4] = wblk[32 * h + g]
    return out


def make_in_maps(inputs, n_cores=8):
    f = lambda a: np.ascontiguousarray(np.asarray(a, dtype=np.float32))
    x = f(inputs["x"]); c = f(inputs["c"])
    shared = {}
    shared["w_ada"] = f(inputs["w_ada"][0])
    shared["b_ada_fm"] = f(inputs["b_ada"][0].reshape(48, 128).T)
    shared["b_ada_row"] = f(inputs["b_ada"][0].reshape(1, 6 * D))
    shared["g1_fm"] = f(inputs["norm1_g"][0].reshape(8, 128).T)
    shared["g2_row"] = f(inputs["norm2_g"][0].reshape(1, D))
    shared["w_in"] = f(inputs["w_in"][0])
    shared["convw_fm"] = f(np.transpose(inputs["conv_w"][0].reshape(5, 4, 128), (2, 1, 0)))
    shared["convb_fm"] = f(inputs["conv_b"][0].reshape(4, 128).T)
    shared["mng_fm"] = f(inputs["mlstm_norm_g"][0].reshape(4, 128).T)
    shared["mskip_fm"] = f(inputs["mlstm_skip"][0].reshape(4, 128).T)
    shared["wq_bd"] = _blockdiag(np.asarray(inputs["w_q_blk"][0]))
    shared["wk_bd"] = _blockdiag(np.asarray(inputs["w_k_blk"][0]))
    shared["wv_bd"] = _blockdiag(np.asarray(inputs["w_v_blk"][0]))
    bi = np.asarray(inputs["b_igate"][0]); bf = np.asarray(inputs["b_fgate"][0])
    shared["gate_bias"] = f(np.concatenate([bi[0], bf[0], bi[1], bf[1]]).reshape(16, 1))
    shared["gq"] = f(np.tile(np.asarray(inputs["q_norm_g"][0]), 2).reshape(128, 1))
    shared["gk"] = f(np.tile(np.asarray(inputs["k_norm_g"][0]), 2).reshape(128, 1))
    shared["rel_bias"] = f(inputs["rel_bias"])
    shared["w_out"] = f(inputs["w_out"][0])
    shared["wr_fm"] = f(np.transpose(np.asarray(inputs["w_router"][0]).reshape(8, 128, 16), (1, 0, 2)))
    shared["b_router"] = f(inputs["b_router"][0].reshape(1, 16))
    shared["w_gate"] = f(inputs["w_gate"][0])
    shared["w_up"] = f(inputs["w_up"][0])
    shared["w_down"] = f(inputs["w_down"][0])
    shared.update(_consts())
    maps = []
    for i in range(n_cores):
        m = dict(shared)
        m["x"] = np.ascontiguousarray(x[2 * i:2 * i + 2])
        cc = c[2 * i:2 * i + 2]
        m["cT"] = np.ascontiguousarray(np.transpose(cc.reshape(2, 8, 128), (2, 1, 0)))
        maps.append(m)
    return maps


def kernel(**inputs):
    if "nc" not in _NC_CACHE:
        _NC_CACHE["nc"] = build_program()
    nc = _NC_CACHE["nc"]
    maps = make_in_maps(inputs, 8)
    res = run_bass_kernel_spmd(nc, maps, core_ids=list(range(8)))
    out = np.concatenate([np.asarray(r["out"]) for r in res.results], axis=0)
    return out.astype(np.float32)
```
